# Optimizing a Trainium2 kernel written in Bass

```python
import jax, jax.numpy as jnp
from jax import lax
import numpy as np

D_MODEL = 1024
BATCH = 4
SEQ = 8192
DEPTH = 2

M_HEADS = 4
M_HEAD_DIM = D_MODEL // 8
M_WIDTH = M_HEADS * M_HEAD_DIM
A_HEADS = 8
A_HEAD_DIM = D_MODEL // 16
A_WIDTH = A_HEADS * A_HEAD_DIM
D_MIX = M_WIDTH + A_WIDTH
CONV_K = 4
MLSTM_CHUNK = 64
GATE_SOFTCAP = 15.0
MOBA_BLOCK = 256
MOBA_TOPK = 3
MOBA_Q_BLOCK = 64
ROPE_THETA = 500000.0
ROPE_DIM = A_HEAD_DIM // 4
N_EXPERTS = 32
TOP_K = 4
D_FF = D_MODEL
SWIGLU_LIMIT = 7.0
SWIGLU_ALPHA = 1.702
EXPERT_ROWS = 128
NORM_EPS = 1e-5
IN_SPLITS = (M_WIDTH, M_WIDTH, M_WIDTH, M_WIDTH, M_HEADS, M_HEADS, A_WIDTH, A_WIDTH, A_WIDTH)
N_IN = sum(IN_SPLITS)

kernel_name = 'hybrid_mlstm_moba_moe_adaln'


def rms_norm(x, g):
    xf = x.astype(jnp.float32)
    y = xf * lax.rsqrt(jnp.mean(xf * xf, axis=-1, keepdims=True) + NORM_EPS)
    return (y * g.astype(jnp.float32)).astype(x.dtype)


def soft_cap(t):
    t = t.astype(jnp.float32)
    return GATE_SOFTCAP * jnp.tanh(t / GATE_SOFTCAP)


def causal_depthwise_conv(x, w, b):
    y = lax.conv_general_dilated(x, w[:, None, :].astype(x.dtype), window_strides=(1,),
                                 padding=[(CONV_K - 1, 0)],
                                 dimension_numbers=('NWC', 'WIO', 'NWC'),
                                 feature_group_count=x.shape[-1])
    return y + b


def partial_rotary(x, positions):
    half = ROPE_DIM // 2
    inv_freq = ROPE_THETA ** (-jnp.arange(0, ROPE_DIM, 2, dtype=jnp.float32) / ROPE_DIM)
    ang = positions.astype(jnp.float32)[:, None, :, None] * inv_freq
    cos, sin = jnp.cos(ang), jnp.sin(ang)
    xr = x[..., :ROPE_DIM].astype(jnp.float32)
    x1, x2 = xr[..., :half], xr[..., half:]
    rot = jnp.concatenate([x1 * cos - x2 * sin, x2 * cos + x1 * sin], axis=-1)
    return jnp.concatenate([rot.astype(x.dtype), x[..., ROPE_DIM:]], axis=-1)


def mlstm_chunkwise(q, k, v, i_pre, f_pre):
    B, H, S, d = q.shape
    L = MLSTM_CHUNK
    nc = S // L
    f32 = jnp.float32
    q = q.astype(f32).reshape(B, H, nc, L, d)
    k = (k.astype(f32) * d ** -0.5).reshape(B, H, nc, L, d)
    v = v.astype(f32).reshape(B, H, nc, L, d)
    ig = i_pre.astype(f32).reshape(B, H, nc, L)
    b = jnp.cumsum(jax.nn.log_sigmoid(f_pre.astype(f32)).reshape(B, H, nc, L), axis=-1)
    g = b[..., -1]
    w = g[..., None] - b + ig
    wmax = jnp.max(w, axis=-1)
    ew = jnp.exp(w - wmax[..., None])
    kv = jnp.einsum('bhcl,bhcld,bhcle->cbhde', ew, k, v)
    ksum = jnp.einsum('bhcl,bhcld->cbhd', ew, k)

    def step(carry, inp):
        c_st, n_st, m_st = carry
        kv_c, ks_c, g_c, wm_c = inp
        m_new = jnp.maximum(g_c + m_st, wm_c)
        decay = jnp.exp(g_c + m_st - m_new)
        inj = jnp.exp(wm_c - m_new)
        c_new = decay[..., None, None] * c_st + inj[..., None, None] * kv_c
        n_new = decay[..., None] * n_st + inj[..., None] * ks_c
        return (c_new, n_new, m_new), (c_st, n_st, m_st)

    init = (jnp.zeros((B, H, d, d), f32), jnp.zeros((B, H, d), f32), jnp.full((B, H), -jnp.inf, f32))
    _, (c_prev, n_prev, m_prev) = lax.scan(step, init, (kv, ksum, g.transpose(2, 0, 1), wmax.transpose(2, 0, 1)))
    m_prev = m_prev.transpose(1, 2, 0)
    a = b + m_prev[..., None]
    causal = jnp.tril(jnp.ones((L, L), dtype=bool))
    dmat = jnp.where(causal, b[..., :, None] - b[..., None, :] + ig[..., None, :], -jnp.inf)
    m_t = jnp.maximum(a, jnp.max(dmat, axis=-1))
    s = jnp.einsum('bhctd,bhcsd->bhcts', q, k) * jnp.exp(dmat - m_t[..., None])
    inter = jnp.exp(a - m_t)
    num = jnp.einsum('bhcts,bhcse->bhcte', s, v) + inter[..., None] * jnp.einsum('bhctd,cbhde->bhcte', q, c_prev)
    den = jnp.sum(s, axis=-1) + inter * jnp.einsum('bhctd,cbhd->bhct', q, n_prev)
    h = num / jnp.maximum(jnp.abs(den), jnp.exp(-m_t))[..., None]
    return h.reshape(B, H, S, d)


def moba_attention(q, k, v):
    B, H, S, dh = q.shape
    nb = -(-S // MOBA_BLOCK)
    pad = nb * MOBA_BLOCK - S
    kp = jnp.pad(k, ((0, 0), (0, 0), (0, pad), (0, 0)))
    vp = jnp.pad(v, ((0, 0), (0, 0), (0, pad), (0, 0)))
    kb = kp.reshape(B, H, nb, MOBA_BLOCK, dh)
    vb = vp.reshape(B, H, nb, MOBA_BLOCK, dh)
    kmean = jnp.mean(kb.astype(jnp.float32), axis=3)
    n_sel = min(MOBA_TOPK, nb)
    scale = dh ** -0.5
    bi = jnp.arange(B)[:, None, None, None]
    hi = jnp.arange(H)[None, :, None, None]
    blk_ids = jnp.arange(nb)

    def one_query_block(ci):
        start = ci * MOBA_Q_BLOCK
        own = start // MOBA_BLOCK
        qb = lax.dynamic_slice_in_dim(q, start, MOBA_Q_BLOCK, axis=2).astype(jnp.float32)
        gate = jnp.einsum('bhqd,bhnd->bhqn', qb, kmean)
        gate = jnp.where(blk_ids < own, gate, -jnp.inf)
        topv, topi = lax.top_k(gate, n_sel)
        sel_ok = topv > -jnp.inf
        kg = kb[bi, hi, topi]
        vg = vb[bi, hi, topi]
        s_sel = jnp.einsum('bhqd,bhqjkd->bhqjk', qb, kg) * scale
        s_sel = jnp.where(sel_ok[..., None], s_sel, -jnp.inf).reshape(B, H, MOBA_Q_BLOCK, n_sel * MOBA_BLOCK)
        kown = lax.dynamic_slice_in_dim(kp, own * MOBA_BLOCK, MOBA_BLOCK, axis=2)
        vown = lax.dynamic_slice_in_dim(vp, own * MOBA_BLOCK, MOBA_BLOCK, axis=2)
        s_own = jnp.einsum('bhqd,bhkd->bhqk', qb, kown) * scale
        qpos = start + jnp.arange(MOBA_Q_BLOCK)
        kpos = own * MOBA_BLOCK + jnp.arange(MOBA_BLOCK)
        s_own = jnp.where(kpos[None, :] <= qpos[:, None], s_own, -jnp.inf)
        p = jax.nn.softmax(jnp.concatenate([s_sel, s_own], axis=-1), axis=-1)
        p_sel = p[..., :n_sel * MOBA_BLOCK].reshape(B, H, MOBA_Q_BLOCK, n_sel, MOBA_BLOCK)
        out = jnp.einsum('bhqjk,bhqjkd->bhqd', p_sel, vg) + jnp.einsum('bhqk,bhkd->bhqd', p[..., n_sel * MOBA_BLOCK:], vown)
        return out.astype(q.dtype)

    out = lax.map(one_query_block, jnp.arange(S // MOBA_Q_BLOCK))
    return out.transpose(1, 2, 0, 3, 4).reshape(B, H, S, dh)


def hybrid_mixer(h, positions, w_in, conv_w, conv_b, b_igate, b_fgate, mlstm_norm_g, w_out):
    B, S, _ = h.shape
    offsets = np.cumsum(IN_SPLITS)[:-1].tolist()
    z = h @ w_in
    mq, mk, mv, mo, mi, mf, aq, ak, av = jnp.split(z, offsets, axis=-1)

    def heads(t, n, d):
        return t.reshape(B, S, n, d).transpose(0, 2, 1, 3)

    qk = jax.nn.silu(causal_depthwise_conv(jnp.concatenate([mq, mk], axis=-1), conv_w, conv_b))
    mq, mk = jnp.split(qk, 2, axis=-1)
    i_pre = soft_cap(mi + b_igate).transpose(0, 2, 1)
    f_pre = soft_cap(mf + b_fgate).transpose(0, 2, 1)
    hm = mlstm_chunkwise(heads(mq, M_HEADS, M_HEAD_DIM), heads(mk, M_HEADS, M_HEAD_DIM),
                         heads(mv, M_HEADS, M_HEAD_DIM), i_pre, f_pre)
    hm = hm.transpose(0, 2, 1, 3) * jax.nn.sigmoid(mo.astype(jnp.float32)).reshape(B, S, M_HEADS, M_HEAD_DIM)
    hm = rms_norm(hm, mlstm_norm_g.reshape(M_HEADS, M_HEAD_DIM)).reshape(B, S, M_WIDTH).astype(h.dtype)

    qa = partial_rotary(heads(aq, A_HEADS, A_HEAD_DIM), positions)
    ka = partial_rotary(heads(ak, A_HEADS, A_HEAD_DIM), positions)
    ha = moba_attention(qa, ka, heads(av, A_HEADS, A_HEAD_DIM))
    ha = ha.transpose(0, 2, 1, 3).reshape(B, S, A_WIDTH)

    return jnp.concatenate([hm, ha], axis=-1) @ w_out


def moe_ffn(h, w_router, b_router, w_gate, b_gate, w_up, b_up, w_down, b_down):
    B, S, D = h.shape
    T = B * S
    A = T * TOP_K
    xt = h.reshape(T, D)
    logits = (xt @ w_router + b_router).astype(jnp.float32)
    topv, topi = lax.top_k(logits, TOP_K)
    probs = jax.nn.softmax(topv, axis=-1)
    flat_e = topi.reshape(A)
    flat_tok = jnp.arange(A, dtype=jnp.int32) // TOP_K
    flat_p = probs.reshape(A)
    order = jnp.argsort(flat_e)
    se, stok, sp = flat_e[order], flat_tok[order], flat_p[order]
    counts = jnp.bincount(flat_e, length=N_EXPERTS)
    start = jnp.cumsum(counts) - counts
    nblk = (counts + EXPERT_ROWS - 1) // EXPERT_ROWS
    blk_end = jnp.cumsum(nblk)
    blk_start = blk_end - nblk
    slot = blk_start[se] * EXPERT_ROWS + (jnp.arange(A) - start[se])
    n_blocks = (A + EXPERT_ROWS - 1) // EXPERT_ROWS + N_EXPERTS
    P = n_blocks * EXPERT_ROWS
    slot_tok = jnp.zeros((P,), jnp.int32).at[slot].set(stok)
    slot_p = jnp.zeros((P,), jnp.float32).at[slot].set(sp)
    block_e = jnp.minimum(jnp.searchsorted(blk_end, jnp.arange(n_blocks), side='right'), N_EXPERTS - 1)

    def expert_block(args):
        tok, e = args
        xb = xt[tok]
        gt = jnp.minimum(xb @ w_gate[e] + b_gate[e], SWIGLU_LIMIT)
        up = jnp.clip(xb @ w_up[e] + b_up[e], -SWIGLU_LIMIT, SWIGLU_LIMIT)
        act = (up + 1.0) * (gt * jax.nn.sigmoid(SWIGLU_ALPHA * gt))
        return act @ w_down[e] + b_down[e]

    yb = lax.map(expert_block, (slot_tok.reshape(n_blocks, EXPERT_ROWS), block_e))
    yb = yb.reshape(P, D) * slot_p[:, None].astype(yb.dtype)
    y = jnp.zeros((T, D), yb.dtype).at[slot_tok].add(yb)
    return y.reshape(B, S, D).astype(h.dtype)


def setup_inputs(seed: int = 0) -> dict:
    key = jax.random.key(seed)
    ks = jax.random.split(key, 24)
    f32 = jnp.float32

    def nrm(k, shape, scale):
        return jax.random.normal(k, shape, f32) * scale

    x = nrm(ks[0], (BATCH, SEQ, D_MODEL), 1.0)
    c = nrm(ks[1], (BATCH, D_MODEL), 1.0)
    positions = jnp.arange(SEQ, dtype=jnp.int32)[None, :] + jax.random.randint(ks[2], (BATCH, 1), 0, 4096, dtype=jnp.int32)
    w_ada = nrm(ks[3], (DEPTH, D_MODEL, 6 * D_MODEL), 0.5 * D_MODEL ** -0.5)
    b_ada = nrm(ks[4], (DEPTH, 6 * D_MODEL), 0.02)
    norm1_g = 1.0 + nrm(ks[5], (DEPTH, D_MODEL), 0.05)
    w_in = nrm(ks[6], (DEPTH, D_MODEL, N_IN), D_MODEL ** -0.5)
    conv_w = nrm(ks[7], (DEPTH, CONV_K, 2 * M_WIDTH), CONV_K ** -0.5)
    conv_b = nrm(ks[8], (DEPTH, 2 * M_WIDTH), 0.02)
    b_igate = nrm(ks[9], (DEPTH, M_HEADS), 0.1)
    b_fgate = jnp.linspace(3.0, 6.0, M_HEADS, dtype=f32)[None, :] + nrm(ks[10], (DEPTH, M_HEADS), 0.1)
    mlstm_norm_g = 1.0 + nrm(ks[11], (DEPTH, M_WIDTH), 0.05)
    w_out = nrm(ks[12], (DEPTH, D_MIX, D_MODEL), D_MIX ** -0.5)
    norm2_g = 1.0 + nrm(ks[13], (DEPTH, D_MODEL), 0.05)
    w_router = nrm(ks[14], (DEPTH, D_MODEL, N_EXPERTS), D_MODEL ** -0.5)
    b_router = nrm(ks[15], (DEPTH, N_EXPERTS), 0.01)
    w_gate = nrm(ks[16], (DEPTH, N_EXPERTS, D_MODEL, D_FF), D_MODEL ** -0.5)
    b_gate = nrm(ks[17], (DEPTH, N_EXPERTS, D_FF), 0.01)
    w_up = nrm(ks[18], (DEPTH, N_EXPERTS, D_MODEL, D_FF), D_MODEL ** -0.5)
    b_up = nrm(ks[19], (DEPTH, N_EXPERTS, D_FF), 0.01)
    w_down = nrm(ks[20], (DEPTH, N_EXPERTS, D_FF, D_MODEL), D_FF ** -0.5)
    b_down = nrm(ks[21], (DEPTH, N_EXPERTS, D_MODEL), 0.01)
    final_norm_g = 1.0 + nrm(ks[22], (D_MODEL,), 0.05)
    return {'x': x, 'c': c, 'positions': positions, 'w_ada': w_ada, 'b_ada': b_ada, 'norm1_g': norm1_g,
            'w_in': w_in, 'conv_w': conv_w, 'conv_b': conv_b, 'b_igate': b_igate, 'b_fgate': b_fgate,
            'mlstm_norm_g': mlstm_norm_g, 'w_out': w_out, 'norm2_g': norm2_g, 'w_router': w_router,
            'b_router': b_router, 'w_gate': w_gate, 'b_gate': b_gate, 'w_up': w_up, 'b_up': b_up,
            'w_down': w_down, 'b_down': b_down, 'final_norm_g': final_norm_g}


def reference(x, c, positions, w_ada, b_ada, norm1_g, w_in, conv_w, conv_b, b_igate, b_fgate,
              mlstm_norm_g, w_out, norm2_g, w_router, b_router, w_gate, b_gate, w_up, b_up,
              w_down, b_down, final_norm_g):
    cond = jax.nn.silu(c)
    for l in range(DEPTH):
        mod = cond @ w_ada[l] + b_ada[l]
        sh1, sc1, g1, sh2, sc2, g2 = [m[:, None, :] for m in jnp.split(mod, 6, axis=-1)]
        h = rms_norm(x, norm1_g[l]) * (1.0 + sc1) + sh1
        y = hybrid_mixer(h, positions, w_in[l], conv_w[l], conv_b[l], b_igate[l], b_fgate[l],
                         mlstm_norm_g[l], w_out[l])
        x = x + g1 * y
        h = rms_norm(x, norm2_g[l]) * (1.0 + sc2) + sh2
        y = moe_ffn(h, w_router[l], b_router[l], w_gate[l], b_gate[l], w_up[l], b_up[l], w_down[l], b_down[l])
        x = x + g2 * y
    return rms_norm(x, final_norm_g)
```

```python
import numpy as np
from contextlib import ExitStack, contextmanager
import concourse.bass as bass
import concourse.mybir as mybir
from concourse.bass_utils import run_bass_kernel_spmd

F32 = mybir.dt.float32
BF16 = mybir.dt.bfloat16
I32 = mybir.dt.int32
U32 = mybir.dt.uint32
AF = mybir.ActivationFunctionType
ALU = mybir.AluOpType
AX = mybir.AxisListType

COMPUTE = ('pe', 'act', 'dve', 'pool')
ENGS = ('pe', 'act', 'dve', 'pool', 'sp')

D = 1024
NT = 4096
NG = NT // 512
NTILE = NT // 128
N_IN = 3592
N_TM = 2568
EPS = 1e-5


class Sched:
    def __init__(self, nc, n_dma_sems=32):
        self.nc = nc
        self.ops = {e: [] for e in ENGS}
        self.count = {e: 0 for e in COMPUTE}
        self.last_w = {}
        self.readers = {}
        self.waited = {e: {} for e in ENGS}
        self.n_dma_sems = n_dma_sems
        self.dma_tot = [0] * n_dma_sems
        self.dma_rr = 0

    def _need(self, eng, ref, waits):
        sem, val, src = ref
        if self.waited[eng].get(sem, 0) >= val:
            return
        self.waited[eng][sem] = val
        waits.append((sem, val))

    def _deps(self, eng, reads, writes, is_dma):
        waits = []
        for k in reads:
            w = self.last_w.get(k)
            if w is not None:
                if not (w[2] == eng and not is_dma and w[0] in COMPUTE and eng == 'pe'):
                    self._need(eng, w, waits)
            if isinstance(k, tuple) and k[0] == 'ps':
                for r in self.readers.get(k, ()):
                    if r[2] != eng:
                        self._need(eng, r, waits)
        for k in writes:
            w = self.last_w.get(k)
            if w is not None:
                if not (w[2] == eng and not is_dma and w[0] in COMPUTE):
                    self._need(eng, w, waits)
            for r in self.readers.get(k, ()):
                if r[2] == eng and not is_dma and r[0] in COMPUTE:
                    continue
                self._need(eng, r, waits)
        return waits

    def _record(self, ref, reads, writes):
        for k in reads:
            self.readers.setdefault(k, []).append(ref)
        for k in writes:
            self.last_w[k] = ref
            self.readers[k] = []

    def op(self, eng, fn, reads=(), writes=()):
        waits = self._deps(eng, reads, writes, False)
        self.count[eng] += 1
        ref = (eng, self.count[eng], eng)
        self.ops[eng].append((waits, fn, (eng, 1)))
        self._record(ref, reads, writes)
        return ref

    def dma(self, eng, fn, reads=(), writes=()):
        waits = self._deps(eng, reads, writes, True)
        i = self.dma_rr
        self.dma_rr = (self.dma_rr + 1) % self.n_dma_sems
        sem = 'dma%d' % i
        if self.dma_tot[i] > 0:
            self._need(eng, (sem, self.dma_tot[i], 'dmaq'), waits)
        self.dma_tot[i] += 16
        ref = (sem, self.dma_tot[i], 'dmaq')
        self.ops[eng].append((waits, fn, (sem, 16)))
        self._record(ref, reads, writes)
        return ref

    def collective(self, fn, inc=16):
        self.cc_tot = getattr(self, 'cc_tot', 0) + inc
        self.ops['pool'].append(([], fn, ('cc', inc)))
        for e in ENGS:
            self.ops[e].append(([('cc', self.cc_tot)], None, None))

    def barrier(self):
        for e in ENGS:
            waits = []
            for c in COMPUTE:
                if self.count[c] > 0:
                    self._need(e, (c, self.count[c], c), waits)
            for i in range(self.n_dma_sems):
                if self.dma_tot[i] > 0:
                    self._need(e, ('dma%d' % i, self.dma_tot[i], 'dmaq'), waits)
            if waits:
                self.ops[e].append((waits, None, None))
        self.last_w = {}
        self.readers = {}

    def emit(self):
        nc = self.nc
        with ExitStack() as st:
            sems = {}
            for e in COMPUTE:
                sems[e] = st.enter_context(nc.semaphore('prog_' + e))
            for i in range(self.n_dma_sems):
                sems['dma%d' % i] = st.enter_context(nc.semaphore('dmas%d' % i))
            sems['cc'] = st.enter_context(nc.semaphore('cc_sem'))
            block = st.enter_context(nc.Block())

            def run(eng_name):
                def body(e):
                    for waits, fn, inc in self.ops[eng_name]:
                        for s, v in waits:
                            e.wait_ge(sems[s], v)
                        if fn is not None:
                            ins = fn(e)
                            ins.then_inc(sems[inc[0]], inc[1])
                return body

            block.tensor(run('pe'))
            block.scalar(run('act'))
            block.vector(run('dve'))
            block.gpsimd(run('pool'))
            block.sync(run('sp'))


class Phase:
    def __init__(self, B):
        self.B = B
        self.st = ExitStack()

    def sb(self, name, shape, dt=F32):
        self.B.uid += 1
        return self.st.enter_context(self.B.nc.sbuf_tensor("%s_%d" % (name, self.B.uid), list(shape), dt))


class Builder:
    def __init__(self):
        self.nc = bass.Bass("TRN2", target_bir_lowering=False)
        self.S = Sched(self.nc)
        self.st = ExitStack()
        self.uid = 0
        self.scratch_kind = "Internal"
        self.ps = [self.st.enter_context(self.nc.psum_tensor("psb%d" % i, [128, 512], F32)) for i in range(8)]
        self.PS = [('ps', i) for i in range(8)]
        self.psb = [p[:].bitcast(BF16) for p in self.ps]

    def dram(self, name, shape, dt=F32, kind="Internal"):
        return self.nc.dram_tensor(name, list(shape), dt, kind=kind).ap()

    def sbp(self, name, shape, dt=F32):
        return self.st.enter_context(self.nc.sbuf_tensor(name, list(shape), dt))

    @contextmanager
    def phase(self):
        ph = Phase(self)
        try:
            yield ph
        finally:
            self.S.barrier()
            ph.st.close()

    def bc_reg(self, e, val):
        if getattr(self, '_bc', None) is None or self._bc[0] != val:
            self._bc = (val, e.to_reg(val))
        return self._bc[1]

    def mm(self, out, lhsT, rhs, start, stop, r, w):
        return self.S.op('pe', lambda e: e.matmul(out, lhsT=lhsT, rhs=rhs, start=start, stop=stop), r, w)

    def tr(self, out, in_, ident, r, w):
        return self.S.op('pe', lambda e: e.transpose(out=out, in_=in_, identity=ident), r, w)

    def act(self, out, in_, func, r, w, bias=0.0, scale=1.0, accum_out=None):
        if accum_out is None:
            return self.S.op('act', lambda e: e.activation(out=out, in_=in_, func=func, bias=bias, scale=scale), r, w)
        return self.S.op('act', lambda e: e.activation(out=out, in_=in_, func=func, bias=bias, scale=scale, accum_out=accum_out), r, w)

    def tt(self, eng, out, in0, in1, op, r, w):
        return self.S.op(eng, lambda e: e.tensor_tensor(out=out, in0=in0, in1=in1, op=op), r, w)

    def ts(self, eng, out, in0, s1, op0, r, w, s2=None, op1=None, accum_out=None):
        if op1 is None:
            return self.S.op(eng, lambda e: e.tensor_scalar(out=out, in0=in0, scalar1=s1, scalar2=None, op0=op0), r, w)
        if accum_out is None:
            return self.S.op(eng, lambda e: e.tensor_scalar(out=out, in0=in0, scalar1=s1, scalar2=s2, op0=op0, op1=op1), r, w)
        return self.S.op(eng, lambda e: e.tensor_scalar(out=out, in0=in0, scalar1=s1, scalar2=s2, op0=op0, op1=op1, accum_out=accum_out), r, w)

    def stt(self, out, in0, scalar, in1, op0, op1, r, w):
        return self.S.op('dve', lambda e: e.scalar_tensor_tensor(out=out, in0=in0, scalar=scalar, in1=in1, op0=op0, op1=op1), r, w)

    def cp(self, eng, out, in_, r, w):
        if eng == 'act':
            return self.S.op('act', lambda e: e.activation(out=out, in_=in_, func=AF.Copy), r, w)
        return self.S.op(eng, lambda e: e.tensor_copy(out=out, in_=in_), r, w)

    def memset(self, eng, ap, val, w):
        return self.S.op(eng, lambda e: e.memset(ap, val), (), w)

    def dma(self, q, out, in_, r, w):
        return self.S.dma(q, lambda e: e.dma_start(out=out, in_=in_), r, w)

    def finish(self):
        self.S.barrier()
        self.S.emit()
        self.st.close()
        return self.nc

    def make_consts(self):
        self.ones_f = self.sbp("ones_f", [128, 128], F32)
        self.ident_f = self.sbp("ident_f", [128, 128], F32)
        self.ident_b = self.sbp("ident_b", [128, 128], BF16)
        self.memset('pool', self.ones_f[:], 1.0, ['ones_f'])
        self.S.op('pool', lambda e: e.affine_select(out=self.ident_f[:], in_=self.ones_f[:], pattern=[[1, 128]],
                                                    compare_op=ALU.is_equal, fill=0.0, base=0, channel_multiplier=-1),
                  ['ones_f'], ['ident_f'])
        self.cp('dve', self.ident_b[:], self.ident_f[:], ['ident_f'], ['ident_b'])
        self.c7 = self.sbp("c7", [128, 1], F32)
        self.memset('pool', self.c7[:], 7.0, ['c7'])
        self.S.barrier()

    def p_x_to_xT(self, x_d, xT_d):
        ps, PS = self.ps, self.PS
        xT_v = xT_d.rearrange("(kc p) t -> p kc t", p=128)
        with self.phase() as ph:
            xt = [ph.sb("xt", [128, 4, D]) for _ in range(2)]
            xg = [ph.sb("xg", [128, 8, 512]) for _ in range(2)]
            for g in range(NG):
                b = g % 2
                self.dma('sp', xt[b][:], x_d[g * 512:(g + 1) * 512, :].rearrange("(j p) d -> p j d", p=128), [], [('xt', b)])
                for kc in range(8):
                    bk = kc % 4
                    for j in range(4):
                        self.tr(ps[bk][:, j * 128:(j + 1) * 128], xt[b][:, j, kc * 128:(kc + 1) * 128], self.ident_f[:],
                                [('xt', b)], [PS[bk]])
                    self.cp('act' if kc % 2 else 'dve', xg[b][:, kc, :], ps[bk][:], [PS[bk]], [('xg', b)])
                self.dma('sp', xT_v[:, :, g * 512:(g + 1) * 512], xg[b][:], [('xg', b)], [('xT', g)])

    def p_mods(self, cT_d, w_ada_d, b_adaT_d, modT):
        ps, PS = self.ps, self.PS
        wv = w_ada_d.rearrange("(kc p) n -> p kc n", p=128)
        with self.phase() as ph:
            cc = ph.sb("cc", [128, 8]); ex = ph.sb("ex", [128, 8]); cond = ph.sb("cond", [128, 8])
            bT = ph.sb("bT", [128, 48])
            wa = [ph.sb("wa", [128, 8, 512]) for _ in range(2)]
            self.dma('sp', cc[:], cT_d, [], ['cc'])
            self.dma('sp', bT[:], b_adaT_d, [], ['bT'])
            self.act(ex[:], cc[:], AF.Exp, ['cc'], ['ex'], scale=-1.0)
            self.ts('dve', ex[:], ex[:], 1.0, ALU.add, ['ex'], ['ex'])
            self.S.op('dve', lambda e: e.reciprocal(out=ex[:], in_=ex[:]), ['ex'], ['ex'])
            self.tt('dve', cond[:], cc[:], ex[:], ALU.mult, ['cc', 'ex'], ['cond'])
            for blk in range(12):
                b = blk % 2
                self.dma('sp', wa[b][:], wv[:, :, blk * 512:(blk + 1) * 512], [], [('wa', b)])
                for jj in range(4):
                    j = blk * 4 + jj
                    for kc in range(8):
                        self.mm(ps[0][:, j:j + 1], wa[b][:, kc, jj * 128:(jj + 1) * 128], cond[:, kc:kc + 1],
                                kc == 0, kc == 7, [('wa', b), 'cond'], [PS[0]])
            self.tt('dve', modT[:], ps[0][:, 0:48], bT[:], ALU.add, [PS[0], 'bT'], ['modT'])

    def p_load_w_bf16(self, ph, w_d, ncols, dst, key, col0=0):
        wv = w_d.rearrange("(kc p) n -> p kc n", p=128)
        with self.phase() as ph2:
            stg = [ph2.sb("wstg", [128, 8, 512]) for _ in range(2)]
            nblk = (ncols + 511) // 512
            for blk in range(nblk):
                b = blk % 2
                c0 = blk * 512
                c1 = min(ncols, c0 + 512)
                self.dma('sp', stg[b][:, :, 0:c1 - c0], wv[:, :, col0 + c0:col0 + c1], [], [('wstg', key, b)])
                for kc in range(8):
                    self.cp('act' if kc % 2 else 'pool', dst[:, kc, c0:c1], stg[b][:, kc, 0:c1 - c0], [('wstg', key, b)], [key])

    def p_norm_group(self, ph, g, xT_d, Acol, Bcol, hT, hkey, bufs):
        ps, PS = self.ps, self.PS
        xg, sq, rstd = bufs
        xT_v = xT_d.rearrange("(kc p) t -> p kc t", p=128)
        self.dma('sp', xg[:], xT_v[:, :, g * 512:(g + 1) * 512], [('xT', g)], ['n_xg'])
        self.act(sq[:], xg[:], AF.Square, ['n_xg'], [('n_sq', kc) for kc in range(8)])
        for kc in range(8):
            self.mm(ps[7][:], self.ones_f[:], sq[:, kc, :], kc == 0, kc == 7, [('n_sq', kc)], [PS[7]])
        self.act(rstd[:], ps[7][:], AF.Ln, [PS[7]], ['n_rstd'], bias=EPS, scale=1.0 / D)
        self.act(rstd[:], rstd[:], AF.Exp, ['n_rstd'], ['n_rstd'], scale=-0.5)
        for kc in range(8):
            self.stt(sq[:, kc, :], xg[:, kc, :], Acol[:, kc:kc + 1], rstd[:], ALU.mult, ALU.mult, ['n_xg', 'n_rstd', 'lconst'], [('n_sq', kc)])
            self.act(hT[:, kc, :], sq[:, kc, :], AF.Identity, [('n_sq', kc), 'lconst'], [hkey], bias=Bcol[:, kc:kc + 1])

    def p_inproj(self, l, xT_d, w_in_d, A1, B1, ztm_d, zqk_d, zvo_d=None):
        ps, PS = self.ps, self.PS
        zqk_v = zqk_d.rearrange("(cc p) t -> p cc t", p=128)
        with self.phase() as ph:
            w_fm = ph.sb("w_fm", [128, 8, 1024], BF16)
            w_tm = ph.sb("w_tm", [128, 8, N_TM], BF16)
            self.p_load_w_bf16(ph, w_in_d, 1024, w_fm, 'w_fm', 0)
            self.p_load_w_bf16(ph, w_in_d, N_TM, w_tm, 'w_tm', 1024)
            xg = ph.sb("n_xg", [128, 8, 512]); sq = ph.sb("n_sq", [128, 8, 512])
            rstd = ph.sb("n_rstd", [128, 512])
            hT = [ph.sb("hT", [128, 8, 512], BF16) for _ in range(2)]
            zt = [ph.sb("zt", [128, N_TM]) for _ in range(2)]
            zf = [ph.sb("zf", [128, 8, 512]) for _ in range(2)]
            NB = 6
            CB = N_TM // NB
            import os
            DBG = int(os.environ.get("DBG", "9"))
            for g in range(NG if DBG > 0 else 0):
                hb = g % 2
                self.p_norm_group(ph, g, xT_d, A1, B1, hT[hb], ('hT', hb), (xg, sq, rstd))
                for cc in range(8 if DBG > 1 else 0):
                    bk = cc % 2
                    for kc in range(8):
                        self.mm(ps[bk][:], w_fm[:, kc, cc * 128:(cc + 1) * 128], hT[hb][:, kc, :], kc == 0, kc == 7,
                                ['w_fm', ('hT', hb)], [PS[bk]])
                    self.cp('act' if cc % 2 else 'dve', zf[hb][:, cc, :], ps[bk][:], [PS[bk]], [('zf', hb)])
                self.dma('sp', zqk_v[:, :, g * 512:(g + 1) * 512], zf[hb][:], [('zf', hb)], [('zqk', g)])
                for j in range(4 if DBG > 2 else 0):
                    ti = g * 4 + j
                    zb = ti % 2
                    for nb in range(NB):
                        bk = 2 + (nb % 4)
                        for kc in range(8):
                            self.mm(ps[bk][:, 0:CB], hT[hb][:, kc, j * 128:(j + 1) * 128], w_tm[:, kc, nb * CB:(nb + 1) * CB],
                                    kc == 0, kc == 7, ['w_tm', ('hT', hb)], [PS[bk]])
                        self.cp('act' if nb % 2 else 'dve', zt[zb][:, nb * CB:(nb + 1) * CB], ps[bk][:, 0:CB], [PS[bk]], [('zt', zb)])
                    self.dma('sp', ztm_d[ti * 128:(ti + 1) * 128, :], zt[zb][:], [('zt', zb)], [('ztm', ti)])
                    if zvo_d is not None:
                        self.dma('sp', zvo_d[ti * 128:(ti + 1) * 128, :], zt[zb][:, 0:1024], [('zt', zb)], [('zvo', ti)])


    def p_layer_consts(self, modT, g1n_d, g2n_d, LC):
        with self.phase() as ph:
            gn = ph.sb("gn", [128, 2, 8])
            self.dma('sp', gn[:, 0, :], g1n_d, [], ['gn'])
            self.dma('sp', gn[:, 1, :], g2n_d, [], ['gn'])
            for i in range(2):
                o = 24 * i
                self.stt(LC[:, 3 * i + 0, :], modT[:, o + 8:o + 16], 1.0, gn[:, i, :], ALU.add, ALU.mult, ['modT', 'gn'], ['lconst'])
                self.cp('dve', LC[:, 3 * i + 1, :], modT[:, o:o + 8], ['modT'], ['lconst'])
                self.cp('dve', LC[:, 3 * i + 2, :], modT[:, o + 16:o + 24], ['modT'], ['lconst'])


    def conv_chunk(self, zin, n, cw, cb, ncb, cc, acc, ee, out, rk, wk):
        self.ts('dve', acc[:, 0:n], zin[:, 3:3 + n], cw[:, cc, 3:4], ALU.mult, rk + ['cw'], ['c_acc'])
        for j in (2, 1, 0):
            self.stt(acc[:, 0:n], zin[:, j:j + n], cw[:, cc, j:j + 1], acc[:, 0:n], ALU.mult, ALU.add, rk + ['cw', 'c_acc'], ['c_acc'])
        self.act(ee[:, 0:n], acc[:, 0:n], AF.Exp, ['c_acc', 'cw'], ['c_ee'], bias=ncb[:, cc:cc + 1], scale=-1.0)
        sk = 1.0 if cc < 4 else 128.0 ** 0.5
        self.ts('pool', ee[:, 0:n], ee[:, 0:n], 1.0, ALU.add, ['c_ee'], ['c_ee'], s2=sk, op1=ALU.mult)
        self.S.op('dve', lambda e: e.reciprocal(out=ee[:, 0:n], in_=ee[:, 0:n]), ['c_ee'], ['c_ee'])
        self.stt(out, acc[:, 0:n], cb[:, cc:cc + 1], ee[:, 0:n], ALU.add, ALU.mult, ['c_acc', 'c_ee', 'cw'], wk)

    def p_mlstm_prep(self, zqk_d, ztm_d, cwT_d, cbT_d, bgate_d, qkT_d, ktm_d, gates, cw, cb, ncb):
        ps, PS = self.ps, self.PS
        ktm_v = ktm_d.rearrange("(i p) c -> p i c", p=128)
        with self.phase() as ph:
            self.dma('sp', cw[:], cwT_d, [], ['cw'])
            self.dma('sp', cb[:], cbT_d, [], ['cw'])
            self.ts('dve', ncb[:], cb[:], -1.0, ALU.mult, ['cw'], ['cw'])
            graw = ph.sb("graw", [128, NTILE, 8]); gb = ph.sb("gb", [128, 8]); ge = ph.sb("ge", [128, NTILE, 8])
            self.dma('sp', graw[:], ztm_d.rearrange("(i p) c -> p i c", p=128)[:, :, 1024:1032], [('ztm', i) for i in range(NTILE)], ['graw'])
            self.dma('sp', gb[:], bgate_d.partition_broadcast(128), [], ['gb'])
            self.tt('dve', graw[:], graw[:], gb[:].unsqueeze(1).to_broadcast([128, NTILE, 8]), ALU.add, ['graw', 'gb'], ['graw'])
            self.act(ge[:], graw[:], AF.Exp, ['graw'], ['ge'], scale=-2.0 / 15.0)
            self.ts('dve', ge[:], ge[:], 1.0, ALU.add, ['ge'], ['ge'])
            self.S.op('dve', lambda e: e.reciprocal(out=ge[:], in_=ge[:]), ['ge'], ['ge'])
            self.ts('dve', gates[:], ge[:], 30.0, ALU.mult, ['ge'], ['gates'], s2=-15.0, op1=ALU.add)
            self.act(ge[:, :, 4:8], gates[:, :, 4:8], AF.Exp, ['gates'], ['ge'], scale=-1.0)
            self.act(ge[:, :, 4:8], ge[:, :, 4:8], AF.Ln, ['ge'], ['ge'], bias=1.0)
            self.ts('dve', gates[:, :, 4:8], ge[:, :, 4:8], -1.0, ALU.mult, ['ge'], ['gates'])
            zin = [ph.sb("zin", [128, 3 + NT]) for _ in range(2)]
            acc = ph.sb("acc", [128, NT]); ee = ph.sb("ee", [128, NT])
            qo = [ph.sb("qo", [128, NT], BF16) for _ in range(2)]
            ktsb = ph.sb("ktsb", [128, NTILE, 128], BF16)
            for b in range(2):
                self.memset('pool', zin[b][:, 0:3], 0.0, [('zin', b)])
            for cc in range(8):
                b = cc % 2
                self.dma('sp', zin[b][:, 3:3 + NT], zqk_d[cc * 128:(cc + 1) * 128, :], [('zqk', g) for g in range(NG)], [('zin', b)])
                self.conv_chunk(zin[b], NT, cw, cb, ncb, cc, acc, ee, qo[b][:], [('zin', b)], [('qo', b)])
                self.dma('sp', qkT_d[cc * 128:(cc + 1) * 128, :], qo[b][:], [('qo', b)], [('qkT', cc)])
                if cc >= 4:
                    h = cc - 4
                    for i in range(NTILE):
                        bk = (i // 8) % 2
                        self.tr(self.psb[bk][:, (i % 8) * 128:(i % 8 + 1) * 128], qo[b][:, i * 128:(i + 1) * 128], self.ident_b[:],
                                [('qo', b)], [PS[bk]])
                        if i % 8 == 7:
                            self.cp('act' if bk else 'dve', ktsb[:, i - 7:i + 1, :], self.psb[bk][:].rearrange("p (i c) -> p i c", c=128),
                                    [PS[bk]], ['ktsb'])
                    self.dma('sp', ktm_v[:, :, h * 128:(h + 1) * 128], ktsb[:], ['ktsb'], [('ktm', h)])

    def p_mlstm(self, full, qkT_d, ktm_d, ztm_d, gates, Cst, gmbc_d=None, catT_d=None, fix=None):
        ps, PS, psb = self.ps, self.PS, self.psb
        with self.phase() as ph:
            triu = ph.sb("triu", [128, 128])
            self.S.op('pool', lambda e: e.affine_select(out=triu[:], in_=self.ones_f[:], pattern=[[1, 128]], compare_op=ALU.is_ge,
                                                        fill=0.0, base=0, channel_multiplier=-1), ['ones_f'], ['triu'])
            ktile = [ph.sb("ktile", [128, 512], BF16) for _ in range(2)]
            vraw = [ph.sb("vraw", [128, 512]) for _ in range(2)]
            vaug = [ph.sb("vaug", [128, 4, 129], BF16) for _ in range(2)]
            kt = ph.sb("kt", [128, 4, 128], BF16)
            u = ph.sb("u", [128, 4]); ug = ph.sb("ug", [128, 4]); w = ph.sb("w", [128, 4]); eg = ph.sb("eg", [128, 4])
            for b in range(2):
                self.memset('pool', vaug[b][:, :, 128:129], 1.0, [('vaug', b)])
            if full:
                Cb = ph.sb("Cb", [128, 4, 129], BF16)
                self.cp('act', Cb[:], Cst[:], ['Cst'], ['Cb'])
                gm = ph.sb("gm", [128, 512])
                self.dma('sp', gm[:], gmbc_d.partition_broadcast(128), [], ['gm'])
                qT = [ph.sb("qT", [128, 4, 128], BF16) for _ in range(2)]
                kT = [ph.sb("kT", [128, 4, 128], BF16) for _ in range(2)]
                mo = [ph.sb("mo", [128, 512]) for _ in range(2)]
                lfbc = ph.sb("lfbc", [128, 4, 128])
                EB = ph.sb("EB", [128, 512]); E = ph.sb("E", [128, 4, 128]); Em = ph.sb("Em", [128, 512], BF16)
                St = ph.sb("St", [128, 512], BF16); qtl = ph.sb("qtl", [128, 512], BF16)
                dab = ph.sb("dab", [128, 4]); rec = ph.sb("rec", [128, 4]); og = ph.sb("og", [128, 512])
                hg = ph.sb("hg", [128, 4, 128]); junk = ph.sb("junk", [128, 128]); ss = ph.sb("ss", [128, 4]); rstd = ph.sb("rstd", [128, 4])
                hn = ph.sb("hn", [128, 512], BF16)
                cat = [ph.sb("cat", [128, 4, 128], BF16) for _ in range(2)]
                catT_v = catT_d[0:512, :].rearrange("(h p) t -> p h t", p=128)
                qT_v = qkT_d[0:512, :].rearrange("(h p) t -> p h t", p=128)
                kT_v = qkT_d[512:1024, :].rearrange("(h p) t -> p h t", p=128)
            for i in range(NTILE):
                b = i % 2
                rows = slice(i * 128, (i + 1) * 128)
                if fix is not None and i == 0:
                    self.dma('sp', ktile[b][:], fix[1], [], [('ktile', b)])
                else:
                    self.dma('sp', ktile[b][:], ktm_d[rows, :], [('ktm', h) for h in range(4)], [('ktile', b)])
                self.dma('sp', vraw[b][:], ztm_d[rows, 0:512], [('ztm', i)], [('vraw', b)])
                self.cp('pool', vaug[b][:, :, 0:128], vraw[b][:].rearrange("p (h d) -> p h d", d=128), [('vraw', b)], [('vaug', b)])
                lf = gates[:, i, 4:8]
                ig = gates[:, i, 0:4]
                self.mm(ps[0][:, 0:4], triu[:], lf, True, True, ['triu', 'gates'], [PS[0]])
                if full:
                    if fix is not None and i == 0:
                        self.dma('sp', qT[b][:], fix[0][0:512, :].rearrange("(h p) t -> p h t", p=128), [], [('qT', b)])
                        self.dma('sp', kT[b][:], fix[0][512:1024, :].rearrange("(h p) t -> p h t", p=128), [], [('kT', b)])
                    else:
                        self.dma('sp', qT[b][:], qT_v[:, :, rows], [('qkT', c) for c in range(4)], [('qT', b)])
                        self.dma('sp', kT[b][:], kT_v[:, :, rows], [('qkT', c) for c in range(4, 8)], [('kT', b)])
                    self.dma('sp', mo[b][:], ztm_d[rows, 512:1024], [('ztm', i)], [('mo', b)])
                    for h in range(4):
                        self.ts('dve', lfbc[:, h, :], self.ones_f[:], lf[:, h:h + 1], ALU.mult, ['gates'], [('lfbc', h)])
                        self.mm(ps[1][:, h * 128:(h + 1) * 128], lfbc[:, h, :], triu[:], True, True, [('lfbc', h), 'triu'], [PS[1]])
                    Gap = ps[1][:].rearrange("p (h t) -> p h t", t=128)[:, :, 127]
                    GK = PS[1]
                else:
                    self.mm(ps[0][:, 4:8], self.ones_f[:], lf, True, True, ['gates'], [PS[0]])
                    Gap = ps[0][:, 4:8]
                    GK = PS[0]
                self.tt('dve', u[:], ig, ps[0][:, 0:4], ALU.subtract, ['gates', PS[0]], ['u'])
                self.tt('dve', ug[:], u[:], Gap, ALU.add, ['u', GK], ['ug'])
                self.act(w[:], ug[:], AF.Exp, ['ug'], ['w'])
                self.act(eg[:], Gap, AF.Exp, [GK], ['eg'])
                for h in range(4):
                    self.act(kt[:, h, :], ktile[b][:, h * 128:(h + 1) * 128], AF.Identity, [('ktile', b), 'w'], [('kt', h)], scale=w[:, h:h + 1])
                if full:
                    self.act(EB[:], ps[1][:], AF.Exp, [PS[1]], ['EB'])
                    for h in range(4):
                        self.act(E[:, h, :], ps[1][:, h * 128:(h + 1) * 128], AF.Exp, [PS[1], 'u'], ['E'], bias=u[:, h:h + 1])
                    self.S.op('pool', lambda e: e.affine_select(out=Em[:].rearrange("p (h t) -> p h t", t=128), in_=E[:], pattern=[[0, 4], [1, 128]],
                                                                compare_op=ALU.is_ge, fill=0.0, base=0, channel_multiplier=-1), ['E'], ['Em'])
                    for h in range(4):
                        self.mm(ps[4][:, h * 128:(h + 1) * 128], kT[b][:, h, :], qT[b][:, h, :], True, True, [('kT', b), ('qT', b)], [PS[4]])
                    self.tt('dve', St[:], ps[4][:], Em[:], ALU.mult, [PS[4], 'Em'], ['St'])
                    self.tt('dve', qtl[:], qT[b][:].rearrange("p h t -> p (h t)"), EB[:], ALU.mult, [('qT', b), 'EB'], ['qtl'])
                    for h in range(4):
                        o = ps[5 + h // 2][:, (h % 2) * 129:(h % 2) * 129 + 129]
                        self.mm(o, St[:, h * 128:(h + 1) * 128], vaug[b][:, h, :], True, False, ['St', ('vaug', b)], [PS[5 + h // 2]])
                        self.mm(o, qtl[:, h * 128:(h + 1) * 128], Cb[:, h, :], False, True, ['qtl', 'Cb'], [PS[5 + h // 2]])
                    for k2 in range(2):
                        den2 = ps[5 + k2][:, 0:258].rearrange("p (h c) -> p h c", c=129)[:, :, 128]
                        self.ts('dve', dab[:, 2 * k2:2 * k2 + 2], den2, 1.0, ALU.max, [PS[5 + k2]], ['dab'])
                        self.stt(dab[:, 2 * k2:2 * k2 + 2], den2, -1.0, dab[:, 2 * k2:2 * k2 + 2], ALU.mult, ALU.max, [PS[5 + k2], 'dab'], ['dab'])
                    self.S.op('dve', lambda e: e.reciprocal(out=rec[:], in_=dab[:]), ['dab'], ['rec'])
                    self.act(og[:], mo[b][:], AF.Exp, [('mo', b)], ['og'], scale=-1.0)
                    self.ts('pool', og[:], og[:], 1.0, ALU.add, ['og'], ['og'])
                    self.S.op('dve', lambda e: e.reciprocal(out=og[:], in_=og[:]), ['og'], ['og'])
                    for h in range(4):
                        self.stt(hg[:, h, :], ps[5 + h // 2][:, (h % 2) * 129:(h % 2) * 129 + 128], rec[:, h:h + 1], og[:, h * 128:(h + 1) * 128],
                                 ALU.mult, ALU.mult, [PS[5 + h // 2], 'rec', 'og'], [('hg', h)])
                        self.act(junk[:], hg[:, h, :], AF.Square, [('hg', h)], ['junk', 'ss'], accum_out=ss[:, h:h + 1])
                    self.act(rstd[:], ss[:], AF.Ln, ['ss'], ['rstd'], bias=EPS, scale=1.0 / 128.0)
                    self.act(rstd[:], rstd[:], AF.Exp, ['rstd'], ['rstd'], scale=-0.5)
                    for h in range(4):
                        self.stt(hn[:, h * 128:(h + 1) * 128], hg[:, h, :], rstd[:, h:h + 1], gm[:, h * 128:(h + 1) * 128], ALU.mult, ALU.mult,
                                 [('hg', h), 'rstd', 'gm'], ['hn'])
                    for h in range(4):
                        self.tr(psb[7][:, h * 128:(h + 1) * 128], hn[:, h * 128:(h + 1) * 128], self.ident_b[:], ['hn'], [PS[7]])
                    self.cp('act', cat[b][:], psb[7][:, 0:512].rearrange("p (h t) -> p h t", t=128), [PS[7]], [('cat', b)])
                    self.dma('sp', catT_v[:, :, rows], cat[b][:], [('cat', b)], [('catT', i)])
                for h in range(4):
                    self.mm(ps[2 + h // 2][:, (h % 2) * 129:(h % 2) * 129 + 129], kt[:, h, :], vaug[b][:, h, :], True, True,
                            [('kt', h), ('vaug', b)], [PS[2 + h // 2]])
                for h in range(4):
                    self.stt(Cst[:, h, :], Cst[:, h, :], eg[:, h:h + 1], ps[2 + h // 2][:, (h % 2) * 129:(h % 2) * 129 + 129], ALU.mult, ALU.add,
                             ['Cst', 'eg', PS[2 + h // 2]], ['Cst'])
                if full:
                    self.cp('act', Cb[:], Cst[:], ['Cst'], ['Cb'])


    def p_rotary_tables(self, pos_d, cs, sn):
        import math
        TWO_PI = 2.0 * math.pi
        C1 = 6.28125
        C2 = TWO_PI - C1
        with self.phase() as ph:
            pi_ = ph.sb("pos_i", [128, NTILE], I32); pf = ph.sb("pos_f", [128, NTILE])
            invf = ph.sb("invf", [128, 8]); ang = ph.sb("ang", [128, NTILE, 8])
            ki = ph.sb("ki", [128, NTILE, 8], I32); kf = ph.sb("kf", [128, NTILE, 8]); m = ph.sb("rm", [128, NTILE, 8])
            r = ph.sb("rr", [128, NTILE, 8]); r2 = ph.sb("rr2", [128, NTILE, 8])
            self.dma('sp', pi_[:], pos_d, [], ['pos_i'])
            self.cp('dve', pf[:], pi_[:], ['pos_i'], ['pos_f'])
            for f in range(8):
                self.memset('pool', invf[:, f:f + 1], float(np.float32(500000.0) ** np.float32(-(2.0 * f) / 16.0)), ['invf'])
            self.tt('dve', ang[:], pf[:].unsqueeze(2).to_broadcast([128, NTILE, 8]), invf[:].unsqueeze(1).to_broadcast([128, NTILE, 8]),
                    ALU.mult, ['pos_f', 'invf'], ['ang'])

            def wrap(x, k):
                self.ts('dve', m[:], x[:], math.pi, ALU.is_gt, [k], ['rm'])
                self.stt(x[:], m[:], -TWO_PI, x[:], ALU.mult, ALU.add, ['rm', k], [k])
                self.ts('dve', m[:], x[:], -math.pi, ALU.is_lt, [k], ['rm'])
                self.stt(x[:], m[:], TWO_PI, x[:], ALU.mult, ALU.add, ['rm', k], [k])

            self.ts('dve', kf[:], ang[:], 1.0 / TWO_PI, ALU.mult, ['ang'], ['kf'])
            self.cp('dve', ki[:], kf[:], ['kf'], ['ki'])
            self.cp('dve', kf[:], ki[:], ['ki'], ['kf'])
            self.stt(r[:], kf[:], -C1, ang[:], ALU.mult, ALU.add, ['kf', 'ang'], ['rr'])
            self.stt(r[:], kf[:], -C2, r[:], ALU.mult, ALU.add, ['kf', 'rr'], ['rr'])
            wrap(r, 'rr')
            self.ts('dve', r2[:], r[:], math.pi / 2.0, ALU.add, ['rr'], ['rr2'])
            wrap(r2, 'rr2')
            for x, k in ((r, 'rr'), (r2, 'rr2')):
                self.ts('dve', x[:], x[:], math.pi, ALU.min, [k], [k], s2=-math.pi, op1=ALU.max)
            self.act(sn[:], r[:], AF.Sin, ['rr'], ['rot'])
            self.act(cs[:], r2[:], AF.Sin, ['rr2'], ['rot'])

    def p_moba_prep(self, ztm_d, cs, sn, qrot_d, KTa_d, Vaug_d, kmT_d):
        ps, PS, psb = self.ps, self.PS, self.psb
        with self.phase() as ph:
            zt = [ph.sb("mzt", [128, 2, 1536]) for _ in range(2)]
            t1 = ph.sb("rt1", [128, 2, 16, 8]); t2 = ph.sb("rt2", [128, 2, 16, 8]); t3 = ph.sb("rt3", [128, 2, 16, 8]); t4 = ph.sb("rt4", [128, 2, 16, 8])
            kaug = [ph.sb("kaug", [128, 2, 8, 128], BF16) for _ in range(2)]
            vaug = [ph.sb("mvaug", [128, 2, 8, 65], BF16) for _ in range(2)]
            kta = [ph.sb("kta", [128, 8, 256], BF16) for _ in range(2)]
            kmT = ph.sb("kmT", [64, 8, 16])
            for b in range(2):
                self.memset('pool', kaug[b][:], 0.0, [('kaug', b)])
                self.memset('pool', vaug[b][:, :, :, 64:65], 1.0, [('mvaug', b)])
            ztm_v = ztm_d.rearrange("(n j p) c -> n p j c", p=128, j=2)
            qrot_v = qrot_d.rearrange("(n j p) c -> n p j c", p=128, j=2)
            for n in range(16):
                b = n % 2
                self.dma('sp', zt[b][:], ztm_v[n][:, :, 1032:2568], [('ztm', 2 * n), ('ztm', 2 * n + 1)], [('mzt', b)])
                qk = zt[b][:, :, 0:1024].rearrange("p j (h d) -> p j h d", d=64)
                x1 = qk[:, :, :, 0:8]; x2 = qk[:, :, :, 8:16]
                cosb = cs[:, 2 * n:2 * n + 2, :].unsqueeze(2).to_broadcast([128, 2, 16, 8])
                sinb = sn[:, 2 * n:2 * n + 2, :].unsqueeze(2).to_broadcast([128, 2, 16, 8])
                zk = [('mzt', b)]
                self.tt('dve', t1[:], x1, cosb, ALU.mult, zk + ['rot'], ['rt1'])
                self.tt('dve', t2[:], x2, sinb, ALU.mult, zk + ['rot'], ['rt2'])
                self.tt('dve', t3[:], x2, cosb, ALU.mult, zk + ['rot'], ['rt3'])
                self.tt('dve', t4[:], x1, sinb, ALU.mult, zk + ['rot'], ['rt4'])
                self.tt('dve', x1, t1[:], t2[:], ALU.subtract, ['rt1', 'rt2'], zk)
                self.tt('dve', x2, t3[:], t4[:], ALU.add, ['rt3', 'rt4'], zk)
                self.dma('sp', qrot_v[n], zt[b][:, :, 0:512], zk, [('qrot', n)])
                self.cp('act', kaug[b][:, :, :, 0:64], zt[b][:, :, 512:1024].rearrange("p j (h d) -> p j h d", d=64), zk, [('kaug', b)])
                if n >= 2:
                    self.memset('pool', kaug[b][:, :, :, 64 + n - 2:64 + n - 1], 0.0, [('kaug', b)])
                self.memset('pool', kaug[b][:, :, :, 64 + n:64 + n + 1], 1.0, [('kaug', b)])
                self.cp('pool', vaug[b][:, :, :, 0:64], zt[b][:, :, 1024:1536].rearrange("p j (h d) -> p j h d", d=64), zk, [('mvaug', b)])
                for j in range(2):
                    self.dma('sp', Vaug_d[:, (2 * n + j) * 128:(2 * n + j + 1) * 128, :].rearrange("h p c -> p h c"), vaug[b][:, j, :, :],
                             [('mvaug', b)], [('Vaug', n, j)])
                for h in range(8):
                    for j in range(2):
                        self.mm(ps[6][0:64, h:h + 1], zt[b][:, j, 512 + h * 64:512 + (h + 1) * 64], self.ones_f[:, 0:1], j == 0, j == 1,
                                zk, [PS[6]])
                self.ts('dve', kmT[:, :, n], ps[6][0:64, 0:8], 1.0 / 256.0, ALU.mult, [PS[6]], ['kmT'])
                for j in range(2):
                    bk = j
                    for h in range(8):
                        self.tr(psb[bk][:, h * 128:(h + 1) * 128], kaug[b][:, j, h, :], self.ident_b[:], [('kaug', b)], [PS[bk]])
                    self.cp('act' if j else 'dve', kta[b][:, :, j * 128:(j + 1) * 128], psb[bk][:].rearrange("p (h t) -> p h t", t=128),
                            [PS[bk]], [('kta', b)])
                self.dma('sp', KTa_d.rearrange("h r t -> r h t")[:, :, n * 256:(n + 1) * 256], kta[b][:], [('kta', b)], [('KTa', n)])
            self.dma('sp', kmT_d, kmT[:], ['kmT'], ['kmT_d'])


    def p_moba_attn(self, qrot_d, KTa_o, Vaug_o, kmT_o, KTa_p, Vaug_p, kmT_p, selb, catT_d, nq_tiles=NTILE, heads=range(8)):
        ps, PS, psb = self.ps, self.PS, self.psb
        BIG2 = 30000.0
        with self.phase() as ph:
            km = ph.sb("km", [128, 8, 32])
            self.memset('dve', km[:], 0.0, ['km'])
            for e_ in range(2):
                kmv = km[e_ * 64:(e_ + 1) * 64, :, :].rearrange("p (j e) n -> p j e n", e=2)[:, :, e_, :]
                self.dma('sp', kmv[:, :, 0:16], kmT_p.rearrange("d (j e) n -> d j e n", e=2)[:, :, e_, :], [], ['km'])
                self.dma('sp', kmv[:, :, 16:32], kmT_o.rearrange("d (j e) n -> d j e n", e=2)[:, :, e_, :], [], ['km'])
            QTp_d = self.dram("QTp_scr%d" % self.uid, [NTILE, 128, 8, 128], BF16, kind=self.scratch_kind)
            QTo_d = self.dram("QTo_scr%d" % self.uid, [NTILE, 128, 8, 128], BF16, kind=self.scratch_kind)
            self.uid += 1
            qr = [ph.sb("qr", [128, 512]) for _ in range(2)]
            qt32 = ph.sb("qt32", [128, 4, 128])
            qtp = [ph.sb("qtp", [128, 8, 128], BF16) for _ in range(2)]
            qto = [ph.sb("qto", [128, 8, 128], BF16) for _ in range(2)]
            G = ph.sb("G", [128, 8, 32]); top8 = ph.sb("top8", [128, 8, 8]); m1 = ph.sb("m1", [128, 8, 32]); m2 = ph.sb("m2", [128, 8, 32])
            MA = ph.sb("MA", [128, 8, 128], BF16)
            self.memset('pool', MA[:], 0.0, ['MA'])
            for b in range(2):
                self.memset('pool', qtp[b][:], 0.0, [('qtp', b)])
                self.memset('pool', qto[b][:], 0.0, [('qto', b)])
            for i in range(nq_tiles):
                b = i % 2
                nb = i // 2
                nc_ = 16 + nb
                self.dma('sp', qr[b][:], qrot_d[i * 128:(i + 1) * 128, :], [('qrot', nb)], [('qr', b)])
                import os
                DBGM = int(os.environ.get("DBGM", "9"))
                for j in range(4):
                    self.tr(ps[0][:, j * 128:(j + 1) * 128], qr[b][:, j * 128:(j + 1) * 128], self.ident_f[:], [('qr', b)], [PS[0]])
                self.cp('dve', qt32[:], ps[0][:].rearrange("p (j t) -> p j t", t=128), [PS[0]], ['qt32'])
                for e_ in range(2 if DBGM >= 1 else 0):
                    src = qt32[e_ * 64:(e_ + 1) * 64, :, :]
                    dp = qtp[b][0:64, :, :].rearrange("p (j e) t -> p j e t", e=2)[:, :, e_, :]
                    do = qto[b][0:64, :, :].rearrange("p (j e) t -> p j e t", e=2)[:, :, e_, :]
                    self.ts('dve', dp, src, 0.125, ALU.mult, ['qt32'], [('qtp', b)])
                    self.ts('pool' if e_ == 0 else 'dve', do, src, 0.125, ALU.mult, ['qt32'], [('qto', b)])
                if DBGM < 2: continue
                for h in range(8):
                    e_ = h % 2
                    self.mm(ps[2][:, h * 32:(h + 1) * 32], qt32[:, h // 2, :], km[:, h, :], True, True, ['qt32', 'km'], [PS[2]])
                Gv = ps[2][:, 0:256].rearrange("p (h n) -> p h n", n=32)
                self.ts('dve', G[:, :, 0:16], Gv[:, :, 0:16], selb[:, 0:1], ALU.add, [PS[2], 'selb'], ['G'])
                self.cp('dve', G[:, :, 16:32], Gv[:, :, 16:32], [PS[2]], ['G'])
                if DBGM < 3: continue
                for h in range(8):
                    self.S.op('dve', lambda e, h=h, nc_=nc_: e.max(out=top8[:, h, :], in_=G[:, h, 0:nc_]), ['G'], ['top8'])
                self.tt('dve', m1[:, :, 0:nc_], G[:, :, 0:nc_], top8[:, :, 2:3].to_broadcast([128, 8, nc_]), ALU.is_ge, ['G', 'top8'], ['m1'])
                self.ts('dve', m2[:, :, 0:nc_], G[:, :, 0:nc_], -1e29, ALU.is_gt, ['G'], ['m2'])
                self.tt('dve', m1[:, :, 0:nc_], m1[:, :, 0:nc_], m2[:, :, 0:nc_], ALU.mult, ['m1', 'm2'], ['m1'])
                if DBGM < 4: continue
                self.ts('dve', MA[:, :, 64:80], m1[:, :, 0:16], BIG2, ALU.mult, ['m1'], ['MA'], s2=-BIG2, op1=ALU.add)
                if nb > 0:
                    self.ts('dve', MA[:, :, 96:96 + nb], m1[:, :, 16:16 + nb], BIG2, ALU.mult, ['m1'], ['MA'], s2=-BIG2, op1=ALU.add)
                for h in range(8):
                    self.tr(psb[3][:, h * 128:(h + 1) * 128], MA[:, h, :], self.ident_b[:], ['MA'], [PS[3]])
                if DBGM < 5: continue
                mt = psb[3][:].rearrange("p (h t) -> p h t", t=128)
                self.cp('dve', qtp[b][64:80, :, :], mt[64:80, :, :], [PS[3]], [('qtp', b)])
                self.cp('dve', qto[b][64:80, :, :], mt[96:112, :, :], [PS[3]], [('qto', b)])
                if DBGM < 6: continue
                self.dma('sp', QTp_d[i], qtp[b][:], [('qtp', b)], [('QTp', i)])
                self.dma('sp', QTo_d[i], qto[b][:], [('qto', b)], [('QTo', i)])
        with self.phase() as ph:
            kp = ph.sb("kp", [128, NT], BF16); ko = ph.sb("ko", [128, NT], BF16)
            vp = ph.sb("vp", [128, NTILE, 65], BF16); vo = ph.sb("vo", [128, NTILE, 65], BF16)
            qhp = ph.sb("qhp", [128, NTILE, 128], BF16); qho = ph.sb("qho", [128, NTILE, 128], BF16)
            triu_b = ph.sb("triu_b2", [128, 128], BF16)
            self.S.op('pool', lambda e: e.affine_select(out=triu_b[:], in_=self.ones_f[:], pattern=[[1, 128]], compare_op=ALU.is_ge,
                                                        fill=0.0, base=0, channel_multiplier=-1), ['ones_f'], ['triu_b'])
            PT = [ph.sb("PT", [128, 512], BF16) for _ in range(3)]
            rec = ph.sb("mrec", [128, 1]); ob = ph.sb("ob", [128, 64], BF16)
            haT = ph.sb("haT", [64, NT], BF16)
            pt_i = 0
            sb_i = 0
            for h in heads:
                self.dma('sp', kp[:], KTa_p[h], [], ['kp'])
                self.dma('sp', ko[:], KTa_o[h], [('KTa', n) for n in range(16)], ['ko'])
                self.dma('sp', vp[:], Vaug_p[h].rearrange("(i p) c -> p i c", p=128), [], ['vp'])
                self.dma('sp', vo[:], Vaug_o[h].rearrange("(i p) c -> p i c", p=128), [('Vaug', n, j) for n in range(16) for j in range(2)], ['vo'])
                self.dma('sp', qhp[:], QTp_d[:, :, h, :].rearrange("i r t -> r i t"), [('QTp', i) for i in range(nq_tiles)], ['qhp'])
                self.dma('sp', qho[:], QTo_d[:, :, h, :].rearrange("i r t -> r i t"), [('QTo', i) for i in range(nq_tiles)], ['qho'])
                work = []
                for i in range(nq_tiles):
                    kts = [('p', j) for j in range(NTILE)] + [('o', j) for j in range(i + 1)]
                    nk = len(kts)
                    for c0 in range(0, nk, 4):
                        work.append((i, c0, kts[c0:c0 + 4], nk))

                def emit_S(wk):
                    nonlocal sb_i
                    i, c0, chunk, nk = wk
                    bk = sb_i % 4
                    sb_i += 1
                    for jj, (kind, j) in enumerate(chunk):
                        ksrc, qsrc, kk, qk_ = (kp, qhp, 'kp', 'qhp') if kind == 'p' else (ko, qho, 'ko', 'qho')
                        self.mm(ps[bk][:, jj * 128:(jj + 1) * 128], ksrc[:, j * 128:(j + 1) * 128], qsrc[:, i, :], True, True,
                                [kk, qk_], [PS[bk]])
                    return bk

                def emit_rest(wk, bk):
                    nonlocal pt_i
                    i, c0, chunk, nk = wk
                    accb = 6 + (i % 2)
                    acc = ps[accb][:, 0:65]
                    pt = PT[pt_i % 3]
                    pk = ('PT', pt_i % 3)
                    pt_i += 1
                    w_ = len(chunk) * 128
                    self.act(pt[:, 0:w_], ps[bk][:, 0:w_], AF.Exp, [PS[bk]], [pk])
                    if chunk[-1] == ('o', i):
                        jj = len(chunk) - 1
                        self.tt('pool', pt[:, jj * 128:(jj + 1) * 128], pt[:, jj * 128:(jj + 1) * 128], triu_b[:], ALU.mult, [pk, 'triu_b'], [pk])
                    for jj, (kind, j) in enumerate(chunk):
                        vsrc, vk = (vp, 'vp') if kind == 'p' else (vo, 'vo')
                        first = (c0 + jj == 0)
                        last = (c0 + jj == nk - 1)
                        self.mm(acc, pt[:, jj * 128:(jj + 1) * 128], vsrc[:, j, :], first, last, [pk, vk], [PS[accb]])
                    if c0 + len(chunk) == nk:
                        self.S.op('dve', lambda e, accb=accb: e.reciprocal(out=rec[:], in_=ps[accb][:, 64:65]), [PS[accb]], ['mrec'])
                        self.ts('dve', ob[:], ps[accb][:, 0:64], rec[:, 0:1], ALU.mult, [PS[accb], 'mrec'], ['ob'])
                        self.tr(psb[5][0:64, (i % 8) * 128:(i % 8 + 1) * 128], ob[:], self.ident_b[:], ['ob'], [PS[5]])
                        if i % 8 == 7 or i == nq_tiles - 1:
                            i0 = (i // 8) * 8
                            n_ = i - i0 + 1
                            self.cp('act', haT[:, i0 * 128:(i + 1) * 128], psb[5][0:64, 0:n_ * 128], [PS[5]], ['haT'])

                pend = []
                for wk in work:
                    bk = emit_S(wk)
                    pend.append((wk, bk))
                    if len(pend) > 2:
                        emit_rest(*pend.pop(0))
                while pend:
                    emit_rest(*pend.pop(0))
                self.dma('sp', catT_d[512 + h * 64:512 + (h + 1) * 64, 0:nq_tiles * 128], haT[:, 0:nq_tiles * 128], ['haT'], [('catT_a', h)])


    def p_outproj(self, catT_d, w_out_d, G1, xT_d, dbg_out=None, xT_src=None):
        ps, PS = self.ps, self.PS
        xT_v = xT_d.rearrange("(kc p) t -> p kc t", p=128)
        xs_v = xT_v if xT_src is None else xT_src.rearrange("(kc p) t -> p kc t", p=128)
        cat_v = catT_d.rearrange("(kc p) t -> p kc t", p=128)
        with self.phase() as ph:
            wo = ph.sb("wo", [128, 8, 1024], BF16)
            self.p_load_w_bf16(ph, w_out_d, 1024, wo, 'wo', 0)
            cg = [ph.sb("cg", [128, 8, 512], BF16) for _ in range(2)]
            xg = [ph.sb("oxg", [128, 8, 512]) for _ in range(2)]
            for g in range(NG):
                b = g % 2
                cols = slice(g * 512, (g + 1) * 512)
                self.dma('sp', cg[b][:], cat_v[:, :, cols], [], [('cg', b)])
                self.dma('sp', xg[b][:], xs_v[:, :, cols], [('xT', g)], [('oxg', b)])
                for c in range(8):
                    bk = c % 4
                    for kc in range(8):
                        self.mm(ps[bk][:], wo[:, kc, c * 128:(c + 1) * 128], cg[b][:, kc, :], kc == 0, kc == 7, ['wo', ('cg', b)], [PS[bk]])
                    self.stt(xg[b][:, c, :], ps[bk][:], G1[:, c:c + 1], xg[b][:, c, :], ALU.mult, ALU.add, [PS[bk], ('oxg', b), 'lconst'], [('oxg', b)])
                self.dma('sp', xT_v[:, :, cols], xg[b][:], [('oxg', b)], [('xT', g)])
                if dbg_out is not None:
                    self.dma('sp', dbg_out.rearrange("(kc p) t -> p kc t", p=128)[:, :, cols], xg[b][:], [('oxg', b)], [('dbg', g)])

    def p_router(self, xT_d, A2, B2, w_router_d, brbc_d, h2T_d, PT):
        ps, PS = self.ps, self.PS
        h2_v = h2T_d.rearrange("(kc p) t -> p kc t", p=128)
        with self.phase() as ph:
            wr = ph.sb("wr", [128, 8, 32], BF16); wrs = ph.sb("wrs", [128, 8, 32]); br = ph.sb("br", [128, 32])
            self.dma('sp', wrs[:], w_router_d.rearrange("(kc p) n -> p kc n", p=128), [], ['wrs'])
            self.cp('dve', wr[:], wrs[:], ['wrs'], ['wr'])
            self.dma('sp', br[:], brbc_d.partition_broadcast(128), [], ['br'])
            xg = ph.sb("n_xg", [128, 8, 512]); sq = ph.sb("n_sq", [128, 8, 512]); rstd = ph.sb("n_rstd", [128, 512])
            hT = [ph.sb("h2", [128, 8, 512], BF16) for _ in range(2)]
            lg = ph.sb("lg", [128, 32]); top8 = ph.sb("rtop8", [128, 8]); ntop = ph.sb("ntop", [128, 1]); msk = ph.sb("rmsk", [128, 32])
            ex = ph.sb("rex", [128, 32]); ssum = ph.sb("rsum", [128, 1]); pp = ph.sb("rpp", [128, 128])
            self.memset('dve', pp[:], 0.0, ['rpp'])
            for g in range(NG):
                hb = g % 2
                self.p_norm_group(ph, g, xT_d, A2, B2, hT[hb], ('h2', hb), (xg, sq, rstd))
                self.dma('sp', h2_v[:, :, g * 512:(g + 1) * 512], hT[hb][:], [('h2', hb)], [('h2T', g)])
                for j in range(4):
                    ti = g * 4 + j
                    for kc in range(8):
                        self.mm(ps[0][:, 0:32], hT[hb][:, kc, j * 128:(j + 1) * 128], wr[:, kc, :], kc == 0, kc == 7, [('h2', hb), 'wr'], [PS[0]])
                    self.tt('dve', lg[:], ps[0][:, 0:32], br[:], ALU.add, [PS[0], 'br'], ['lg'])
                    self.S.op('dve', lambda e: e.max(out=top8[:], in_=lg[:]), ['lg'], ['rtop8'])
                    self.ts('dve', msk[:], lg[:], top8[:, 3:4], ALU.is_ge, ['lg', 'rtop8'], ['rmsk'])
                    self.ts('dve', ntop[:], top8[:, 0:1], -1.0, ALU.mult, ['rtop8'], ['ntop'])
                    self.act(ex[:], lg[:], AF.Exp, ['lg', 'ntop'], ['rex'], bias=ntop[:, 0:1])
                    self.tt('dve', ex[:], ex[:], msk[:], ALU.mult, ['rex', 'rmsk'], ['rex'])
                    self.S.op('dve', lambda e: e.tensor_reduce(out=ssum[:], in_=ex[:], axis=AX.X, op=ALU.add), ['rex'], ['rsum'])
                    self.S.op('dve', lambda e: e.reciprocal(out=ssum[:], in_=ssum[:]), ['rsum'], ['rsum'])
                    self.ts('dve', pp[:, 0:32], ex[:], ssum[:, 0:1], ALU.mult, ['rex', 'rsum'], ['rpp'])
                    self.tr(ps[1][:, 0:128], pp[:], self.ident_f[:], ['rpp'], [PS[1]])
                    self.cp('act', PT[:, ti * 128:(ti + 1) * 128], ps[1][0:32, 0:128], [PS[1]], ['PT'])

    def p_moe(self, h2T_d, PT, wg_d, wu_d, wd_d, bgT_d, buT_d, bd_d, G2, xT_d, n_exp=32, n_grp=NT // 1024):
        ps, PS = self.ps, self.PS
        import math
        SIGMAX = 1.0 / (1.0 + math.exp(-1.702 * 7.0))
        xT_v = xT_d.rearrange("(kc p) t -> p kc t", p=128)
        h2_v = h2T_d.rearrange("(kc p) t -> p kc t", p=128)
        with self.phase() as ph:
            pte = [ph.sb("pte", [32, 512]) for _ in range(2)]
            bg = ph.sb("bg", [128, 32, 8]); bu = ph.sb("bu", [128, 32, 8]); nbg = ph.sb("nbg", [128, 32, 8]); bd = ph.sb("bd", [32, 1024])
            self.dma('sp', bg[:], bgT_d, [], ['bg']); self.dma('sp', bu[:], buT_d, [], ['bu']); self.dma('sp', bd[:], bd_d, [], ['bd'])
            self.ts('dve', nbg[:], bg[:], 1.702, ALU.mult, ['bg'], ['nbg'])
            h2 = ph.sb("mh2", [128, 8, 1024], BF16)
            acc = ph.sb("macc", [128, 8, 1024])
            actT = ph.sb("actT", [128, 8, 1024], BF16)
            stg = [ph.sb("mstg", [128, 8, 512]) for _ in range(2)]
            wgb = [ph.sb("wgb", [128, 8, 512], BF16) for _ in range(2)]
            wub = [ph.sb("wub", [128, 8, 512], BF16) for _ in range(2)]
            wdb = [ph.sb("wdb", [128, 8, 512], BF16) for _ in range(2)]
            gtw = [ph.sb("gtw", [128, 1024]) for _ in range(2)]
            e1w = [ph.sb("e1w", [128, 1024]) for _ in range(2)]
            u1w = [ph.sb("u1w", [128, 1024]) for _ in range(2)]
            pbc = ph.sb("pbc", [128, 1024])
            xg = stg[0]
            cnt = {'stg': 0, 'wg': 0, 'wu': 0, 'wd': 0, 'tw': 0}

            import os
            NOLOAD = int(os.environ.get("NOLOAD", "0"))

            def load_blk(w_d, e, blk, dst_list, dk, conv_eng):
                si = cnt['stg'] % 2
                cnt['stg'] += 1
                di = cnt[dk] % 2
                cnt[dk] += 1
                if NOLOAD and e > 0:
                    if NOLOAD == 2:
                        wv = w_d[e].rearrange("(kc p) n -> p kc n", p=128)
                        self.dma('sp', stg[si][:], wv[:, :, blk * 512:(blk + 1) * 512], [], [('mstg', si)])
                    return dst_list[di], (dk, di)
                wv = w_d[e].rearrange("(kc p) n -> p kc n", p=128)
                self.dma('sp', stg[si][:], wv[:, :, blk * 512:(blk + 1) * 512], [], [('mstg', si)])
                for kc in range(8):
                    eng = conv_eng[kc % len(conv_eng)]
                    self.cp(eng, dst_list[di][:, kc, :], stg[si][:, kc, :], [('mstg', si)], [(dk, di)])
                return dst_list[di], (dk, di)

            for gq in range(n_grp):
                tcols = slice(gq * 1024, (gq + 1) * 1024)
                self.dma('sp', h2[:], h2_v[:, :, tcols], [], ['mh2'])
                for hf in range(2):
                    for c in range(8):
                        bk = (hf * 8 + c) % 2
                        self.mm(ps[bk][:], bd[:, c * 128:(c + 1) * 128], PT[:, gq * 1024 + hf * 512:gq * 1024 + (hf + 1) * 512], True, True,
                                ['bd', 'PT'], [PS[bk]])
                        self.cp('act', acc[:, c, hf * 512:(hf + 1) * 512], ps[bk][:], [PS[bk]], [('macc', c, hf)])
                units = []
                for e in range(n_exp):
                    units += [('gu', e, 0), ('gu', e, 1), ('d', e, 0), ('d', e, 1)]

                def prep(u):
                    kind, e, blk = u
                    if kind == 'gu':
                        return (load_blk(wg_d, e, blk, wgb, 'wg', ['act', 'pool', 'act']), load_blk(wu_d, e, blk, wub, 'wu', ['act', 'act', 'pool']))
                    return (load_blk(wd_d, e, blk, wdb, 'wd', ['act', 'pool', 'act']),)

                def compute(u, hd):
                    kind, e, blk = u
                    if kind == 'gu':
                        (wg_t, wgk), (wu_t, wuk) = hd
                        if blk == 0:
                            for hf in range(2):
                                self.ts('dve', pte[hf][:], PT[:, gq * 1024 + hf * 512:gq * 1024 + (hf + 1) * 512], self.ident_f[0:32, e:e + 1], ALU.mult,
                                        ['PT'], [('pte', hf)])
                                self.mm(ps[6 + hf][:], self.ones_f[0:32, :], pte[hf][:], True, True, [('pte', hf)], [PS[6 + hf]])
                                self.cp('act', pbc[:, hf * 512:(hf + 1) * 512], ps[6 + hf][:], [PS[6 + hf]], ['pbc'])
                        for f4 in range(4):
                            fc = blk * 4 + f4
                            ti = cnt['tw'] % 2
                            cnt['tw'] += 1
                            gt, e1, u1 = gtw[ti], e1w[ti], u1w[ti]
                            kg, ke, ku = ('gtw', ti), ('e1w', ti), ('u1w', ti)
                            for hf in range(2):
                                hcols = slice(hf * 512, (hf + 1) * 512)
                                ba, bb = 2 * hf, 2 * hf + 1
                                for kc in range(8):
                                    self.mm(ps[ba][:], wg_t[:, kc, f4 * 128:(f4 + 1) * 128], h2[:, kc, hcols], kc == 0, kc == 7, [wgk, 'mh2'], [PS[ba]])
                                for kc in range(8):
                                    self.mm(ps[bb][:], wu_t[:, kc, f4 * 128:(f4 + 1) * 128], h2[:, kc, hcols], kc == 0, kc == 7, [wuk, 'mh2'], [PS[bb]])
                                self.act(e1[:, hcols], ps[ba][:], AF.Sigmoid, [PS[ba], 'nbg'], [ke], bias=nbg[:, e, fc:fc + 1], scale=1.702)
                                self.ts('dve', gt[:, hcols], ps[ba][:], bg[:, e, fc:fc + 1], ALU.add, [PS[ba], 'bg'], [kg], s2=7.0, op1=ALU.min)
                                self.ts('dve', u1[:, hcols], ps[bb][:], bu[:, e, fc:fc + 1], ALU.add, [PS[bb], 'bu'], [ku], s2=7.0, op1=ALU.min)
                            self.act(u1[:], u1[:], AF.Relu, [ku], [ku], bias=self.c7[:, 0:1])
                            self.stt(gt[:], e1[:], SIGMAX, gt[:], ALU.min, ALU.mult, [ke, kg], [kg])
                            self.stt(gt[:], u1[:], -6.0, gt[:], ALU.add, ALU.mult, [ku, kg], [kg])
                            self.tt('pool', actT[:, fc, :], gt[:], pbc[:], ALU.mult, [kg, 'pbc'], [('actT', fc, 0), ('actT', fc, 1)])
                    else:
                        (wd_t, wdk), = hd
                        for hf in range(2):
                            hcols = slice(hf * 512, (hf + 1) * 512)
                            for c4 in range(4):
                                c = blk * 4 + c4
                                bk = 4 + (c4 % 2)
                                for fc in range(8):
                                    self.mm(ps[bk][:], wd_t[:, fc, c4 * 128:(c4 + 1) * 128], actT[:, fc, hcols], fc == 0, fc == 7,
                                            [wdk, ('actT', fc, hf)], [PS[bk]])
                                self.tt('dve', acc[:, c, hcols], acc[:, c, hcols], ps[bk][:], ALU.add, [PS[bk], ('macc', c, hf)], [('macc', c, hf)])

                hd_next = prep(units[0]) if units else None
                for ui, u in enumerate(units):
                    hd = hd_next
                    hd_next = prep(units[ui + 1]) if ui + 1 < len(units) else None
                    compute(u, hd)
                for hf in range(2):
                    g = gq * 2 + hf
                    self.dma('sp', xg[:], xT_v[:, :, g * 512:(g + 1) * 512], [('xT', g)], [('mstg', 0)])
                    for c in range(8):
                        self.stt(xg[:, c, :], acc[:, c, hf * 512:(hf + 1) * 512], G2[:, c:c + 1], xg[:, c, :], ALU.mult, ALU.add,
                                 [('macc', c, hf), ('mstg', 0), 'lconst'], [('mstg', 0)])
                    self.dma('sp', xT_v[:, :, g * 512:(g + 1) * 512], xg[:], [('mstg', 0)], [('xT', g)])

    def p_router_sparse(self, xT_d, A2, B2, w_router_d, brbc_d, Xg_d, slots_all, CAP, ibuf):
        ps, PS, psb = self.ps, self.PS, self.psb
        R = 32 * CAP
        with self.phase() as ph:
            wr = ph.sb("wr", [128, 8, 32], BF16); wrs = ph.sb("wrs", [128, 8, 32]); br = ph.sb("br", [128, 32])
            self.dma('sp', wrs[:], w_router_d.rearrange("(kc p) n -> p kc n", p=128), [], ['wrs'])
            self.cp('dve', wr[:], wrs[:], ['wrs'], ['wr'])
            self.dma('sp', br[:], brbc_d.partition_broadcast(128), [], ['br'])
            striu = ph.sb("striu", [128, 128], BF16); ones_b = ph.sb("ones_b", [128, 128], BF16)
            self.S.op('pool', lambda e: e.affine_select(out=striu[:], in_=self.ones_f[:], pattern=[[1, 128]], compare_op=ALU.is_gt,
                                                        fill=0.0, base=0, channel_multiplier=-1), ['ones_f'], ['striu'])
            self.cp('dve', ones_b[:], self.ones_f[:], ['ones_f'], ['ones_b'])
            ecol = ph.sb("ecol", [128, 32]); base = ph.sb("rbase", [128, 32])
            self.S.op('pool', lambda e: e.iota(ecol[:], pattern=[[CAP, 32]], base=1, channel_multiplier=0, allow_small_or_imprecise_dtypes=True), [], ['ecol'])
            self.memset('dve', base[:], 0.0, ['rbase'])
            xg = ph.sb("n_xg", [128, 8, 512]); sq = ph.sb("n_sq", [128, 8, 512]); rstd = ph.sb("n_rstd", [128, 512])
            hT = [ph.sb("h2", [128, 8, 512], BF16) for _ in range(2)]
            lg = ph.sb("lg", [128, 32]); top8 = ph.sb("rtop8", [128, 8]); ntop = ph.sb("ntop", [128, 1]); msk = ph.sb("rmsk", [128, 32])
            mskb = ph.sb("rmskb", [128, 32], BF16)
            ex = ph.sb("rex", [128, 32]); ssum = ph.sb("rsum", [128, 1]); pp = ph.sb("rpp", [128, 32])
            pos = ph.sb("rpos", [128, 32]); val = ph.sb("rval", [128, 32]); ovf = ph.sb("rovf", [128, 32]); v8 = ph.sb("rv8", [128, 8])
            junk = ph.sb("rjunk", [128, 32]); slf = ph.sb("rslf", [128, 4])
            hrow = [ph.sb("hrow", [128, 4, 1026], BF16) for rb_ in range(2)]
            for g in range(NG):
                hb = g % 2
                self.p_norm_group(ph, g, xT_d, A2, B2, hT[hb], ('h2', hb), (xg, sq, rstd))
                for j in range(4):
                    ti = g * 4 + j
                    rb = ti % 2
                    for kc in range(8):
                        self.mm(ps[0][:, 0:32], hT[hb][:, kc, j * 128:(j + 1) * 128], wr[:, kc, :], kc == 0, kc == 7, [('h2', hb), 'wr'], [PS[0]])
                    self.tt('dve', lg[:], ps[0][:, 0:32], br[:], ALU.add, [PS[0], 'br'], ['lg'])
                    self.S.op('dve', lambda e: e.max(out=top8[:], in_=lg[:]), ['lg'], ['rtop8'])
                    self.ts('dve', msk[:], lg[:], top8[:, 3:4], ALU.is_ge, ['lg', 'rtop8'], ['rmsk'])
                    self.cp('dve', mskb[:], msk[:], ['rmsk'], ['rmskb'])
                    self.ts('dve', ntop[:], top8[:, 0:1], -1.0, ALU.mult, ['rtop8'], ['ntop'])
                    self.act(ex[:], lg[:], AF.Exp, ['lg', 'ntop'], ['rex'], bias=ntop[:, 0:1])
                    self.tt('dve', ex[:], ex[:], msk[:], ALU.mult, ['rex', 'rmsk'], ['rex'])
                    self.S.op('dve', lambda e: e.tensor_reduce(out=ssum[:], in_=ex[:], axis=AX.X, op=ALU.add), ['rex'], ['rsum'])
                    self.S.op('dve', lambda e: e.reciprocal(out=ssum[:], in_=ssum[:]), ['rsum'], ['rsum'])
                    self.ts('dve', pp[:], ex[:], ssum[:, 0:1], ALU.mult, ['rex', 'rsum'], ['rpp'])
                    self.mm(ps[1][:, 0:32], striu[:], mskb[:], True, True, ['striu', 'rmskb'], [PS[1]])
                    self.mm(ps[1][:, 32:64], ones_b[:], mskb[:], True, True, ['ones_b', 'rmskb'], [PS[1]])
                    self.tt('dve', pos[:], ps[1][:, 0:32], base[:], ALU.add, [PS[1], 'rbase'], ['rpos'])
                    self.tt('dve', base[:], ps[1][:, 32:64], base[:], ALU.add, [PS[1], 'rbase'], ['rbase'])
                    self.ts('dve', ovf[:], pos[:], float(CAP) - 0.5, ALU.is_gt, ['rpos'], ['rovf'], s2=1.0e6, op1=ALU.mult)
                    self.tt('dve', val[:], pos[:], ecol[:], ALU.add, ['rpos', 'ecol'], ['rval'])
                    self.tt('dve', val[:], val[:], ovf[:], ALU.add, ['rval', 'rovf'], ['rval'])
                    self.tt('dve', val[:], val[:], msk[:], ALU.mult, ['rval', 'rmsk'], ['rval'])
                    self.S.op('dve', lambda e: e.max(out=v8[:], in_=val[:]), ['rval'], ['rv8'])
                    self.ts('dve', slf[:], v8[:, 0:4], -1.0, ALU.add, ['rv8'], ['rslf'])
                    self.cp('dve', slots_all[:, ti, :], slf[:], ['rslf'], [('slots', ti)])
                    for kk in range(8):
                        self.tr(psb[2][:, kk * 128:(kk + 1) * 128], hT[hb][:, kk, j * 128:(j + 1) * 128], self.ident_b[:], [('h2', hb)], [PS[2]])
                    for k in range(4):
                        self.cp('act' if k % 2 else 'pool' if False else 'act', hrow[rb][:, k, 0:1024], psb[2][:, :], [PS[2]], [('hrow', rb, k)])
                        pk = hrow[rb][:, k, 1024:1026].bitcast(F32)
                        self.S.op('dve', lambda e, k=k, pk=pk: e.scalar_tensor_tensor(out=junk[:], in0=val[:], scalar=v8[:, k:k + 1], in1=pp[:],
                                                                                    op0=ALU.is_equal, op1=ALU.mult, accum_out=pk),
                                  ['rval', 'rv8', 'rpp'], ['rjunk', ('hrow', rb, k)])
                        self.S.dma('pool', lambda e, k=k, rb=rb, ti=ti: e.indirect_dma_start(
                            out=Xg_d, out_offset=bass.IndirectOffsetOnAxis(ap=slots_all[:, ti, k:k + 1], axis=0),
                            in_=hrow[rb][:, k, :], in_offset=None, bounds_check=self.bc_reg(e, R - 1), oob_is_err=False),
                            [('hrow', rb, k), ('slots', ti)], [('Xg', ti, k)])

    def p_moe_sparse(self, Xg_d, Yg_d, slots_all, wg_d, wu_d, wd_d, bgT_d, buT_d, bd_d, G2, xT_d, CAP, ibuf=None, n_exp=32):
        ps, PS, psb = self.ps, self.PS, self.psb
        import math
        SIGMAX = 1.0 / (1.0 + math.exp(-1.702 * 7.0))
        NH = CAP // 1024
        R = 32 * CAP
        xT_v = xT_d.rearrange("(kc p) t -> p kc t", p=128)
        with self.phase() as ph:
            bg = ph.sb("bg", [128, 32, 8]); bu = ph.sb("bu", [128, 32, 8]); nbg = ph.sb("nbg", [128, 32, 8])
            self.dma('sp', bg[:], bgT_d, [], ['bg']); self.dma('sp', bu[:], buT_d, [], ['bu'])
            self.ts('dve', nbg[:], bg[:], 1.702, ALU.mult, ['bg'], ['nbg'])
            bdr = [ph.sb("bdr", [1, 1024]) for _ in range(2)]
            xs = [ph.sb("xs", [128, 8, 1026], BF16) for _ in range(2)]
            XT = ph.sb("XT", [128, 8, 1024], BF16)
            actT = ph.sb("actT", [128, 8, 1024], BF16)
            stg = [ph.sb("mstg", [128, 8, 512]) for _ in range(2)]
            wgb = [ph.sb("wgb", [128, 8, 512], BF16) for _ in range(2)]
            wub = [ph.sb("wub", [128, 8, 512], BF16) for _ in range(2)]
            wdb = [ph.sb("wdb", [128, 8, 512], BF16) for _ in range(2)]
            gt = ph.sb("gtw", [128, 1024]); e1 = ph.sb("e1w", [128, 1024]); u1 = ph.sb("u1w", [128, 1024])
            yo = [ph.sb("yo", [128, 512]) for _ in range(4)]
            cnt = {'stg': 0, 'wg': 0, 'wu': 0, 'wd': 0, 'yo': 0, 'xs': 0}

            def load_blk(w_d, e, blk, dst_list, dk, conv_eng):
                si = cnt['stg'] % 2
                cnt['stg'] += 1
                di = cnt[dk] % 2
                cnt[dk] += 1
                wv = w_d[e].rearrange("(kc p) n -> p kc n", p=128)
                self.dma('sp', stg[si][:], wv[:, :, blk * 512:(blk + 1) * 512], [], [('mstg', si)])
                for kc in range(8):
                    eng = conv_eng[kc % len(conv_eng)]
                    self.cp(eng, dst_list[di][:, kc, :], stg[si][:, kc, :], [('mstg', si)], [(dk, di)])
                return dst_list[di], (dk, di)

            units = []
            for e in range(n_exp):
                for hh in range(NH):
                    units += [('x', e, hh, 0), ('gu', e, hh, 0), ('gu', e, hh, 1), ('d', e, hh, 0), ('d', e, hh, 1)]

            def prep(u):
                kind, e, hh, blk = u
                if kind == 'x':
                    xb = cnt['xs'] % 2
                    cnt['xs'] += 1
                    r0 = e * CAP + hh * 1024
                    self.dma('sp', xs[xb][:], Xg_d[r0:r0 + 1024, :].rearrange("(s p) c -> p s c", p=128), [], [('xs', xb)])
                    self.dma('sp', bdr[xb][:], bd_d[e:e + 1, :], [], [('bdr', xb)])
                    return (xb,)
                if kind == 'gu':
                    return (load_blk(wg_d, e, blk, wgb, 'wg', ['act', 'pool', 'act']), load_blk(wu_d, e, blk, wub, 'wu', ['act', 'act', 'pool']))
                return (load_blk(wd_d, e, blk, wdb, 'wd', ['act', 'pool', 'act']),)

            cur = {'xb': 0}

            def compute(u, hd):
                kind, e, hh, blk = u
                if kind == 'x':
                    xb = hd[0]
                    cur['xb'] = xb
                    for kc in range(8):
                        bk = 6 + (kc % 2)
                        for st in range(8):
                            self.tr(psb[bk][:, st * 128:(st + 1) * 128], xs[xb][:, st, kc * 128:(kc + 1) * 128], self.ident_b[:], [('xs', xb)], [PS[bk]])
                        self.cp('dve', XT[:, kc, :], psb[bk][:, :], [PS[bk]], [('XT', kc)])
                elif kind == 'gu':
                    (wg_t, wgk), (wu_t, wuk) = hd
                    XTk = [('XT', kc) for kc in range(8)]
                    for f4 in range(4):
                        fc = blk * 4 + f4
                        kg, ke, ku = 'gtw', 'e1w', 'u1w'
                        for hf in range(2):
                            hcols = slice(hf * 512, (hf + 1) * 512)
                            ba, bb = 2 * hf, 2 * hf + 1
                            for kc in range(8):
                                self.mm(ps[ba][:], wg_t[:, kc, f4 * 128:(f4 + 1) * 128], XT[:, kc, hcols], kc == 0, kc == 7, [wgk] + XTk, [PS[ba]])
                            for kc in range(8):
                                self.mm(ps[bb][:], wu_t[:, kc, f4 * 128:(f4 + 1) * 128], XT[:, kc, hcols], kc == 0, kc == 7, [wuk] + XTk, [PS[bb]])
                            self.act(e1[:, hcols], ps[ba][:], AF.Sigmoid, [PS[ba], 'nbg'], [ke], bias=nbg[:, e, fc:fc + 1], scale=1.702)
                            self.ts('dve', gt[:, hcols], ps[ba][:], bg[:, e, fc:fc + 1], ALU.add, [PS[ba], 'bg'], [kg], s2=7.0, op1=ALU.min)
                            self.ts('dve', u1[:, hcols], ps[bb][:], bu[:, e, fc:fc + 1], ALU.add, [PS[bb], 'bu'], [ku], s2=7.0, op1=ALU.min)
                        self.act(u1[:], u1[:], AF.Relu, [ku], [ku], bias=self.c7[:, 0:1])
                        self.stt(gt[:], e1[:], SIGMAX, gt[:], ALU.min, ALU.mult, [ke, kg], [kg])
                        self.stt(actT[:, fc, :], u1[:], -6.0, gt[:], ALU.add, ALU.mult, [ku, kg], [('actT', fc)])
                else:
                    (wd_t, wdk), = hd
                    xb = cur['xb']
                    r0 = e * CAP + hh * 1024
                    for st in range(8):
                        yi = cnt['yo'] % 4
                        cnt['yo'] += 1
                        bk = 4 + (st % 2)
                        pcol = xs[xb][:, st, 1024:1026].bitcast(F32)
                        for fc in range(8):
                            self.mm(ps[bk][:], actT[:, fc, st * 128:(st + 1) * 128], wd_t[:, fc, :], fc == 0, False, [wdk, ('actT', fc)], [PS[bk]])
                        self.mm(ps[bk][:], self.ones_f[0:1, :], bdr[xb][0:1, blk * 512:(blk + 1) * 512], False, True, [('bdr', xb)], [PS[bk]])
                        self.act(yo[yi][:], ps[bk][:], AF.Copy, [PS[bk], ('xs', xb)], [('yo', yi)], scale=pcol)
                        self.dma('sp', Yg_d[r0 + st * 128:r0 + (st + 1) * 128, blk * 512:(blk + 1) * 512], yo[yi][:], [('yo', yi)], [('Yg', e, hh, st, blk)])

            hd_next = prep(units[0])
            for ui, u in enumerate(units):
                hd = hd_next
                hd_next = prep(units[ui + 1]) if ui + 1 < len(units) else None
                compute(u, hd)
        with self.phase() as ph:
            acc4 = [ph.sb("acc4", [128, 4, 1024]) for ab_ in range(2)]
            ysum = ph.sb("ysum", [128, 1024])
            xg = [ph.sb("cxg", [128, 8, 512]) for _ in range(2)]
            for g in range(NG):
                b = g % 2
                self.dma('sp', xg[b][:], xT_v[:, :, g * 512:(g + 1) * 512], [('xT', g)], [('cxg', b)])
                for j in range(4):
                    ti = g * 4 + j
                    ab = ti % 2
                    self.memset('pool', acc4[ab][:], 0.0, [('acc4', ab)])
                    for k in range(4):
                        self.S.dma('pool', lambda e, k=k, ab=ab, ti=ti: e.indirect_dma_start(
                            out=acc4[ab][:, k, :], out_offset=None, in_=Yg_d,
                            in_offset=bass.IndirectOffsetOnAxis(ap=slots_all[:, ti, k:k + 1], axis=0), bounds_check=self.bc_reg(e, R - 1), oob_is_err=False),
                            [('slots', ti)], [('acc4', ab)])
                    self.tt('dve', ysum[:], acc4[ab][:, 0, :], acc4[ab][:, 1, :], ALU.add, [('acc4', ab)], ['ysum'])
                    self.tt('dve', ysum[:], ysum[:], acc4[ab][:, 2, :], ALU.add, [('acc4', ab), 'ysum'], ['ysum'])
                    self.tt('dve', ysum[:], ysum[:], acc4[ab][:, 3, :], ALU.add, [('acc4', ab), 'ysum'], ['ysum'])
                    for kc in range(8):
                        bk = kc % 4
                        self.tr(ps[bk][:, 0:128], ysum[:, kc * 128:(kc + 1) * 128], self.ident_f[:], ['ysum'], [PS[bk]])
                        self.stt(xg[b][:, kc, j * 128:(j + 1) * 128], ps[bk][:, 0:128], G2[:, kc:kc + 1], xg[b][:, kc, j * 128:(j + 1) * 128],
                                 ALU.mult, ALU.add, [PS[bk], ('cxg', b), 'lconst'], [('cxg', b)])
                self.dma('sp', xT_v[:, :, g * 512:(g + 1) * 512], xg[b][:], [('cxg', b)], [('xT', g)])

    def p_router_blocks(self, xT_d, A2, B2, w_router_d, brbc_d, Xg_d, h2rows_d, slots_all, widx, besb, NBLK, BS=1024):
        ps, PS, psb = self.ps, self.PS, self.psb
        R = NBLK * BS
        with self.phase() as ph:
            wr = ph.sb("wr", [128, 8, 32], BF16); wrs = ph.sb("wrs", [128, 8, 32]); br = ph.sb("br", [128, 32])
            self.dma('sp', wrs[:], w_router_d.rearrange("(kc p) n -> p kc n", p=128), [], ['wrs'])
            self.cp('dve', wr[:], wrs[:], ['wrs'], ['wr'])
            self.dma('sp', br[:], brbc_d.partition_broadcast(128), [], ['br'])
            striu = ph.sb("striu", [128, 128], BF16); ones_b = ph.sb("ones_b", [128, 128], BF16)
            self.S.op('pool', lambda e: e.affine_select(out=striu[:], in_=self.ones_f[:], pattern=[[1, 128]], compare_op=ALU.is_gt,
                                                        fill=0.0, base=0, channel_multiplier=-1), ['ones_f'], ['striu'])
            self.cp('dve', ones_b[:], self.ones_f[:], ['ones_f'], ['ones_b'])
            base = ph.sb("rbase", [128, 32])
            self.memset('dve', base[:], 0.0, ['rbase'])
            mskA = ph.sb("mskA", [128, NTILE, 32]); ppA = ph.sb("ppA", [128, NTILE, 32]); posA = ph.sb("posA", [128, NTILE, 32])
            xg = ph.sb("n_xg", [128, 8, 512]); sq = ph.sb("n_sq", [128, 8, 512]); rstd = ph.sb("n_rstd", [128, 512])
            hT = [ph.sb("h2", [128, 8, 512], BF16) for _ in range(2)]
            lg = ph.sb("lg", [128, 32]); top8 = ph.sb("rtop8", [128, 8]); ntop = ph.sb("ntop", [128, 1])
            mskb = ph.sb("rmskb", [128, 32], BF16); ex = ph.sb("rex", [128, 32]); ssum = ph.sb("rsum", [128, 1])
            hr = [ph.sb("hr", [128, 1024], BF16) for _ in range(2)]
            for g in range(NG):
                hb = g % 2
                self.p_norm_group(ph, g, xT_d, A2, B2, hT[hb], ('h2', hb), (xg, sq, rstd))
                for j in range(4):
                    ti = g * 4 + j
                    rb = ti % 2
                    msk = mskA[:, ti, :]
                    for kc in range(8):
                        self.mm(ps[0][:, 0:32], hT[hb][:, kc, j * 128:(j + 1) * 128], wr[:, kc, :], kc == 0, kc == 7, [('h2', hb), 'wr'], [PS[0]])
                    self.tt('dve', lg[:], ps[0][:, 0:32], br[:], ALU.add, [PS[0], 'br'], ['lg'])
                    self.S.op('dve', lambda e: e.max(out=top8[:], in_=lg[:]), ['lg'], ['rtop8'])
                    self.ts('dve', msk, lg[:], top8[:, 3:4], ALU.is_ge, ['lg', 'rtop8'], [('mskA', ti)])
                    self.cp('dve', mskb[:], msk, [('mskA', ti)], ['rmskb'])
                    self.ts('dve', ntop[:], top8[:, 0:1], -1.0, ALU.mult, ['rtop8'], ['ntop'])
                    self.act(ex[:], lg[:], AF.Exp, ['lg', 'ntop'], ['rex'], bias=ntop[:, 0:1])
                    self.tt('dve', ex[:], ex[:], msk, ALU.mult, ['rex', ('mskA', ti)], ['rex'])
                    self.S.op('dve', lambda e: e.tensor_reduce(out=ssum[:], in_=ex[:], axis=AX.X, op=ALU.add), ['rex'], ['rsum'])
                    self.S.op('dve', lambda e: e.reciprocal(out=ssum[:], in_=ssum[:]), ['rsum'], ['rsum'])
                    self.ts('dve', ppA[:, ti, :], ex[:], ssum[:, 0:1], ALU.mult, ['rex', 'rsum'], [('ppA', ti)])
                    self.mm(ps[1][:, 0:32], striu[:], mskb[:], True, True, ['striu', 'rmskb'], [PS[1]])
                    self.mm(ps[1][:, 32:64], ones_b[:], mskb[:], True, True, ['ones_b', 'rmskb'], [PS[1]])
                    self.tt('dve', posA[:, ti, :], ps[1][:, 0:32], base[:], ALU.add, [PS[1], 'rbase'], [('posA', ti)])
                    self.tt('dve', base[:], ps[1][:, 32:64], base[:], ALU.add, [PS[1], 'rbase'], ['rbase'])
                    for kk in range(8):
                        self.tr(psb[2][:, kk * 128:(kk + 1) * 128], hT[hb][:, kk, j * 128:(j + 1) * 128], self.ident_b[:], [('h2', hb)], [PS[2]])
                    self.cp('act', hr[rb][:], psb[2][:, :], [PS[2]], [('hr', rb)])
                    self.dma('sp', h2rows_d[ti * 128:(ti + 1) * 128, :], hr[rb][:], [('hr', rb)], [('h2rows', ti)])
            nblk = ph.sb("nblk", [128, 32]); tmpc = ph.sb("tmpc", [128, 32]); bend = ph.sb("bend", [128, 32]); bst1 = ph.sb("bst1", [128, 32])
            ones32 = ph.sb("ones32", [128, 32])
            self.memset('dve', ones32[:], 1.0, ['ones32'])
            self.ts('dve', nblk[:], base[:], 0.0, ALU.is_gt, ['rbase'], ['nblk'])
            for m in range(1, NT // BS):
                self.ts('dve', tmpc[:], base[:], float(BS * m), ALU.is_gt, ['rbase'], ['tmpc'])
                self.tt('dve', nblk[:], nblk[:], tmpc[:], ALU.add, ['nblk', 'tmpc'], ['nblk'])
            self.S.op('dve', lambda e: e.tensor_tensor_scan(out=bend[:], data0=ones32[:], data1=nblk[:], initial=0.0, op0=ALU.mult, op1=ALU.add),
                      ['ones32', 'nblk'], ['bend'])
            self.tt('dve', bst1[:], bend[:], nblk[:], ALU.subtract, ['bend', 'nblk'], ['bst1'])
            self.ts('dve', bst1[:], bst1[:], float(BS), ALU.mult, ['bst1'], ['bst1'], s2=1.0, op1=ALU.add)
            jidx = ph.sb("jidx", [128, NBLK]); cmp = ph.sb("bcmp", [128, NBLK, 32]); kp = ph.sb("kp", [128, 8]); widf = ph.sb("widf", [128, NBLK, 8])
            self.S.op('pool', lambda e: e.iota(jidx[:], pattern=[[1, NBLK]], base=0, channel_multiplier=0, allow_small_or_imprecise_dtypes=True), [], ['jidx'])
            self.S.op('pool', lambda e: e.iota(kp[:], pattern=[[128, 8]], base=0, channel_multiplier=1, allow_small_or_imprecise_dtypes=True), [], ['kp'])
            self.tt('dve', cmp[:], bend[:].unsqueeze(1).to_broadcast([128, NBLK, 32]), jidx[:].unsqueeze(2).to_broadcast([128, NBLK, 32]), ALU.is_le,
                    ['bend', 'jidx'], ['bcmp'])
            self.S.op('dve', lambda e: e.tensor_reduce(out=besb[:], in_=cmp[:], axis=AX.X, op=ALU.add), ['bcmp'], ['besb'])
            self.stt(widf[:], besb[:].unsqueeze(2).to_broadcast([128, NBLK, 8]), 1024.0, kp[:].unsqueeze(1).to_broadcast([128, NBLK, 8]), ALU.mult, ALU.add,
                     ['besb', 'kp'], ['widf'])
            self.cp('dve', widx[:], widf[:], ['widf'], ['widx'])
            val = ph.sb("rval", [128, 32]); v8 = ph.sb("rv8", [128, 8]); junk = ph.sb("rjunk", [128, 32]); slf = ph.sb("rslf", [128, 4])
            hrow = [ph.sb("hrow", [128, 4, 1026], BF16) for _ in range(2)]
            for ti in range(NTILE):
                rb = ti % 2
                self.dma('sp', hr[rb][:], h2rows_d[ti * 128:(ti + 1) * 128, :], [('h2rows', ti)], [('hr', rb)])
                self.tt('dve', val[:], posA[:, ti, :], bst1[:], ALU.add, [('posA', ti), 'bst1'], ['rval'])
                self.tt('dve', val[:], val[:], mskA[:, ti, :], ALU.mult, ['rval', ('mskA', ti)], ['rval'])
                self.S.op('dve', lambda e: e.max(out=v8[:], in_=val[:]), ['rval'], ['rv8'])
                self.ts('dve', slf[:], v8[:, 0:4], -1.0, ALU.add, ['rv8'], ['rslf'])
                self.cp('dve', slots_all[:, ti, :], slf[:], ['rslf'], [('slots', ti)])
                for k in range(4):
                    self.cp('act' if k % 2 else 'pool', hrow[rb][:, k, 0:1024], hr[rb][:], [('hr', rb)], [('hrow', rb, k)])
                    pk = hrow[rb][:, k, 1024:1026].bitcast(F32)
                    self.S.op('dve', lambda e, k=k, pk=pk, ti=ti: e.scalar_tensor_tensor(out=junk[:], in0=val[:], scalar=v8[:, k:k + 1], in1=ppA[:, ti, :],
                                                                                       op0=ALU.is_equal, op1=ALU.mult, accum_out=pk),
                              ['rval', 'rv8', ('ppA', ti)], ['rjunk', ('hrow', rb, k)])
                    self.S.dma('pool', lambda e, k=k, rb=rb, ti=ti: e.indirect_dma_start(
                        out=Xg_d, out_offset=bass.IndirectOffsetOnAxis(ap=slots_all[:, ti, k:k + 1], axis=0),
                        in_=hrow[rb][:, k, :], in_offset=None, bounds_check=self.bc_reg(e, R - 1), oob_is_err=False),
                        [('hrow', rb, k), ('slots', ti)], [('Xg', ti, k)])

    def p_moe_blocks(self, Xg_d, Yg_d, slots_all, widx, besb, wg_d, wu_d, wd_d, bgT_d, buT_d, bd_d, G2, xT_d, NBLK, BS=1024):
        ps, PS, psb = self.ps, self.PS, self.psb
        import math
        SIGMAX = 1.0 / (1.0 + math.exp(-1.702 * 7.0))
        R = NBLK * BS
        NS = BS // 128
        NHF = BS // 512
        WR = 32 * 1024
        xT_v = xT_d.rearrange("(kc p) t -> p kc t", p=128)
        wflat = {'wg': wg_d.rearrange("e k n -> (e k) n"), 'wu': wu_d.rearrange("e k n -> (e k) n"), 'wd': wd_d.rearrange("e k n -> (e k) n")}
        with self.phase() as ph:
            bg = ph.sb("bg", [128, 32, 8]); bu = ph.sb("bu", [128, 32, 8]); bda = ph.sb("bda", [32, 1024])
            self.dma('sp', bg[:], bgT_d, [], ['bg']); self.dma('sp', bu[:], buT_d, [], ['bg']); self.dma('sp', bda[:], bd_d, [], ['bg'])
            efree = ph.sb("efree", [128, 32]); epart = ph.sb("epart", [32, 1])
            self.S.op('pool', lambda e: e.iota(efree[:], pattern=[[1, 32]], base=0, channel_multiplier=0, allow_small_or_imprecise_dtypes=True), [], ['bg'])
            self.S.op('pool', lambda e: e.iota(epart[:], pattern=[[0, 1]], base=0, channel_multiplier=1, allow_small_or_imprecise_dtypes=True), [], ['bg'])
            oh = ph.sb("oh", [128, 32]); ohc = ph.sb("ohc", [32, 1]); btmp = ph.sb("btmp", [128, 32, 8])
            bsel = [ph.sb("bsel", [128, 3, 8]) for _ in range(2)]
            bdr = [ph.sb("bdr", [1, 1024]) for _ in range(2)]
            xs = [ph.sb("xs", [128, NS, 1026], BF16) for _ in range(2)]
            XT = ph.sb("XT", [128, 8, BS], BF16)
            actT = ph.sb("actT", [128, 8, BS], BF16)
            wfb = {'wg': [ph.sb("wgf", [128, 8, 1024], BF16) for _ in range(2)], 'wu': [ph.sb("wuf", [128, 8, 1024], BF16) for _ in range(2)],
                   'wd': [ph.sb("wdf", [128, 8, 1024], BF16)]}
            gtw = [ph.sb("gtw", [128, BS]) for _ in range(2)]; e1w = [ph.sb("e1w", [128, BS]) for _ in range(2)]; u1w = [ph.sb("u1w", [128, BS]) for _ in range(2)]
            yo = [ph.sb("yo", [128, 512]) for _ in range(4)]
            cnt = {'yo': 0, 'tw': 0}

            def gather_w(kind, j):
                wb = j % len(wfb[kind])
                for kc in range(8):
                    self.S.dma('pool', lambda e, kind=kind, j=j, kc=kc, wb=wb: e.indirect_dma_start(
                        out=wfb[kind][wb][:, kc, :], out_offset=None, in_=wflat[kind],
                        in_offset=bass.IndirectOffsetOnAxis(ap=widx[:, j, kc:kc + 1], axis=0), bounds_check=self.bc_reg(e, WR - 1), oob_is_err=False),
                        ['widx'], [(kind, wb, kc)])

            units = []
            for j in range(NBLK):
                units += [('x', j), ('gu', j), ('d', j)]

            def prep(u):
                kind, j = u
                xb = j % 2
                if kind == 'x':
                    self.dma('sp', xs[xb][:], Xg_d[j * BS:(j + 1) * BS, :].rearrange("(s p) c -> p s c", p=128), [], [('xs', xb)])
                    self.ts('dve', oh[:], efree[:], besb[:, j:j + 1], ALU.is_equal, ['bg', 'besb'], ['oh'])
                    for bi, src in enumerate((bg, bu)):
                        self.tt('dve', btmp[:], src[:], oh[:].unsqueeze(2).to_broadcast([128, 32, 8]), ALU.mult, ['bg', 'oh'], ['btmp'])
                        self.S.op('dve', lambda e, bi=bi, xb=xb: e.tensor_reduce(out=bsel[xb][:, 2 * bi, :], in_=btmp[:].rearrange("p e f -> p f e"), axis=AX.X, op=ALU.add),
                                  ['btmp'], [('bsel', xb)])
                    self.ts('dve', bsel[xb][:, 1, :], bsel[xb][:, 0, :], 1.702, ALU.mult, [('bsel', xb)], [('bsel', xb)])
                    self.ts('dve', ohc[:], epart[:], besb[0:32, j:j + 1], ALU.is_equal, ['bg', 'besb'], ['ohc'])
                    for hf in range(2):
                        self.mm(ps[7][0:1, :], ohc[:, 0:1], bda[:, hf * 512:(hf + 1) * 512], True, True, ['ohc', 'bg'], [PS[7]])
                        self.cp('dve', bdr[xb][0:1, hf * 512:(hf + 1) * 512], ps[7][0:1, :], [PS[7]], [('bdr', xb)])
                elif kind == 'gu':
                    gather_w('wg', j)
                    gather_w('wu', j)
                else:
                    gather_w('wd', j)

            def compute(u):
                kind, j = u
                xb = j % 2
                if kind == 'x':
                    for kc in range(8):
                        bk = 5 + (kc % 2)
                        for st in range(NS):
                            self.tr(psb[bk][:, st * 128:(st + 1) * 128], xs[xb][:, st, kc * 128:(kc + 1) * 128], self.ident_b[:], [('xs', xb)], [PS[bk]])
                        self.cp('dve', XT[:, kc, :], psb[bk][:, 0:BS], [PS[bk]], [('XT', kc)])
                elif kind == 'gu':
                    XTk = [('XT', kc) for kc in range(8)]
                    wgk = [('wg', j % 2, kc) for kc in range(8)]
                    wuk = [('wu', j % 2, kc) for kc in range(8)]
                    wf = {'wg': wfb['wg'][j % 2], 'wu': wfb['wu'][j % 2]}
                    bs = bsel[xb]
                    for fc in range(8):
                        ti_ = cnt['tw'] % 2
                        cnt['tw'] += 1
                        gt, e1, u1 = gtw[ti_], e1w[ti_], u1w[ti_]
                        kg, ke, ku = ('gtw', ti_), ('e1w', ti_), ('u1w', ti_)
                        for hf in range(NHF):
                            hcols = slice(hf * 512, (hf + 1) * 512)
                            ba, bb = 2 * hf, 2 * hf + 1
                            for kc in range(8):
                                self.mm(ps[ba][:], wf['wg'][:, kc, fc * 128:(fc + 1) * 128], XT[:, kc, hcols], kc == 0, kc == 7, wgk + XTk, [PS[ba]])
                            for kc in range(8):
                                self.mm(ps[bb][:], wf['wu'][:, kc, fc * 128:(fc + 1) * 128], XT[:, kc, hcols], kc == 0, kc == 7, wuk + XTk, [PS[bb]])
                            self.act(e1[:, hcols], ps[ba][:], AF.Sigmoid, [PS[ba], ('bsel', xb)], [ke], bias=bs[:, 1, fc:fc + 1], scale=1.702)
                            self.ts('dve', gt[:, hcols], ps[ba][:], bs[:, 0, fc:fc + 1], ALU.add, [PS[ba], ('bsel', xb)], [kg], s2=7.0, op1=ALU.min)
                            self.ts('dve', u1[:, hcols], ps[bb][:], bs[:, 2, fc:fc + 1], ALU.add, [PS[bb], ('bsel', xb)], [ku], s2=7.0, op1=ALU.min)
                        self.act(u1[:], u1[:], AF.Relu, [ku], [ku], bias=self.c7[:, 0:1])
                        self.stt(gt[:], e1[:], SIGMAX, gt[:], ALU.min, ALU.mult, [ke, kg], [kg])
                        self.stt(actT[:, fc, :], u1[:], -6.0, gt[:], ALU.add, ALU.mult, [ku, kg], [('actT', fc)])
                else:
                    wdk = [('wd', 0, kc) for kc in range(8)]
                    wf = {'wd': wfb['wd'][0]}
                    r0 = j * BS
                    for st in range(NS):
                        pcol = xs[xb][:, st, 1024:1026].bitcast(F32)
                        for blk in range(2):
                            yi = cnt['yo'] % 4
                            cnt['yo'] += 1
                            bk = 4 if blk == 0 else 7
                            for fc in range(8):
                                self.mm(ps[bk][:], actT[:, fc, st * 128:(st + 1) * 128], wf['wd'][:, fc, blk * 512:(blk + 1) * 512], fc == 0, False,
                                        wdk + [('actT', fc)], [PS[bk]])
                            self.mm(ps[bk][:], self.ones_f[0:1, :], bdr[xb][0:1, blk * 512:(blk + 1) * 512], False, True, [('bdr', xb)], [PS[bk]])
                            self.act(yo[yi][:], ps[bk][:], AF.Copy, [PS[bk], ('xs', xb)], [('yo', yi)], scale=pcol)
                            self.dma('sp', Yg_d[r0 + st * 128:r0 + (st + 1) * 128, blk * 512:(blk + 1) * 512], yo[yi][:], [('yo', yi)], [('Yg', j, st, blk)])

            prep(units[0])
            for ui0 in range(1, 5):
                if ui0 < len(units) and units[ui0][0] == 'gu':
                    prep(units[ui0])
            for ui, u in enumerate(units):
                if ui + 1 < len(units) and units[ui + 1][0] != 'gu':
                    prep(units[ui + 1])
                if ui + 5 < len(units) and units[ui + 5][0] == 'gu':
                    prep(units[ui + 5])
                compute(u)
        with self.phase() as ph:
            acc4 = [ph.sb("acc4", [128, 4, 1024]) for ab_ in range(2)]
            ysum = ph.sb("ysum", [128, 1024])
            xg = [ph.sb("cxg", [128, 8, 512]) for _ in range(2)]
            for g in range(NG):
                b = g % 2
                self.dma('sp', xg[b][:], xT_v[:, :, g * 512:(g + 1) * 512], [('xT', g)], [('cxg', b)])
                for j in range(4):
                    ti = g * 4 + j
                    ab = ti % 2
                    for k in range(4):
                        self.S.dma('pool', lambda e, k=k, ab=ab, ti=ti: e.indirect_dma_start(
                            out=acc4[ab][:, k, :], out_offset=None, in_=Yg_d,
                            in_offset=bass.IndirectOffsetOnAxis(ap=slots_all[:, ti, k:k + 1], axis=0), bounds_check=self.bc_reg(e, R - 1), oob_is_err=False),
                            [('slots', ti)], [('acc4', ab)])
                    self.tt('dve', ysum[:], acc4[ab][:, 0, :], acc4[ab][:, 1, :], ALU.add, [('acc4', ab)], ['ysum'])
                    self.tt('dve', ysum[:], ysum[:], acc4[ab][:, 2, :], ALU.add, [('acc4', ab), 'ysum'], ['ysum'])
                    self.tt('dve', ysum[:], ysum[:], acc4[ab][:, 3, :], ALU.add, [('acc4', ab), 'ysum'], ['ysum'])
                    for kc in range(8):
                        bk = kc % 4
                        self.tr(ps[bk][:, 0:128], ysum[:, kc * 128:(kc + 1) * 128], self.ident_f[:], ['ysum'], [PS[bk]])
                        self.stt(xg[b][:, kc, j * 128:(j + 1) * 128], ps[bk][:, 0:128], G2[:, kc:kc + 1], xg[b][:, kc, j * 128:(j + 1) * 128],
                                 ALU.mult, ALU.add, [PS[bk], ('cxg', b), 'lconst'], [('cxg', b)])
                self.dma('sp', xT_v[:, :, g * 512:(g + 1) * 512], xg[b][:], [('cxg', b)], [('xT', g)])

    def p_final(self, xT_d, gfT_d, out_d):
        ps, PS = self.ps, self.PS
        with self.phase() as ph:
            gf = ph.sb("gf", [128, 8]); zb = ph.sb("zb", [128, 8])
            self.dma('sp', gf[:], gfT_d, [], ['lconst'])
            self.memset('dve', zb[:], 0.0, ['lconst'])
            xg = ph.sb("n_xg", [128, 8, 512]); sq = ph.sb("n_sq", [128, 8, 512]); rstd = ph.sb("n_rstd", [128, 512])
            yT = [ph.sb("yT", [128, 8, 512]) for _ in range(2)]
            ot = [ph.sb("ot", [128, D]) for _ in range(2)]
            for g in range(NG):
                hb = g % 2
                self.p_norm_group(ph, g, xT_d, gf, zb, yT[hb], ('yT', hb), (xg, sq, rstd))
                for j in range(4):
                    ti = g * 4 + j
                    ob = ti % 2
                    for k2 in range(2):
                        bk = k2
                        for kk in range(4):
                            kc = k2 * 4 + kk
                            self.tr(ps[bk][:, kk * 128:(kk + 1) * 128], yT[hb][:, kc, j * 128:(j + 1) * 128], self.ident_f[:], [('yT', hb)], [PS[bk]])
                        self.cp('act' if k2 else 'dve', ot[ob][:, k2 * 512:(k2 + 1) * 512], ps[bk][:], [PS[bk]], [('ot', ob)])
                    self.dma('sp', out_d[ti * 128:(ti + 1) * 128, :], ot[ob][:], [('ot', ob)], [('out', ti)])


    def p_conv_fix(self, zq0_d, halo_d, selc, cwT_d, cbT_d, cw, cb, ncb, qkfix_d, ktmfix_d, halo_packed=False):
        ps, PS, psb = self.ps, self.PS, self.psb
        with self.phase() as ph:
            zin = ph.sb("fzin", [128, 8, 131]); acc = ph.sb("facc", [128, 128]); ee = ph.sb("fee", [128, 128])
            qo = ph.sb("fqo", [128, 8, 128], BF16); kt = ph.sb("fkt", [128, 512], BF16)
            self.dma('sp', cw[:], cwT_d, [], ['cw'])
            self.dma('sp', cb[:], cbT_d, [], ['cw'])
            self.ts('dve', ncb[:], cb[:], -1.0, ALU.mult, ['cw'], ['cw'])
            if halo_packed:
                self.dma('sp', zin[:, :, 0:3], halo_d.rearrange("p (cc t) -> p cc t", t=3), [], ['fzin'])
            else:
                self.dma('sp', zin[:, :, 0:3], halo_d.rearrange("(cc p) t -> p cc t", p=128), [], ['fzin'])
            self.dma('sp', zin[:, :, 3:131], zq0_d.rearrange("(cc p) t -> p cc t", p=128), [], ['fzin'])
            self.ts('dve', zin[:, :, 0:3], zin[:, :, 0:3], selc[:, 0:1], ALU.mult, ['fzin', 'selb'], ['fzin'])
            for cc in range(8):
                self.conv_chunk(zin[:, cc, :], 128, cw, cb, ncb, cc, acc, ee, qo[:, cc, :], ['fzin'], ['fqo'])
            self.dma('sp', qkfix_d.rearrange("(cc p) t -> p cc t", p=128), qo[:], ['fqo'], ['qkfix'])
            for h in range(4):
                self.tr(psb[0][:, h * 128:(h + 1) * 128], qo[:, 4 + h, :], self.ident_b[:], ['fqo'], [PS[0]])
            self.cp('dve', kt[:], psb[0][:, 0:512], [PS[0]], ['fkt'])
            self.dma('sp', ktmfix_d, kt[:], ['fkt'], ['ktmfix'])


SCRATCH_KIND = "Internal"


def build_launch(first, last):
    B = Builder()
    B.scratch_kind = SCRATCH_KIND
    I = lambda name, shape, dt=F32: B.dram(name, shape, dt, kind="ExternalInput")
    O = lambda name, shape, dt=F32: B.dram(name, shape, dt, kind="ExternalOutput")
    T = lambda name, shape, dt=F32: B.dram(name, shape, dt, kind=SCRATCH_KIND)
    B.make_consts()
    modT = B.sbp("modT", [128, 48]); LC = B.sbp("LC", [128, 6, 8])
    gates = B.sbp("gates", [128, NTILE, 8])
    cw = B.sbp("cw", [128, 8, 4]); cb = B.sbp("cb", [128, 8]); ncb = B.sbp("ncb", [128, 8])
    Cst = B.sbp("Cst", [128, 4, 129])
    cs = B.sbp("cs", [128, NTILE, 8]); sn = B.sbp("sn", [128, NTILE, 8])
    PT = B.sbp("PT", [32, NT])
    selc = B.sbp("selc", [128, 1]); selb = B.sbp("selb", [128, 1])
    pos_d = I("pos", [128, NTILE], I32)
    B.p_rotary_tables(pos_d, cs, sn)
    if first:
        x_d = I("x", [NT, D])
        xT = O("xT_out", [D, NT])
        B.p_x_to_xT(x_d, xT)
    else:
        xT_in = I("xT_in", [D, NT]); zvo = I("zvo_in", [NT, 1024]); qkT = I("qkT_in", [D, NT], BF16); ktm = I("ktm_in", [NT, 512], BF16)
        qrot = I("qrot_in", [NT, 512]); gat_in = I("gates_in", [128, NTILE, 8])
        KTa_o = I("KTa_o", [8, 128, NT], BF16); Vaug_o = I("Vaug_o", [8, NT, 65], BF16); kmT_o = I("kmT_o", [64, 8, 16])
        KTa_p = I("KTa_p", [8, 128, NT], BF16); Vaug_p = I("Vaug_p", [8, NT, 65], BF16); kmT_p = I("kmT_p", [64, 8, 16])
        st_p = I("st_p", [128, 4, 129]); halo_p = I("halo_p", [D, 3]); zq0 = I("zq0_in", [D, 128]); modT_in = I("modT_in", [128, 48])
        sel_d = I("sel", [128, 1])
        b_g1n = I("b_g1n", [128, 8]); b_g2n = I("b_g2n", [128, 8]); b_cwT = I("b_cwT", [128, 8, 4]); b_cbT = I("b_cbT", [128, 8])
        b_gmbc = I("b_gmbc", [1, 512]); b_wout = I("b_wout", [D, D]); b_wr = I("b_wr", [D, 32]); b_brbc = I("b_brbc", [1, 32])
        b_wg = I("b_wg", [32, D, D]); b_wu = I("b_wu", [32, D, D]); b_wd = I("b_wd", [32, D, D])
        b_bgT = I("b_bgT", [128, 32, 8]); b_buT = I("b_buT", [128, 32, 8]); b_bd = I("b_bd", [32, D])
        xT = T("xT_work", [D, NT]) if last else O("xT_out", [D, NT])
        catT = T("catT", [D, NT], BF16); h2T = T("h2T", [D, NT], BF16)
        qkfix = T("qkfix", [D, 128], BF16); ktmfix = T("ktmfix", [128, 512], BF16)
        B.dma('sp', selc[:], sel_d, [], ['selb'])
        B.ts('dve', selb[:], selc[:], -1.0, ALU.add, ['selb'], ['selb'], s2=1e30, op1=ALU.mult)
        B.dma('sp', modT[:], modT_in, [], ['modT'])
        B.dma('sp', gates[:], gat_in, [], ['gates'])
        B.dma('sp', Cst[:], st_p, [], ['Cst'])
        B.ts('dve', Cst[:], Cst[:], selc[:, 0:1], ALU.mult, ['Cst', 'selb'], ['Cst'])
        B.S.barrier()
        B.p_layer_consts(modT, b_g1n, b_g2n, LC)
        B.p_conv_fix(zq0, halo_p, selc, b_cwT, b_cbT, cw, cb, ncb, qkfix, ktmfix)
        B.p_mlstm(True, qkT, ktm, zvo, gates, Cst, b_gmbc, catT, fix=(qkfix, ktmfix))
        B.p_moba_attn(qrot, KTa_o, Vaug_o, kmT_o, KTa_p, Vaug_p, kmT_p, selb, catT)
        B.p_outproj(catT, b_wout, LC[:, 2, :], xT, xT_src=xT_in)
        B.p_router(xT, LC[:, 3, :], LC[:, 4, :], b_wr, b_brbc, h2T, PT)
        B.p_moe(h2T, PT, b_wg, b_wu, b_wd, b_bgT, b_buT, b_bd, LC[:, 5, :], xT)
    if not last:
        cT = I("a_cT", [128, 8]); w_ada = I("a_w_ada", [D, 6 * D]); b_adaT = I("a_b_adaT", [128, 48])
        a_g1n = I("a_g1n", [128, 8]); a_g2n = I("a_g2n", [128, 8]); w_in = I("a_w_in", [D, N_IN])
        a_cwT = I("a_cwT", [128, 8, 4]); a_cbT = I("a_cbT", [128, 8]); a_bgate = I("a_bgate", [1, 8])
        ztm = T("ztm", [NT, N_TM]); zqk = T("zqk", [D, NT])
        zvo_o = O("zvo_out", [NT, 1024]); qkT_o = O("qkT_out", [D, NT], BF16); ktm_o = O("ktm_out", [NT, 512], BF16)
        qrot_o = O("qrot_out", [NT, 512]); gat_o = O("gates_out", [128, NTILE, 8])
        KTa = O("KTa_out", [8, 128, NT], BF16); Vaug = O("Vaug_out", [8, NT, 65], BF16); kmT = O("kmT_out", [64, 8, 16])
        st_o = O("st_out", [128, 4, 129]); halo_o = O("halo_out", [D, 3]); zq0_o = O("zq0_out", [D, 128]); modT_o = O("modT_out", [128, 48])
        B.p_mods(cT, w_ada, b_adaT, modT)
        B.dma('sp', modT_o, modT[:], ['modT'], ['modT_o'])
        B.p_layer_consts(modT, a_g1n, a_g2n, LC)
        B.p_inproj(0, xT, w_in, LC[:, 0, :], LC[:, 1, :], ztm, zqk, zvo_o)
        B.dma('sp', halo_o, zqk[:, NT - 3:NT], [], ['halo_o'])
        B.dma('sp', zq0_o, zqk[:, 0:128], [], ['zq0_o'])
        B.p_mlstm_prep(zqk, ztm, a_cwT, a_cbT, a_bgate, qkT_o, ktm_o, gates, cw, cb, ncb)
        B.dma('sp', gat_o, gates[:], ['gates'], ['gat_o'])
        B.memset('dve', Cst[:], 0.0, ['Cst'])
        B.p_mlstm(False, qkT_o, ktm_o, ztm, gates, Cst)
        B.dma('sp', st_o, Cst[:], ['Cst'], ['st_o'])
        B.p_moba_prep(ztm, cs, sn, qrot_o, KTa, Vaug, kmT)
    else:
        gfT = I("gfT", [128, 8]); out_d = O("out", [NT, D])
        B.p_final(xT, gfT, out_d)
    B.finish()
    return B


MOE_BLOCKS = 64
MOE_BS = 512
MOE_SPARSE_CAP = 0


def build_fused(ncores=8):
    B = Builder()
    I = lambda name, shape, dt=F32: B.dram(name, shape, dt, kind="ExternalInput")
    O = lambda name, shape, dt=F32: B.dram(name, shape, dt, kind="ExternalOutput")
    T = lambda name, shape, dt=F32: B.dram(name, shape, dt, kind="Internal")
    B.make_consts()
    modT = B.sbp("modT", [128, 48]); LC = B.sbp("LC", [128, 6, 8])
    gates = B.sbp("gates", [128, NTILE, 8])
    cw = B.sbp("cw", [128, 8, 4]); cb = B.sbp("cb", [128, 8]); ncb = B.sbp("ncb", [128, 8])
    Cst = B.sbp("Cst", [128, 4, 129])
    cs = B.sbp("cs", [128, NTILE, 8]); sn = B.sbp("sn", [128, NTILE, 8])
    CAPS = MOE_SPARSE_CAP
    NBLK = MOE_BLOCKS
    if NBLK:
        PT = None
        slots_all = B.sbp("slots_all", [128, NTILE, 4], I32); widx = B.sbp("widx", [128, NBLK, 8], I32); besb = B.sbp("besb", [128, NBLK])
        Xg = T("Xg", [NBLK * MOE_BS, 1026], BF16); Yg = T("Yg", [NBLK * MOE_BS, 1024]); h2rows = T("h2rows", [NT, 1024], BF16)
    elif CAPS:
        PT = None
        slots_all = B.sbp("slots_all", [128, NTILE, 4], I32)
        Xg = T("Xg", [32 * CAPS, 1026], BF16); Yg = T("Yg", [32 * CAPS, 1024])
    else:
        PT = B.sbp("PT", [32, NT])
    selc = B.sbp("selc", [128, 1]); selb = B.sbp("selb", [128, 1])
    pos_d = I("pos", [128, NTILE], I32)
    sel_d = I("sel", [128, 1])
    x_d = I("x", [NT, D]); cT = I("cT", [128, 8]); gfT = I("gfT", [128, 8]); out_d = O("out", [NT, D])
    W = []
    for l in range(2):
        p = "l%d_" % l
        W.append(dict(
            w_ada=I(p + "w_ada", [D, 6 * D]), b_adaT=I(p + "b_adaT", [128, 48]), g1n=I(p + "g1n", [128, 8]), g2n=I(p + "g2n", [128, 8]),
            w_in=I(p + "w_in", [D, N_IN]), cwT=I(p + "cwT", [128, 8, 4]), cbT=I(p + "cbT", [128, 8]), bgate=I(p + "bgate", [1, 8]),
            gmbc=I(p + "gmbc", [1, 512]), wout=I(p + "wout", [D, D]), wr=I(p + "wr", [D, 32]), brbc=I(p + "brbc", [1, 32]),
            wg=I(p + "wg", [32, D, D]), wu=I(p + "wu", [32, D, D]), wd=I(p + "wd", [32, D, D]),
            bgT=I(p + "bgT", [128, 32, 8]), buT=I(p + "buT", [128, 32, 8]), bd=I(p + "bd", [32, D])))
    xT = T("xT", [D, NT])
    ztm = T("ztm", [NT, N_TM]); zqk = T("zqk", [D, NT]); qkT = T("qkT", [D, NT], BF16); ktm = T("ktm", [NT, 512], BF16)
    qrot = T("qrot", [NT, 512]); catT = T("catT", [D, NT], BF16); h2T = T("h2T", [D, NT], BF16)
    qkfix = T("qkfix", [D, 128], BF16); ktmfix = T("ktmfix", [128, 512], BF16)
    ex_K = T("ex_K", [8 * 128, NT], BF16); ga_K = T("ga_K", [2 * 8 * 128, NT], BF16)
    VR = 8 * NT * 65 // 512
    ex_V = T("ex_V", [VR, 512], BF16); ga_V = T("ga_V", [2 * VR, 512], BF16)
    ex_S = T("ex_S", [128, 668]); ga_S = T("ga_S", [256, 668])
    KTa_o = ex_K.rearrange("(h r) t -> h r t", r=128)
    Vaug_o = ex_V.rearrange("r c -> (r c)").rearrange("(h n c) -> h n c", h=8, c=65)
    KTa_p = ga_K.rearrange("(h s r) t -> h s r t", h=8, s=2)[:, 0]
    Vaug_p = ga_V.rearrange("(h s r) c -> h s (r c)", h=8, s=2)[:, 0].rearrange("h (n c) -> h n c", c=65)
    gaK2 = ga_K.rearrange("(h s r) t -> h (s r) t", h=8, s=2)
    gaV2 = ga_V.rearrange("(h s r) c -> h (s r) c", h=8, s=2)
    exV2 = ex_V.rearrange("(h r) c -> h r c", h=8)
    ex_list = [(ex_K[h * 128:(h + 1) * 128, :], gaK2[h]) for h in range(8)] + [(exV2[h], gaV2[h]) for h in range(8)] + [(ex_S, ga_S)]
    st_o = ex_S[:, 0:516].rearrange("p (h c) -> p h c", c=129)
    kmT_o = ex_S[0:64, 516:644].rearrange("p (h n) -> p h n", n=16)
    halo_o = ex_S[:, 644:668]
    st_p = ga_S[0:128, 0:516].rearrange("p (h c) -> p h c", c=129)
    kmT_p = ga_S[0:64, 516:644].rearrange("p (h n) -> p h n", n=16)
    halo_p = ga_S[0:128, 644:668]
    groups = [[2 * i, 2 * i + 1] for i in range(ncores // 2)]

    B.dma('sp', selc[:], sel_d, [], ['selb'])
    B.ts('dve', selb[:], selc[:], -1.0, ALU.add, ['selb'], ['selb'], s2=1e30, op1=ALU.mult)
    B.p_rotary_tables(pos_d, cs, sn)
    B.p_x_to_xT(x_d, xT)
    for l in range(2):
        w = W[l]
        B.p_mods(cT, w['w_ada'], w['b_adaT'], modT)
        B.p_layer_consts(modT, w['g1n'], w['g2n'], LC)
        B.p_inproj(l, xT, w['w_in'], LC[:, 0, :], LC[:, 1, :], ztm, zqk)
        B.dma('sp', halo_o.rearrange("p (cc t) -> p cc t", t=3), zqk[:, NT - 3:NT].rearrange("(cc p) t -> p cc t", p=128), [], ['halo_o'])
        B.p_mlstm_prep(zqk, ztm, w['cwT'], w['cbT'], w['bgate'], qkT, ktm, gates, cw, cb, ncb)
        B.memset('dve', Cst[:], 0.0, ['Cst'])
        B.p_mlstm(False, qkT, ktm, ztm, gates, Cst)
        B.dma('sp', st_o, Cst[:], ['Cst'], ['st_o'])
        B.p_moba_prep(ztm, cs, sn, qrot, KTa_o, Vaug_o, kmT_o)
        B.S.barrier()
        for src_, dst_ in ex_list:
            B.S.collective(lambda e, src_=src_, dst_=dst_: e.collective_compute("AllGather", ALU.bypass, replica_groups=groups,
                                                                              ins=[src_.opt()], outs=[dst_.opt()]), 1)
        B.S.barrier()
        B.dma('sp', Cst[:], st_p, [], ['Cst'])
        B.ts('dve', Cst[:], Cst[:], selc[:, 0:1], ALU.mult, ['Cst', 'selb'], ['Cst'])
        B.S.barrier()
        B.p_conv_fix(zqk[:, 0:128], halo_p, selc, w['cwT'], w['cbT'], cw, cb, ncb, qkfix, ktmfix, halo_packed=True)
        B.p_mlstm(True, qkT, ktm, ztm, gates, Cst, w['gmbc'], catT, fix=(qkfix, ktmfix))
        B.p_moba_attn(qrot, KTa_o, Vaug_o, kmT_o, KTa_p, Vaug_p, kmT_p, selb, catT)
        B.p_outproj(catT, w['wout'], LC[:, 2, :], xT)
        if NBLK:
            B.p_router_blocks(xT, LC[:, 3, :], LC[:, 4, :], w['wr'], w['brbc'], Xg, h2rows, slots_all, widx, besb, NBLK, MOE_BS)
            B.p_moe_blocks(Xg, Yg, slots_all, widx, besb, w['wg'], w['wu'], w['wd'], w['bgT'], w['buT'], w['bd'], LC[:, 5, :], xT, NBLK, MOE_BS)
        elif CAPS:
            B.p_router_sparse(xT, LC[:, 3, :], LC[:, 4, :], w['wr'], w['brbc'], Xg, slots_all, CAPS, None)
            B.p_moe_sparse(Xg, Yg, slots_all, w['wg'], w['wu'], w['wd'], w['bgT'], w['buT'], w['bd'], LC[:, 5, :], xT, CAPS)
        else:
            B.p_router(xT, LC[:, 3, :], LC[:, 4, :], w['wr'], w['brbc'], h2T, PT)
            B.p_moe(h2T, PT, w['wg'], w['wu'], w['wd'], w['bgT'], w['buT'], w['bd'], LC[:, 5, :], xT)
    B.p_final(xT, gfT, out_d)
    B.finish()
    return B


def _fused_inputs(inp, c):
    b, s = c // 2, c & 1
    m = {"pos": np.ascontiguousarray(np.asarray(inp['positions'][b, s * NT:(s + 1) * NT], np.int32).reshape(NTILE, 128).T),
         "sel": np.full((128, 1), float(s), np.float32), "x": np.ascontiguousarray(inp['x'][b, s * NT:(s + 1) * NT]),
         "cT": colT(inp['c'][b]), "gfT": colT(inp['final_norm_g'])}
    for l in range(2):
        p = "l%d_" % l
        a = _a_weights(inp, l, b)
        bw = _b_weights(inp, l)
        m.update({p + "w_ada": a["a_w_ada"], p + "b_adaT": a["a_b_adaT"], p + "g1n": a["a_g1n"], p + "g2n": a["a_g2n"], p + "w_in": a["a_w_in"],
                  p + "cwT": a["a_cwT"], p + "cbT": a["a_cbT"], p + "bgate": a["a_bgate"], p + "gmbc": bw["b_gmbc"], p + "wout": bw["b_wout"],
                  p + "wr": bw["b_wr"], p + "brbc": bw["b_brbc"], p + "wg": bw["b_wg"], p + "wu": bw["b_wu"], p + "wd": bw["b_wd"],
                  p + "bgT": bw["b_bgT"], p + "buT": bw["b_buT"], p + "bd": bw["b_bd"]})
    return m


def _fused_pipeline(inp, NCORE=8):
    inp = {k: np.asarray(v) for k, v in inp.items()}
    if 'fused' not in _PROGS:
        _PROGS['fused'] = build_fused(NCORE)
    prog = _PROGS['fused']
    cores = list(range(NCORE))
    shared = {}
    ims = []
    for c in cores:
        m = _fused_inputs(inp, c)
        for k in list(m.keys()):
            if k[0] == 'l' and k[2] == '_':
                m[k] = shared.setdefault(k, m[k])
        ims.append(m)
    r = _run(prog, ims, cores)
    out = np.zeros((inp['x'].shape[0], 2 * NT, D), np.float32)
    for c in cores:
        out[c // 2, (c & 1) * NT:((c & 1) + 1) * NT] = r[c]["out"]
    return out


_PROGS = {}


def get_prog(first, last):
    k = (first, last)
    if k not in _PROGS:
        _PROGS[k] = build_launch(first, last)
    return _PROGS[k]


def _bT(b):
    return np.ascontiguousarray(np.asarray(b, np.float32).reshape(32, 8, 128).transpose(2, 0, 1))


def _a_weights(inp, l, b):
    return {"a_cT": colT(inp['c'][b]), "a_w_ada": np.ascontiguousarray(inp['w_ada'][l]), "a_b_adaT": colT(inp['b_ada'][l]),
            "a_g1n": colT(inp['norm1_g'][l]), "a_g2n": colT(inp['norm2_g'][l]), "a_w_in": np.ascontiguousarray(inp['w_in'][l]),
            "a_cwT": np.ascontiguousarray(np.asarray(inp['conv_w'][l], np.float32).T.reshape(8, 128, 4).transpose(1, 0, 2)),
            "a_cbT": colT(inp['conv_b'][l]),
            "a_bgate": np.concatenate([inp['b_igate'][l], inp['b_fgate'][l]])[None, :].astype(np.float32)}


def _b_weights(inp, l):
    return {"b_g1n": colT(inp['norm1_g'][l]), "b_g2n": colT(inp['norm2_g'][l]),
            "b_cwT": np.ascontiguousarray(np.asarray(inp['conv_w'][l], np.float32).T.reshape(8, 128, 4).transpose(1, 0, 2)),
            "b_cbT": colT(inp['conv_b'][l]), "b_gmbc": np.asarray(inp['mlstm_norm_g'][l], np.float32)[None, :],
            "b_wout": np.ascontiguousarray(inp['w_out'][l]), "b_wr": np.ascontiguousarray(inp['w_router'][l]),
            "b_brbc": np.asarray(inp['b_router'][l], np.float32)[None, :],
            "b_wg": np.ascontiguousarray(inp['w_gate'][l]), "b_wu": np.ascontiguousarray(inp['w_up'][l]), "b_wd": np.ascontiguousarray(inp['w_down'][l]),
            "b_bgT": _bT(inp['b_gate'][l]), "b_buT": _bT(inp['b_up'][l]), "b_bd": np.ascontiguousarray(inp['b_down'][l])}


def _handoff(prev, c):
    o = prev[c]
    p = prev[c ^ 1]
    return {"xT_in": o["xT_out"], "zvo_in": o["zvo_out"], "qkT_in": o["qkT_out"], "ktm_in": o["ktm_out"], "qrot_in": o["qrot_out"],
            "gates_in": o["gates_out"], "KTa_o": o["KTa_out"], "Vaug_o": o["Vaug_out"], "kmT_o": o["kmT_out"],
            "KTa_p": p["KTa_out"], "Vaug_p": p["Vaug_out"], "kmT_p": p["kmT_out"], "st_p": p["st_out"], "halo_p": p["halo_out"],
            "zq0_in": o["zq0_out"], "modT_in": o["modT_out"], "sel": np.full((128, 1), float(c & 1), np.float32)}


def kernel(**inp):
    return _fused_pipeline(inp, 8)


def _run(prog, ims, cores):
    import time
    t0 = time.time()
    res = run_bass_kernel_spmd(prog.nc, ims, core_ids=cores).results
    print("[kernel] launch done in %.1fs" % (time.time() - t0), flush=True)
    return res


def _pipeline(inp, NCORE):
    inp = {k: np.asarray(v) for k, v in inp.items()}
    x = inp['x']
    Bn = x.shape[0]
    pos = [np.ascontiguousarray(np.asarray(inp['positions'][c // 2, (c & 1) * NT:((c & 1) + 1) * NT], np.int32).reshape(NTILE, 128).T)
           for c in range(NCORE)]
    cores = list(range(NCORE))
    p1 = get_prog(True, False)
    ims = []
    for c in cores:
        b, s = c // 2, c & 1
        m = {"pos": pos[c], "x": np.ascontiguousarray(x[b, s * NT:(s + 1) * NT])}
        m.update(_a_weights(inp, 0, b))
        ims.append(m)
    r = _run(p1, ims, cores)
    p2 = get_prog(False, False)
    ims = []
    for c in cores:
        m = {"pos": pos[c]}
        m.update(_handoff(r, c))
        m.update(_b_weights(inp, 0))
        m.update(_a_weights(inp, 1, c // 2))
        ims.append(m)
    r = _run(p2, ims, cores)
    p3 = get_prog(False, True)
    ims = []
    for c in cores:
        m = {"pos": pos[c], "gfT": colT(inp['final_norm_g'])}
        m.update(_handoff(r, c))
        m.update(_b_weights(inp, 1))
        ims.append(m)
    r = _run(p3, ims, cores)
    out = np.zeros((Bn, 2 * NT, D), np.float32)
    for c in cores:
        out[c // 2, (c & 1) * NT:((c & 1) + 1) * NT] = r[c]["out"]
    return out


def colT(v):
    v = np.asarray(v, dtype=np.float32)
    return np.ascontiguousarray(v.reshape(-1, 128).T)
```

```python
import numpy as np
from contextlib import ExitStack, contextmanager
import concourse.bass as bass
import concourse.mybir as mybir
from concourse.bass_utils import run_bass_kernel_spmd

F32 = mybir.dt.float32
BF16 = mybir.dt.bfloat16
I32 = mybir.dt.int32
U32 = mybir.dt.uint32
AF = mybir.ActivationFunctionType
ALU = mybir.AluOpType
AX = mybir.AxisListType

COMPUTE = ('pe', 'act', 'dve', 'pool')
ENGS = ('pe', 'act', 'dve', 'pool', 'sp')

D = 1024
NT = 4096
NG = NT // 512
NTILE = NT // 128
N_IN = 3592
N_TM = 2568
EPS = 1e-5


class Sched:
    def __init__(self, nc, n_dma_sems=32):
        self.nc = nc
        self.ops = {e: [] for e in ENGS}
        self.count = {e: 0 for e in COMPUTE}
        self.last_w = {}
        self.readers = {}
        self.waited = {e: {} for e in ENGS}
        self.n_dma_sems = n_dma_sems
        self.dma_tot = [0] * n_dma_sems
        self.dma_rr = 0

    def _need(self, eng, ref, waits):
        sem, val, src = ref
        if self.waited[eng].get(sem, 0) >= val:
            return
        self.waited[eng][sem] = val
        waits.append((sem, val))

    def _deps(self, eng, reads, writes, is_dma):
        waits = []
        for k in reads:
            w = self.last_w.get(k)
            if w is not None:
                if not (w[2] == eng and not is_dma and w[0] in COMPUTE and eng == 'pe'):
                    self._need(eng, w, waits)
            if isinstance(k, tuple) and k[0] == 'ps':
                for r in self.readers.get(k, ()):
                    if r[2] != eng:
                        self._need(eng, r, waits)
        for k in writes:
            w = self.last_w.get(k)
            if w is not None:
                if not (w[2] == eng and not is_dma and w[0] in COMPUTE):
                    self._need(eng, w, waits)
            for r in self.readers.get(k, ()):
                if r[2] == eng and not is_dma and r[0] in COMPUTE:
                    continue
                self._need(eng, r, waits)
        return waits

    def _record(self, ref, reads, writes):
        for k in reads:
            self.readers.setdefault(k, []).append(ref)
        for k in writes:
            self.last_w[k] = ref
            self.readers[k] = []

    def op(self, eng, fn, reads=(), writes=()):
        waits = self._deps(eng, reads, writes, False)
        self.count[eng] += 1
        ref = (eng, self.count[eng], eng)
        self.ops[eng].append((waits, fn, (eng, 1)))
        self._record(ref, reads, writes)
        return ref

    def dma(self, eng, fn, reads=(), writes=()):
        waits = self._deps(eng, reads, writes, True)
        i = self.dma_rr
        self.dma_rr = (self.dma_rr + 1) % self.n_dma_sems
        sem = 'dma%d' % i
        if self.dma_tot[i] > 0:
            self._need(eng, (sem, self.dma_tot[i], 'dmaq'), waits)
        self.dma_tot[i] += 16
        ref = (sem, self.dma_tot[i], 'dmaq')
        self.ops[eng].append((waits, fn, (sem, 16)))
        self._record(ref, reads, writes)
        return ref

    def collective(self, fn, inc=16):
        self.cc_tot = getattr(self, 'cc_tot', 0) + inc
        self.ops['pool'].append(([], fn, ('cc', inc)))
        for e in ENGS:
            self.ops[e].append(([('cc', self.cc_tot)], None, None))

    def barrier(self):
        for e in ENGS:
            waits = []
            for c in COMPUTE:
                if self.count[c] > 0:
                    self._need(e, (c, self.count[c], c), waits)
            for i in range(self.n_dma_sems):
                if self.dma_tot[i] > 0:
                    self._need(e, ('dma%d' % i, self.dma_tot[i], 'dmaq'), waits)
            if waits:
                self.ops[e].append((waits, None, None))
        self.last_w = {}
        self.readers = {}

    def emit(self):
        nc = self.nc
        with ExitStack() as st:
            sems = {}
            for e in COMPUTE:
                sems[e] = st.enter_context(nc.semaphore('prog_' + e))
            for i in range(self.n_dma_sems):
                sems['dma%d' % i] = st.enter_context(nc.semaphore('dmas%d' % i))
            sems['cc'] = st.enter_context(nc.semaphore('cc_sem'))
            block = st.enter_context(nc.Block())

            def run(eng_name):
                def body(e):
                    for waits, fn, inc in self.ops[eng_name]:
                        for s, v in waits:
                            e.wait_ge(sems[s], v)
                        if fn is not None:
                            ins = fn(e)
                            ins.then_inc(sems[inc[0]], inc[1])
                return body

            block.tensor(run('pe'))
            block.scalar(run('act'))
            block.vector(run('dve'))
            block.gpsimd(run('pool'))
            block.sync(run('sp'))


class Phase:
    def __init__(self, B):
        self.B = B
        self.st = ExitStack()

    def sb(self, name, shape, dt=F32):
        self.B.uid += 1
        return self.st.enter_context(self.B.nc.sbuf_tensor("%s_%d" % (name, self.B.uid), list(shape), dt))


class Builder:
    def __init__(self):
        self.nc = bass.Bass("TRN2", target_bir_lowering=False)
        self.S = Sched(self.nc)
        self.st = ExitStack()
        self.uid = 0
        self.scratch_kind = "Internal"
        self.ps = [self.st.enter_context(self.nc.psum_tensor("psb%d" % i, [128, 512], F32)) for i in range(8)]
        self.PS = [('ps', i) for i in range(8)]
        self.psb = [p[:].bitcast(BF16) for p in self.ps]

    def dram(self, name, shape, dt=F32, kind="Internal"):
        return self.nc.dram_tensor(name, list(shape), dt, kind=kind).ap()

    def sbp(self, name, shape, dt=F32):
        return self.st.enter_context(self.nc.sbuf_tensor(name, list(shape), dt))

    @contextmanager
    def phase(self):
        ph = Phase(self)
        try:
            yield ph
        finally:
            self.S.barrier()
            ph.st.close()

    def bc_reg(self, e, val):
        if getattr(self, '_bc', None) is None or self._bc[0] != val:
            self._bc = (val, e.to_reg(val))
        return self._bc[1]

    def mm(self, out, lhsT, rhs, start, stop, r, w):
        return self.S.op('pe', lambda e: e.matmul(out, lhsT=lhsT, rhs=rhs, start=start, stop=stop), r, w)

    def tr(self, out, in_, ident, r, w):
        return self.S.op('pe', lambda e: e.transpose(out=out, in_=in_, identity=ident), r, w)

    def act(self, out, in_, func, r, w, bias=0.0, scale=1.0, accum_out=None):
        if accum_out is None:
            return self.S.op('act', lambda e: e.activation(out=out, in_=in_, func=func, bias=bias, scale=scale), r, w)
        return self.S.op('act', lambda e: e.activation(out=out, in_=in_, func=func, bias=bias, scale=scale, accum_out=accum_out), r, w)

    def tt(self, eng, out, in0, in1, op, r, w):
        return self.S.op(eng, lambda e: e.tensor_tensor(out=out, in0=in0, in1=in1, op=op), r, w)

    def ts(self, eng, out, in0, s1, op0, r, w, s2=None, op1=None, accum_out=None):
        if op1 is None:
            return self.S.op(eng, lambda e: e.tensor_scalar(out=out, in0=in0, scalar1=s1, scalar2=None, op0=op0), r, w)
        if accum_out is None:
            return self.S.op(eng, lambda e: e.tensor_scalar(out=out, in0=in0, scalar1=s1, scalar2=s2, op0=op0, op1=op1), r, w)
        return self.S.op(eng, lambda e: e.tensor_scalar(out=out, in0=in0, scalar1=s1, scalar2=s2, op0=op0, op1=op1, accum_out=accum_out), r, w)

    def stt(self, out, in0, scalar, in1, op0, op1, r, w):
        return self.S.op('dve', lambda e: e.scalar_tensor_tensor(out=out, in0=in0, scalar=scalar, in1=in1, op0=op0, op1=op1), r, w)

    def cp(self, eng, out, in_, r, w):
        if eng == 'act':
            return self.S.op('act', lambda e: e.activation(out=out, in_=in_, func=AF.Copy), r, w)
        return self.S.op(eng, lambda e: e.tensor_copy(out=out, in_=in_), r, w)

    def memset(self, eng, ap, val, w):
        return self.S.op(eng, lambda e: e.memset(ap, val), (), w)

    def dma(self, q, out, in_, r, w):
        return self.S.dma(q, lambda e: e.dma_start(out=out, in_=in_), r, w)

    def finish(self):
        self.S.barrier()
        self.S.emit()
        self.st.close()
        return self.nc

    def make_consts(self):
        self.ones_f = self.sbp("ones_f", [128, 128], F32)
        self.ident_f = self.sbp("ident_f", [128, 128], F32)
        self.ident_b = self.sbp("ident_b", [128, 128], BF16)
        self.memset('pool', self.ones_f[:], 1.0, ['ones_f'])
        self.S.op('pool', lambda e: e.affine_select(out=self.ident_f[:], in_=self.ones_f[:], pattern=[[1, 128]],
                                                    compare_op=ALU.is_equal, fill=0.0, base=0, channel_multiplier=-1),
                  ['ones_f'], ['ident_f'])
        self.cp('dve', self.ident_b[:], self.ident_f[:], ['ident_f'], ['ident_b'])
        self.c7 = self.sbp("c7", [128, 1], F32)
        self.memset('pool', self.c7[:], 7.0, ['c7'])
        self.S.barrier()

    def p_x_to_xT(self, x_d, xT_d):
        ps, PS = self.ps, self.PS
        xT_v = xT_d.rearrange("(kc p) t -> p kc t", p=128)
        with self.phase() as ph:
            xt = [ph.sb("xt", [128, 4, D]) for _ in range(2)]
            xg = [ph.sb("xg", [128, 8, 512]) for _ in range(2)]
            for g in range(NG):
                b = g % 2
                self.dma('sp', xt[b][:], x_d[g * 512:(g + 1) * 512, :].rearrange("(j p) d -> p j d", p=128), [], [('xt', b)])
                for kc in range(8):
                    bk = kc % 4
                    for j in range(4):
                        self.tr(ps[bk][:, j * 128:(j + 1) * 128], xt[b][:, j, kc * 128:(kc + 1) * 128], self.ident_f[:],
                                [('xt', b)], [PS[bk]])
                    self.cp('act' if kc % 2 else 'dve', xg[b][:, kc, :], ps[bk][:], [PS[bk]], [('xg', b)])
                self.dma('sp', xT_v[:, :, g * 512:(g + 1) * 512], xg[b][:], [('xg', b)], [('xT', g)])

    def p_mods(self, cT_d, w_ada_d, b_adaT_d, modT):
        ps, PS = self.ps, self.PS
        wv = w_ada_d.rearrange("(kc p) n -> p kc n", p=128)
        with self.phase() as ph:
            cc = ph.sb("cc", [128, 8]); ex = ph.sb("ex", [128, 8]); cond = ph.sb("cond", [128, 8])
            bT = ph.sb("bT", [128, 48])
            wa = [ph.sb("wa", [128, 8, 512]) for _ in range(2)]
            self.dma('sp', cc[:], cT_d, [], ['cc'])
            self.dma('sp', bT[:], b_adaT_d, [], ['bT'])
            self.act(ex[:], cc[:], AF.Exp, ['cc'], ['ex'], scale=-1.0)
            self.ts('dve', ex[:], ex[:], 1.0, ALU.add, ['ex'], ['ex'])
            self.S.op('dve', lambda e: e.reciprocal(out=ex[:], in_=ex[:]), ['ex'], ['ex'])
            self.tt('dve', cond[:], cc[:], ex[:], ALU.mult, ['cc', 'ex'], ['cond'])
            for blk in range(12):
                b = blk % 2
                self.dma('sp', wa[b][:], wv[:, :, blk * 512:(blk + 1) * 512], [], [('wa', b)])
                for jj in range(4):
                    j = blk * 4 + jj
                    for kc in range(8):
                        self.mm(ps[0][:, j:j + 1], wa[b][:, kc, jj * 128:(jj + 1) * 128], cond[:, kc:kc + 1],
                                kc == 0, kc == 7, [('wa', b), 'cond'], [PS[0]])
            self.tt('dve', modT[:], ps[0][:, 0:48], bT[:], ALU.add, [PS[0], 'bT'], ['modT'])

    def p_load_w_bf16(self, ph, w_d, ncols, dst, key, col0=0):
        wv = w_d.rearrange("(kc p) n -> p kc n", p=128)
        with self.phase() as ph2:
            stg = [ph2.sb("wstg", [128, 8, 512]) for _ in range(2)]
            nblk = (ncols + 511) // 512
            for blk in range(nblk):
                b = blk % 2
                c0 = blk * 512
                c1 = min(ncols, c0 + 512)
                self.dma('sp', stg[b][:, :, 0:c1 - c0], wv[:, :, col0 + c0:col0 + c1], [], [('wstg', key, b)])
                for kc in range(8):
                    self.cp('act' if kc % 2 else 'pool', dst[:, kc, c0:c1], stg[b][:, kc, 0:c1 - c0], [('wstg', key, b)], [key])

    def p_norm_group(self, ph, g, xT_d, Acol, Bcol, hT, hkey, bufs):
        ps, PS = self.ps, self.PS
        xg, sq, rstd = bufs
        xT_v = xT_d.rearrange("(kc p) t -> p kc t", p=128)
        self.dma('sp', xg[:], xT_v[:, :, g * 512:(g + 1) * 512], [('xT', g)], ['n_xg'])
        self.act(sq[:], xg[:], AF.Square, ['n_xg'], [('n_sq', kc) for kc in range(8)])
        for kc in range(8):
            self.mm(ps[7][:], self.ones_f[:], sq[:, kc, :], kc == 0, kc == 7, [('n_sq', kc)], [PS[7]])
        self.act(rstd[:], ps[7][:], AF.Ln, [PS[7]], ['n_rstd'], bias=EPS, scale=1.0 / D)
        self.act(rstd[:], rstd[:], AF.Exp, ['n_rstd'], ['n_rstd'], scale=-0.5)
        for kc in range(8):
            self.stt(sq[:, kc, :], xg[:, kc, :], Acol[:, kc:kc + 1], rstd[:], ALU.mult, ALU.mult, ['n_xg', 'n_rstd', 'lconst'], [('n_sq', kc)])
            self.act(hT[:, kc, :], sq[:, kc, :], AF.Identity, [('n_sq', kc), 'lconst'], [hkey], bias=Bcol[:, kc:kc + 1])

    def p_inproj(self, l, xT_d, w_in_d, A1, B1, ztm_d, zqk_d, zvo_d=None):
        ps, PS = self.ps, self.PS
        zqk_v = zqk_d.rearrange("(cc p) t -> p cc t", p=128)
        with self.phase() as ph:
            w_fm = ph.sb("w_fm", [128, 8, 1024], BF16)
            w_tm = ph.sb("w_tm", [128, 8, N_TM], BF16)
            self.p_load_w_bf16(ph, w_in_d, 1024, w_fm, 'w_fm', 0)
            self.p_load_w_bf16(ph, w_in_d, N_TM, w_tm, 'w_tm', 1024)
            xg = ph.sb("n_xg", [128, 8, 512]); sq = ph.sb("n_sq", [128, 8, 512])
            rstd = ph.sb("n_rstd", [128, 512])
            hT = [ph.sb("hT", [128, 8, 512], BF16) for _ in range(2)]
            zt = [ph.sb("zt", [128, N_TM]) for _ in range(2)]
            zf = [ph.sb("zf", [128, 8, 512]) for _ in range(2)]
            NB = 6
            CB = N_TM // NB
            import os
            DBG = int(os.environ.get("DBG", "9"))
            for g in range(NG if DBG > 0 else 0):
                hb = g % 2
                self.p_norm_group(ph, g, xT_d, A1, B1, hT[hb], ('hT', hb), (xg, sq, rstd))
                for cc in range(8 if DBG > 1 else 0):
                    bk = cc % 2
                    for kc in range(8):
                        self.mm(ps[bk][:], w_fm[:, kc, cc * 128:(cc + 1) * 128], hT[hb][:, kc, :], kc == 0, kc == 7,
                                ['w_fm', ('hT', hb)], [PS[bk]])
                    self.cp('act' if cc % 2 else 'dve', zf[hb][:, cc, :], ps[bk][:], [PS[bk]], [('zf', hb)])
                self.dma('sp', zqk_v[:, :, g * 512:(g + 1) * 512], zf[hb][:], [('zf', hb)], [('zqk', g)])
                for j in range(4 if DBG > 2 else 0):
                    ti = g * 4 + j
                    zb = ti % 2
                    for nb in range(NB):
                        bk = 2 + (nb % 4)
                        for kc in range(8):
                            self.mm(ps[bk][:, 0:CB], hT[hb][:, kc, j * 128:(j + 1) * 128], w_tm[:, kc, nb * CB:(nb + 1) * CB],
                                    kc == 0, kc == 7, ['w_tm', ('hT', hb)], [PS[bk]])
                        self.cp('act' if nb % 2 else 'dve', zt[zb][:, nb * CB:(nb + 1) * CB], ps[bk][:, 0:CB], [PS[bk]], [('zt', zb)])
                    self.dma('sp', ztm_d[ti * 128:(ti + 1) * 128, :], zt[zb][:], [('zt', zb)], [('ztm', ti)])
                    if zvo_d is not None:
                        self.dma('sp', zvo_d[ti * 128:(ti + 1) * 128, :], zt[zb][:, 0:1024], [('zt', zb)], [('zvo', ti)])


    def p_layer_consts(self, modT, g1n_d, g2n_d, LC):
        with self.phase() as ph:
            gn = ph.sb("gn", [128, 2, 8])
            self.dma('sp', gn[:, 0, :], g1n_d, [], ['gn'])
            self.dma('sp', gn[:, 1, :], g2n_d, [], ['gn'])
            for i in range(2):
                o = 24 * i
                self.stt(LC[:, 3 * i + 0, :], modT[:, o + 8:o + 16], 1.0, gn[:, i, :], ALU.add, ALU.mult, ['modT', 'gn'], ['lconst'])
                self.cp('dve', LC[:, 3 * i + 1, :], modT[:, o:o + 8], ['modT'], ['lconst'])
                self.cp('dve', LC[:, 3 * i + 2, :], modT[:, o + 16:o + 24], ['modT'], ['lconst'])


    def conv_chunk(self, zin, n, cw, cb, ncb, cc, acc, ee, out, rk, wk):
        self.ts('dve', acc[:, 0:n], zin[:, 3:3 + n], cw[:, cc, 3:4], ALU.mult, rk + ['cw'], ['c_acc'])
        for j in (2, 1, 0):
            self.stt(acc[:, 0:n], zin[:, j:j + n], cw[:, cc, j:j + 1], acc[:, 0:n], ALU.mult, ALU.add, rk + ['cw', 'c_acc'], ['c_acc'])
        self.act(ee[:, 0:n], acc[:, 0:n], AF.Exp, ['c_acc', 'cw'], ['c_ee'], bias=ncb[:, cc:cc + 1], scale=-1.0)
        sk = 1.0 if cc < 4 else 128.0 ** 0.5
        self.ts('pool', ee[:, 0:n], ee[:, 0:n], 1.0, ALU.add, ['c_ee'], ['c_ee'], s2=sk, op1=ALU.mult)
        self.S.op('dve', lambda e: e.reciprocal(out=ee[:, 0:n], in_=ee[:, 0:n]), ['c_ee'], ['c_ee'])
        self.stt(out, acc[:, 0:n], cb[:, cc:cc + 1], ee[:, 0:n], ALU.add, ALU.mult, ['c_acc', 'c_ee', 'cw'], wk)

    def p_mlstm_prep(self, zqk_d, ztm_d, cwT_d, cbT_d, bgate_d, qkT_d, ktm_d, gates, cw, cb, ncb):
        ps, PS = self.ps, self.PS
        ktm_v = ktm_d.rearrange("(i p) c -> p i c", p=128)
        with self.phase() as ph:
            self.dma('sp', cw[:], cwT_d, [], ['cw'])
            self.dma('sp', cb[:], cbT_d, [], ['cw'])
            self.ts('dve', ncb[:], cb[:], -1.0, ALU.mult, ['cw'], ['cw'])
            graw = ph.sb("graw", [128, NTILE, 8]); gb = ph.sb("gb", [128, 8]); ge = ph.sb("ge", [128, NTILE, 8])
            self.dma('sp', graw[:], ztm_d.rearrange("(i p) c -> p i c", p=128)[:, :, 1024:1032], [('ztm', i) for i in range(NTILE)], ['graw'])
            self.dma('sp', gb[:], bgate_d.partition_broadcast(128), [], ['gb'])
            self.tt('dve', graw[:], graw[:], gb[:].unsqueeze(1).to_broadcast([128, NTILE, 8]), ALU.add, ['graw', 'gb'], ['graw'])
            self.act(ge[:], graw[:], AF.Exp, ['graw'], ['ge'], scale=-2.0 / 15.0)
            self.ts('dve', ge[:], ge[:], 1.0, ALU.add, ['ge'], ['ge'])
            self.S.op('dve', lambda e: e.reciprocal(out=ge[:], in_=ge[:]), ['ge'], ['ge'])
            self.ts('dve', gates[:], ge[:], 30.0, ALU.mult, ['ge'], ['gates'], s2=-15.0, op1=ALU.add)
            self.act(ge[:, :, 4:8], gates[:, :, 4:8], AF.Exp, ['gates'], ['ge'], scale=-1.0)
            self.act(ge[:, :, 4:8], ge[:, :, 4:8], AF.Ln, ['ge'], ['ge'], bias=1.0)
            self.ts('dve', gates[:, :, 4:8], ge[:, :, 4:8], -1.0, ALU.mult, ['ge'], ['gates'])
            zin = [ph.sb("zin", [128, 3 + NT]) for _ in range(2)]
            acc = ph.sb("acc", [128, NT]); ee = ph.sb("ee", [128, NT])
            qo = [ph.sb("qo", [128, NT], BF16) for _ in range(2)]
            ktsb = ph.sb("ktsb", [128, NTILE, 128], BF16)
            for b in range(2):
                self.memset('pool', zin[b][:, 0:3], 0.0, [('zin', b)])
            for cc in range(8):
                b = cc % 2
                self.dma('sp', zin[b][:, 3:3 + NT], zqk_d[cc * 128:(cc + 1) * 128, :], [('zqk', g) for g in range(NG)], [('zin', b)])
                self.conv_chunk(zin[b], NT, cw, cb, ncb, cc, acc, ee, qo[b][:], [('zin', b)], [('qo', b)])
                self.dma('sp', qkT_d[cc * 128:(cc + 1) * 128, :], qo[b][:], [('qo', b)], [('qkT', cc)])
                if cc >= 4:
                    h = cc - 4
                    for i in range(NTILE):
                        bk = (i // 8) % 2
                        self.tr(self.psb[bk][:, (i % 8) * 128:(i % 8 + 1) * 128], qo[b][:, i * 128:(i + 1) * 128], self.ident_b[:],
                                [('qo', b)], [PS[bk]])
                        if i % 8 == 7:
                            self.cp('act' if bk else 'dve', ktsb[:, i - 7:i + 1, :], self.psb[bk][:].rearrange("p (i c) -> p i c", c=128),
                                    [PS[bk]], ['ktsb'])
                    self.dma('sp', ktm_v[:, :, h * 128:(h + 1) * 128], ktsb[:], ['ktsb'], [('ktm', h)])

    def p_mlstm(self, full, qkT_d, ktm_d, ztm_d, gates, Cst, gmbc_d=None, catT_d=None, fix=None):
        ps, PS, psb = self.ps, self.PS, self.psb
        with self.phase() as ph:
            triu = ph.sb("triu", [128, 128])
            self.S.op('pool', lambda e: e.affine_select(out=triu[:], in_=self.ones_f[:], pattern=[[1, 128]], compare_op=ALU.is_ge,
                                                        fill=0.0, base=0, channel_multiplier=-1), ['ones_f'], ['triu'])
            ktile = [ph.sb("ktile", [128, 512], BF16) for _ in range(2)]
            vraw = [ph.sb("vraw", [128, 512]) for _ in range(2)]
            vaug = [ph.sb("vaug", [128, 4, 129], BF16) for _ in range(2)]
            kt = ph.sb("kt", [128, 4, 128], BF16)
            u = ph.sb("u", [128, 4]); ug = ph.sb("ug", [128, 4]); w = ph.sb("w", [128, 4]); eg = ph.sb("eg", [128, 4])
            for b in range(2):
                self.memset('pool', vaug[b][:, :, 128:129], 1.0, [('vaug', b)])
            if full:
                Cb = ph.sb("Cb", [128, 4, 129], BF16)
                self.cp('act', Cb[:], Cst[:], ['Cst'], ['Cb'])
                gm = ph.sb("gm", [128, 512])
                self.dma('sp', gm[:], gmbc_d.partition_broadcast(128), [], ['gm'])
                qT = [ph.sb("qT", [128, 4, 128], BF16) for _ in range(2)]
                kT = [ph.sb("kT", [128, 4, 128], BF16) for _ in range(2)]
                mo = [ph.sb("mo", [128, 512]) for _ in range(2)]
                lfbc = ph.sb("lfbc", [128, 4, 128])
                EB = ph.sb("EB", [128, 512]); E = ph.sb("E", [128, 4, 128]); Em = ph.sb("Em", [128, 512], BF16)
                St = ph.sb("St", [128, 512], BF16); qtl = ph.sb("qtl", [128, 512], BF16)
                dab = ph.sb("dab", [128, 4]); rec = ph.sb("rec", [128, 4]); og = ph.sb("og", [128, 512])
                hg = ph.sb("hg", [128, 4, 128]); junk = ph.sb("junk", [128, 128]); ss = ph.sb("ss", [128, 4]); rstd = ph.sb("rstd", [128, 4])
                hn = ph.sb("hn", [128, 512], BF16)
                cat = [ph.sb("cat", [128, 4, 128], BF16) for _ in range(2)]
                catT_v = catT_d[0:512, :].rearrange("(h p) t -> p h t", p=128)
                qT_v = qkT_d[0:512, :].rearrange("(h p) t -> p h t", p=128)
                kT_v = qkT_d[512:1024, :].rearrange("(h p) t -> p h t", p=128)
            for i in range(NTILE):
                b = i % 2
                rows = slice(i * 128, (i + 1) * 128)
                if fix is not None and i == 0:
                    self.dma('sp', ktile[b][:], fix[1], [], [('ktile', b)])
                else:
                    self.dma('sp', ktile[b][:], ktm_d[rows, :], [('ktm', h) for h in range(4)], [('ktile', b)])
                self.dma('sp', vraw[b][:], ztm_d[rows, 0:512], [('ztm', i)], [('vraw', b)])
                self.cp('pool', vaug[b][:, :, 0:128], vraw[b][:].rearrange("p (h d) -> p h d", d=128), [('vraw', b)], [('vaug', b)])
                lf = gates[:, i, 4:8]
                ig = gates[:, i, 0:4]
                self.mm(ps[0][:, 0:4], triu[:], lf, True, True, ['triu', 'gates'], [PS[0]])
                if full:
                    if fix is not None and i == 0:
                        self.dma('sp', qT[b][:], fix[0][0:512, :].rearrange("(h p) t -> p h t", p=128), [], [('qT', b)])
                        self.dma('sp', kT[b][:], fix[0][512:1024, :].rearrange("(h p) t -> p h t", p=128), [], [('kT', b)])
                    else:
                        self.dma('sp', qT[b][:], qT_v[:, :, rows], [('qkT', c) for c in range(4)], [('qT', b)])
                        self.dma('sp', kT[b][:], kT_v[:, :, rows], [('qkT', c) for c in range(4, 8)], [('kT', b)])
                    self.dma('sp', mo[b][:], ztm_d[rows, 512:1024], [('ztm', i)], [('mo', b)])
                    for h in range(4):
                        self.ts('dve', lfbc[:, h, :], self.ones_f[:], lf[:, h:h + 1], ALU.mult, ['gates'], [('lfbc', h)])
                        self.mm(ps[1][:, h * 128:(h + 1) * 128], lfbc[:, h, :], triu[:], True, True, [('lfbc', h), 'triu'], [PS[1]])
                    Gap = ps[1][:].rearrange("p (h t) -> p h t", t=128)[:, :, 127]
                    GK = PS[1]
                else:
                    self.mm(ps[0][:, 4:8], self.ones_f[:], lf, True, True, ['gates'], [PS[0]])
                    Gap = ps[0][:, 4:8]
                    GK = PS[0]
                self.tt('dve', u[:], ig, ps[0][:, 0:4], ALU.subtract, ['gates', PS[0]], ['u'])
                self.tt('dve', ug[:], u[:], Gap, ALU.add, ['u', GK], ['ug'])
                self.act(w[:], ug[:], AF.Exp, ['ug'], ['w'])
                self.act(eg[:], Gap, AF.Exp, [GK], ['eg'])
                for h in range(4):
                    self.act(kt[:, h, :], ktile[b][:, h * 128:(h + 1) * 128], AF.Identity, [('ktile', b), 'w'], [('kt', h)], scale=w[:, h:h + 1])
                if full:
                    self.act(EB[:], ps[1][:], AF.Exp, [PS[1]], ['EB'])
                    for h in range(4):
                        self.act(E[:, h, :], ps[1][:, h * 128:(h + 1) * 128], AF.Exp, [PS[1], 'u'], ['E'], bias=u[:, h:h + 1])
                    self.S.op('pool', lambda e: e.affine_select(out=Em[:].rearrange("p (h t) -> p h t", t=128), in_=E[:], pattern=[[0, 4], [1, 128]],
                                                                compare_op=ALU.is_ge, fill=0.0, base=0, channel_multiplier=-1), ['E'], ['Em'])
                    for h in range(4):
                        self.mm(ps[4][:, h * 128:(h + 1) * 128], kT[b][:, h, :], qT[b][:, h, :], True, True, [('kT', b), ('qT', b)], [PS[4]])
                    self.tt('dve', St[:], ps[4][:], Em[:], ALU.mult, [PS[4], 'Em'], ['St'])
                    self.tt('dve', qtl[:], qT[b][:].rearrange("p h t -> p (h t)"), EB[:], ALU.mult, [('qT', b), 'EB'], ['qtl'])
                    for h in range(4):
                        o = ps[5 + h // 2][:, (h % 2) * 129:(h % 2) * 129 + 129]
                        self.mm(o, St[:, h * 128:(h + 1) * 128], vaug[b][:, h, :], True, False, ['St', ('vaug', b)], [PS[5 + h // 2]])
                        self.mm(o, qtl[:, h * 128:(h + 1) * 128], Cb[:, h, :], False, True, ['qtl', 'Cb'], [PS[5 + h // 2]])
                    for k2 in range(2):
                        den2 = ps[5 + k2][:, 0:258].rearrange("p (h c) -> p h c", c=129)[:, :, 128]
                        self.ts('dve', dab[:, 2 * k2:2 * k2 + 2], den2, 1.0, ALU.max, [PS[5 + k2]], ['dab'])
                        self.stt(dab[:, 2 * k2:2 * k2 + 2], den2, -1.0, dab[:, 2 * k2:2 * k2 + 2], ALU.mult, ALU.max, [PS[5 + k2], 'dab'], ['dab'])
                    self.S.op('dve', lambda e: e.reciprocal(out=rec[:], in_=dab[:]), ['dab'], ['rec'])
                    self.act(og[:], mo[b][:], AF.Exp, [('mo', b)], ['og'], scale=-1.0)
                    self.ts('pool', og[:], og[:], 1.0, ALU.add, ['og'], ['og'])
                    self.S.op('dve', lambda e: e.reciprocal(out=og[:], in_=og[:]), ['og'], ['og'])
                    for h in range(4):
                        self.stt(hg[:, h, :], ps[5 + h // 2][:, (h % 2) * 129:(h % 2) * 129 + 128], rec[:, h:h + 1], og[:, h * 128:(h + 1) * 128],
                                 ALU.mult, ALU.mult, [PS[5 + h // 2], 'rec', 'og'], [('hg', h)])
                        self.act(junk[:], hg[:, h, :], AF.Square, [('hg', h)], ['junk', 'ss'], accum_out=ss[:, h:h + 1])
                    self.act(rstd[:], ss[:], AF.Ln, ['ss'], ['rstd'], bias=EPS, scale=1.0 / 128.0)
                    self.act(rstd[:], rstd[:], AF.Exp, ['rstd'], ['rstd'], scale=-0.5)
                    for h in range(4):
                        self.stt(hn[:, h * 128:(h + 1) * 128], hg[:, h, :], rstd[:, h:h + 1], gm[:, h * 128:(h + 1) * 128], ALU.mult, ALU.mult,
                                 [('hg', h), 'rstd', 'gm'], ['hn'])
                    for h in range(4):
                        self.tr(psb[7][:, h * 128:(h + 1) * 128], hn[:, h * 128:(h + 1) * 128], self.ident_b[:], ['hn'], [PS[7]])
                    self.cp('act', cat[b][:], psb[7][:, 0:512].rearrange("p (h t) -> p h t", t=128), [PS[7]], [('cat', b)])
                    self.dma('sp', catT_v[:, :, rows], cat[b][:], [('cat', b)], [('catT', i)])
                for h in range(4):
                    self.mm(ps[2 + h // 2][:, (h % 2) * 129:(h % 2) * 129 + 129], kt[:, h, :], vaug[b][:, h, :], True, True,
                            [('kt', h), ('vaug', b)], [PS[2 + h // 2]])
                for h in range(4):
                    self.stt(Cst[:, h, :], Cst[:, h, :], eg[:, h:h + 1], ps[2 + h // 2][:, (h % 2) * 129:(h % 2) * 129 + 129], ALU.mult, ALU.add,
                             ['Cst', 'eg', PS[2 + h // 2]], ['Cst'])
                if full:
                    self.cp('act', Cb[:], Cst[:], ['Cst'], ['Cb'])


    def p_rotary_tables(self, pos_d, cs, sn):
        import math
        TWO_PI = 2.0 * math.pi
        C1 = 6.28125
        C2 = TWO_PI - C1
        with self.phase() as ph:
            pi_ = ph.sb("pos_i", [128, NTILE], I32); pf = ph.sb("pos_f", [128, NTILE])
            invf = ph.sb("invf", [128, 8]); ang = ph.sb("ang", [128, NTILE, 8])
            ki = ph.sb("ki", [128, NTILE, 8], I32); kf = ph.sb("kf", [128, NTILE, 8]); m = ph.sb("rm", [128, NTILE, 8])
            r = ph.sb("rr", [128, NTILE, 8]); r2 = ph.sb("rr2", [128, NTILE, 8])
            self.dma('sp', pi_[:], pos_d, [], ['pos_i'])
            self.cp('dve', pf[:], pi_[:], ['pos_i'], ['pos_f'])
            for f in range(8):
                self.memset('pool', invf[:, f:f + 1], float(np.float32(500000.0) ** np.float32(-(2.0 * f) / 16.0)), ['invf'])
            self.tt('dve', ang[:], pf[:].unsqueeze(2).to_broadcast([128, NTILE, 8]), invf[:].unsqueeze(1).to_broadcast([128, NTILE, 8]),
                    ALU.mult, ['pos_f', 'invf'], ['ang'])

            def wrap(x, k):
                self.ts('dve', m[:], x[:], math.pi, ALU.is_gt, [k], ['rm'])
                self.stt(x[:], m[:], -TWO_PI, x[:], ALU.mult, ALU.add, ['rm', k], [k])
                self.ts('dve', m[:], x[:], -math.pi, ALU.is_lt, [k], ['rm'])
                self.stt(x[:], m[:], TWO_PI, x[:], ALU.mult, ALU.add, ['rm', k], [k])

            self.ts('dve', kf[:], ang[:], 1.0 / TWO_PI, ALU.mult, ['ang'], ['kf'])
            self.cp('dve', ki[:], kf[:], ['kf'], ['ki'])
            self.cp('dve', kf[:], ki[:], ['ki'], ['kf'])
            self.stt(r[:], kf[:], -C1, ang[:], ALU.mult, ALU.add, ['kf', 'ang'], ['rr'])
            self.stt(r[:], kf[:], -C2, r[:], ALU.mult, ALU.add, ['kf', 'rr'], ['rr'])
            wrap(r, 'rr')
            self.ts('dve', r2[:], r[:], math.pi / 2.0, ALU.add, ['rr'], ['rr2'])
            wrap(r2, 'rr2')
            for x, k in ((r, 'rr'), (r2, 'rr2')):
                self.ts('dve', x[:], x[:], math.pi, ALU.min, [k], [k], s2=-math.pi, op1=ALU.max)
            self.act(sn[:], r[:], AF.Sin, ['rr'], ['rot'])
            self.act(cs[:], r2[:], AF.Sin, ['rr2'], ['rot'])

    def p_moba_prep(self, ztm_d, cs, sn, qrot_d, KTa_d, Vaug_d, kmT_d):
        ps, PS, psb = self.ps, self.PS, self.psb
        with self.phase() as ph:
            zt = [ph.sb("mzt", [128, 2, 1536]) for _ in range(2)]
            t1 = ph.sb("rt1", [128, 2, 16, 8]); t2 = ph.sb("rt2", [128, 2, 16, 8]); t3 = ph.sb("rt3", [128, 2, 16, 8]); t4 = ph.sb("rt4", [128, 2, 16, 8])
            kaug = [ph.sb("kaug", [128, 2, 8, 128], BF16) for _ in range(2)]
            vaug = [ph.sb("mvaug", [128, 2, 8, 65], BF16) for _ in range(2)]
            kta = [ph.sb("kta", [128, 8, 256], BF16) for _ in range(2)]
            kmT = ph.sb("kmT", [64, 8, 16])
            for b in range(2):
                self.memset('pool', kaug[b][:], 0.0, [('kaug', b)])
                self.memset('pool', vaug[b][:, :, :, 64:65], 1.0, [('mvaug', b)])
            ztm_v = ztm_d.rearrange("(n j p) c -> n p j c", p=128, j=2)
            qrot_v = qrot_d.rearrange("(n j p) c -> n p j c", p=128, j=2)
            for n in range(16):
                b = n % 2
                self.dma('sp', zt[b][:], ztm_v[n][:, :, 1032:2568], [('ztm', 2 * n), ('ztm', 2 * n + 1)], [('mzt', b)])
                qk = zt[b][:, :, 0:1024].rearrange("p j (h d) -> p j h d", d=64)
                x1 = qk[:, :, :, 0:8]; x2 = qk[:, :, :, 8:16]
                cosb = cs[:, 2 * n:2 * n + 2, :].unsqueeze(2).to_broadcast([128, 2, 16, 8])
                sinb = sn[:, 2 * n:2 * n + 2, :].unsqueeze(2).to_broadcast([128, 2, 16, 8])
                zk = [('mzt', b)]
                self.tt('dve', t1[:], x1, cosb, ALU.mult, zk + ['rot'], ['rt1'])
                self.tt('dve', t2[:], x2, sinb, ALU.mult, zk + ['rot'], ['rt2'])
                self.tt('dve', t3[:], x2, cosb, ALU.mult, zk + ['rot'], ['rt3'])
                self.tt('dve', t4[:], x1, sinb, ALU.mult, zk + ['rot'], ['rt4'])
                self.tt('dve', x1, t1[:], t2[:], ALU.subtract, ['rt1', 'rt2'], zk)
                self.tt('dve', x2, t3[:], t4[:], ALU.add, ['rt3', 'rt4'], zk)
                self.dma('sp', qrot_v[n], zt[b][:, :, 0:512], zk, [('qrot', n)])
                self.cp('act', kaug[b][:, :, :, 0:64], zt[b][:, :, 512:1024].rearrange("p j (h d) -> p j h d", d=64), zk, [('kaug', b)])
                if n >= 2:
                    self.memset('pool', kaug[b][:, :, :, 64 + n - 2:64 + n - 1], 0.0, [('kaug', b)])
                self.memset('pool', kaug[b][:, :, :, 64 + n:64 + n + 1], 1.0, [('kaug', b)])
                self.cp('pool', vaug[b][:, :, :, 0:64], zt[b][:, :, 1024:1536].rearrange("p j (h d) -> p j h d", d=64), zk, [('mvaug', b)])
                for j in range(2):
                    self.dma('sp', Vaug_d[:, (2 * n + j) * 128:(2 * n + j + 1) * 128, :].rearrange("h p c -> p h c"), vaug[b][:, j, :, :],
                             [('mvaug', b)], [('Vaug', n, j)])
                for h in range(8):
                    for j in range(2):
                        self.mm(ps[6][0:64, h:h + 1], zt[b][:, j, 512 + h * 64:512 + (h + 1) * 64], self.ones_f[:, 0:1], j == 0, j == 1,
                                zk, [PS[6]])
                self.ts('dve', kmT[:, :, n], ps[6][0:64, 0:8], 1.0 / 256.0, ALU.mult, [PS[6]], ['kmT'])
                for j in range(2):
                    bk = j
                    for h in range(8):
                        self.tr(psb[bk][:, h * 128:(h + 1) * 128], kaug[b][:, j, h, :], self.ident_b[:], [('kaug', b)], [PS[bk]])
                    self.cp('act' if j else 'dve', kta[b][:, :, j * 128:(j + 1) * 128], psb[bk][:].rearrange("p (h t) -> p h t", t=128),
                            [PS[bk]], [('kta', b)])
                self.dma('sp', KTa_d.rearrange("h r t -> r h t")[:, :, n * 256:(n + 1) * 256], kta[b][:], [('kta', b)], [('KTa', n)])
            self.dma('sp', kmT_d, kmT[:], ['kmT'], ['kmT_d'])


    def p_moba_attn(self, qrot_d, KTa_o, Vaug_o, kmT_o, KTa_p, Vaug_p, kmT_p, selb, catT_d, nq_tiles=NTILE, heads=range(8)):
        ps, PS, psb = self.ps, self.PS, self.psb
        BIG2 = 30000.0
        with self.phase() as ph:
            km = ph.sb("km", [128, 8, 32])
            self.memset('dve', km[:], 0.0, ['km'])
            for e_ in range(2):
                kmv = km[e_ * 64:(e_ + 1) * 64, :, :].rearrange("p (j e) n -> p j e n", e=2)[:, :, e_, :]
                self.dma('sp', kmv[:, :, 0:16], kmT_p.rearrange("d (j e) n -> d j e n", e=2)[:, :, e_, :], [], ['km'])
                self.dma('sp', kmv[:, :, 16:32], kmT_o.rearrange("d (j e) n -> d j e n", e=2)[:, :, e_, :], [], ['km'])
            QTp_d = self.dram("QTp_scr%d" % self.uid, [NTILE, 128, 8, 128], BF16, kind=self.scratch_kind)
            QTo_d = self.dram("QTo_scr%d" % self.uid, [NTILE, 128, 8, 128], BF16, kind=self.scratch_kind)
            self.uid += 1
            qr = [ph.sb("qr", [128, 512]) for _ in range(2)]
            qt32 = ph.sb("qt32", [128, 4, 128])
            qtp = [ph.sb("qtp", [128, 8, 128], BF16) for _ in range(2)]
            qto = [ph.sb("qto", [128, 8, 128], BF16) for _ in range(2)]
            G = ph.sb("G", [128, 8, 32]); top8 = ph.sb("top8", [128, 8, 8]); m1 = ph.sb("m1", [128, 8, 32]); m2 = ph.sb("m2", [128, 8, 32])
            MA = ph.sb("MA", [128, 8, 128], BF16)
            self.memset('pool', MA[:], 0.0, ['MA'])
            for b in range(2):
                self.memset('pool', qtp[b][:], 0.0, [('qtp', b)])
                self.memset('pool', qto[b][:], 0.0, [('qto', b)])
            for i in range(nq_tiles):
                b = i % 2
                nb = i // 2
                nc_ = 16 + nb
                self.dma('sp', qr[b][:], qrot_d[i * 128:(i + 1) * 128, :], [('qrot', nb)], [('qr', b)])
                import os
                DBGM = int(os.environ.get("DBGM", "9"))
                for j in range(4):
                    self.tr(ps[0][:, j * 128:(j + 1) * 128], qr[b][:, j * 128:(j + 1) * 128], self.ident_f[:], [('qr', b)], [PS[0]])
                self.cp('dve', qt32[:], ps[0][:].rearrange("p (j t) -> p j t", t=128), [PS[0]], ['qt32'])
                for e_ in range(2 if DBGM >= 1 else 0):
                    src = qt32[e_ * 64:(e_ + 1) * 64, :, :]
                    dp = qtp[b][0:64, :, :].rearrange("p (j e) t -> p j e t", e=2)[:, :, e_, :]
                    do = qto[b][0:64, :, :].rearrange("p (j e) t -> p j e t", e=2)[:, :, e_, :]
                    self.ts('dve', dp, src, 0.125, ALU.mult, ['qt32'], [('qtp', b)])
                    self.ts('pool' if e_ == 0 else 'dve', do, src, 0.125, ALU.mult, ['qt32'], [('qto', b)])
                if DBGM < 2: continue
                for h in range(8):
                    e_ = h % 2
                    self.mm(ps[2][:, h * 32:(h + 1) * 32], qt32[:, h // 2, :], km[:, h, :], True, True, ['qt32', 'km'], [PS[2]])
                Gv = ps[2][:, 0:256].rearrange("p (h n) -> p h n", n=32)
                self.ts('dve', G[:, :, 0:16], Gv[:, :, 0:16], selb[:, 0:1], ALU.add, [PS[2], 'selb'], ['G'])
                self.cp('dve', G[:, :, 16:32], Gv[:, :, 16:32], [PS[2]], ['G'])
                if DBGM < 3: continue
                for h in range(8):
                    self.S.op('dve', lambda e, h=h, nc_=nc_: e.max(out=top8[:, h, :], in_=G[:, h, 0:nc_]), ['G'], ['top8'])
                self.tt('dve', m1[:, :, 0:nc_], G[:, :, 0:nc_], top8[:, :, 2:3].to_broadcast([128, 8, nc_]), ALU.is_ge, ['G', 'top8'], ['m1'])
                self.ts('dve', m2[:, :, 0:nc_], G[:, :, 0:nc_], -1e29, ALU.is_gt, ['G'], ['m2'])
                self.tt('dve', m1[:, :, 0:nc_], m1[:, :, 0:nc_], m2[:, :, 0:nc_], ALU.mult, ['m1', 'm2'], ['m1'])
                if DBGM < 4: continue
                self.ts('dve', MA[:, :, 64:80], m1[:, :, 0:16], BIG2, ALU.mult, ['m1'], ['MA'], s2=-BIG2, op1=ALU.add)
                if nb > 0:
                    self.ts('dve', MA[:, :, 96:96 + nb], m1[:, :, 16:16 + nb], BIG2, ALU.mult, ['m1'], ['MA'], s2=-BIG2, op1=ALU.add)
                for h in range(8):
                    self.tr(psb[3][:, h * 128:(h + 1) * 128], MA[:, h, :], self.ident_b[:], ['MA'], [PS[3]])
                if DBGM < 5: continue
                mt = psb[3][:].rearrange("p (h t) -> p h t", t=128)
                self.cp('dve', qtp[b][64:80, :, :], mt[64:80, :, :], [PS[3]], [('qtp', b)])
                self.cp('dve', qto[b][64:80, :, :], mt[96:112, :, :], [PS[3]], [('qto', b)])
                if DBGM < 6: continue
                self.dma('sp', QTp_d[i], qtp[b][:], [('qtp', b)], [('QTp', i)])
                self.dma('sp', QTo_d[i], qto[b][:], [('qto', b)], [('QTo', i)])
        with self.phase() as ph:
            kp = ph.sb("kp", [128, NT], BF16); ko = ph.sb("ko", [128, NT], BF16)
            vp = ph.sb("vp", [128, NTILE, 65], BF16); vo = ph.sb("vo", [128, NTILE, 65], BF16)
            qhp = ph.sb("qhp", [128, NTILE, 128], BF16); qho = ph.sb("qho", [128, NTILE, 128], BF16)
            triu_b = ph.sb("triu_b2", [128, 128], BF16)
            self.S.op('pool', lambda e: e.affine_select(out=triu_b[:], in_=self.ones_f[:], pattern=[[1, 128]], compare_op=ALU.is_ge,
                                                        fill=0.0, base=0, channel_multiplier=-1), ['ones_f'], ['triu_b'])
            PT = [ph.sb("PT", [128, 512], BF16) for _ in range(3)]
            rec = ph.sb("mrec", [128, 1]); ob = ph.sb("ob", [128, 64], BF16)
            haT = ph.sb("haT", [64, NT], BF16)
            pt_i = 0
            sb_i = 0
            for h in heads:
                self.dma('sp', kp[:], KTa_p[h], [], ['kp'])
                self.dma('sp', ko[:], KTa_o[h], [('KTa', n) for n in range(16)], ['ko'])
                self.dma('sp', vp[:], Vaug_p[h].rearrange("(i p) c -> p i c", p=128), [], ['vp'])
                self.dma('sp', vo[:], Vaug_o[h].rearrange("(i p) c -> p i c", p=128), [('Vaug', n, j) for n in range(16) for j in range(2)], ['vo'])
                self.dma('sp', qhp[:], QTp_d[:, :, h, :].rearrange("i r t -> r i t"), [('QTp', i) for i in range(nq_tiles)], ['qhp'])
                self.dma('sp', qho[:], QTo_d[:, :, h, :].rearrange("i r t -> r i t"), [('QTo', i) for i in range(nq_tiles)], ['qho'])
                work = []
                for i in range(nq_tiles):
                    kts = [('p', j) for j in range(NTILE)] + [('o', j) for j in range(i + 1)]
                    nk = len(kts)
                    for c0 in range(0, nk, 4):
                        work.append((i, c0, kts[c0:c0 + 4], nk))

                def emit_S(wk):
                    nonlocal sb_i
                    i, c0, chunk, nk = wk
                    bk = sb_i % 4
                    sb_i += 1
                    for jj, (kind, j) in enumerate(chunk):
                        ksrc, qsrc, kk, qk_ = (kp, qhp, 'kp', 'qhp') if kind == 'p' else (ko, qho, 'ko', 'qho')
                        self.mm(ps[bk][:, jj * 128:(jj + 1) * 128], ksrc[:, j * 128:(j + 1) * 128], qsrc[:, i, :], True, True,
                                [kk, qk_], [PS[bk]])
                    return bk

                def emit_rest(wk, bk):
                    nonlocal pt_i
                    i, c0, chunk, nk = wk
                    accb = 6 + (i % 2)
                    acc = ps[accb][:, 0:65]
                    pt = PT[pt_i % 3]
                    pk = ('PT', pt_i % 3)
                    pt_i += 1
                    w_ = len(chunk) * 128
                    self.act(pt[:, 0:w_], ps[bk][:, 0:w_], AF.Exp, [PS[bk]], [pk])
                    if chunk[-1] == ('o', i):
                        jj = len(chunk) - 1
                        self.tt('pool', pt[:, jj * 128:(jj + 1) * 128], pt[:, jj * 128:(jj + 1) * 128], triu_b[:], ALU.mult, [pk, 'triu_b'], [pk])
                    for jj, (kind, j) in enumerate(chunk):
                        vsrc, vk = (vp, 'vp') if kind == 'p' else (vo, 'vo')
                        first = (c0 + jj == 0)
                        last = (c0 + jj == nk - 1)
                        self.mm(acc, pt[:, jj * 128:(jj + 1) * 128], vsrc[:, j, :], first, last, [pk, vk], [PS[accb]])
                    if c0 + len(chunk) == nk:
                        self.S.op('dve', lambda e, accb=accb: e.reciprocal(out=rec[:], in_=ps[accb][:, 64:65]), [PS[accb]], ['mrec'])
                        self.ts('dve', ob[:], ps[accb][:, 0:64], rec[:, 0:1], ALU.mult, [PS[accb], 'mrec'], ['ob'])
                        self.tr(psb[5][0:64, (i % 8) * 128:(i % 8 + 1) * 128], ob[:], self.ident_b[:], ['ob'], [PS[5]])
                        if i % 8 == 7 or i == nq_tiles - 1:
                            i0 = (i // 8) * 8
                            n_ = i - i0 + 1
                            self.cp('act', haT[:, i0 * 128:(i + 1) * 128], psb[5][0:64, 0:n_ * 128], [PS[5]], ['haT'])

                pend = []
                for wk in work:
                    bk = emit_S(wk)
                    pend.append((wk, bk))
                    if len(pend) > 3:
                        emit_rest(*pend.pop(0))
                while pend:
                    emit_rest(*pend.pop(0))
                self.dma('sp', catT_d[512 + h * 64:512 + (h + 1) * 64, 0:nq_tiles * 128], haT[:, 0:nq_tiles * 128], ['haT'], [('catT_a', h)])


    def p_outproj(self, catT_d, w_out_d, G1, xT_d, dbg_out=None, xT_src=None):
        ps, PS = self.ps, self.PS
        xT_v = xT_d.rearrange("(kc p) t -> p kc t", p=128)
        xs_v = xT_v if xT_src is None else xT_src.rearrange("(kc p) t -> p kc t", p=128)
        cat_v = catT_d.rearrange("(kc p) t -> p kc t", p=128)
        with self.phase() as ph:
            wo = ph.sb("wo", [128, 8, 1024], BF16)
            self.p_load_w_bf16(ph, w_out_d, 1024, wo, 'wo', 0)
            cg = [ph.sb("cg", [128, 8, 512], BF16) for _ in range(2)]
            xg = [ph.sb("oxg", [128, 8, 512]) for _ in range(2)]
            for g in range(NG):
                b = g % 2
                cols = slice(g * 512, (g + 1) * 512)
                self.dma('sp', cg[b][:], cat_v[:, :, cols], [], [('cg', b)])
                self.dma('sp', xg[b][:], xs_v[:, :, cols], [('xT', g)], [('oxg', b)])
                for c in range(8):
                    bk = c % 4
                    for kc in range(8):
                        self.mm(ps[bk][:], wo[:, kc, c * 128:(c + 1) * 128], cg[b][:, kc, :], kc == 0, kc == 7, ['wo', ('cg', b)], [PS[bk]])
                    self.stt(xg[b][:, c, :], ps[bk][:], G1[:, c:c + 1], xg[b][:, c, :], ALU.mult, ALU.add, [PS[bk], ('oxg', b), 'lconst'], [('oxg', b)])
                self.dma('sp', xT_v[:, :, cols], xg[b][:], [('oxg', b)], [('xT', g)])
                if dbg_out is not None:
                    self.dma('sp', dbg_out.rearrange("(kc p) t -> p kc t", p=128)[:, :, cols], xg[b][:], [('oxg', b)], [('dbg', g)])

    def p_router(self, xT_d, A2, B2, w_router_d, brbc_d, h2T_d, PT):
        ps, PS = self.ps, self.PS
        h2_v = h2T_d.rearrange("(kc p) t -> p kc t", p=128)
        with self.phase() as ph:
            wr = ph.sb("wr", [128, 8, 32], BF16); wrs = ph.sb("wrs", [128, 8, 32]); br = ph.sb("br", [128, 32])
            self.dma('sp', wrs[:], w_router_d.rearrange("(kc p) n -> p kc n", p=128), [], ['wrs'])
            self.cp('dve', wr[:], wrs[:], ['wrs'], ['wr'])
            self.dma('sp', br[:], brbc_d.partition_broadcast(128), [], ['br'])
            xg = ph.sb("n_xg", [128, 8, 512]); sq = ph.sb("n_sq", [128, 8, 512]); rstd = ph.sb("n_rstd", [128, 512])
            hT = [ph.sb("h2", [128, 8, 512], BF16) for _ in range(2)]
            lg = ph.sb("lg", [128, 32]); top8 = ph.sb("rtop8", [128, 8]); ntop = ph.sb("ntop", [128, 1]); msk = ph.sb("rmsk", [128, 32])
            ex = ph.sb("rex", [128, 32]); ssum = ph.sb("rsum", [128, 1]); pp = ph.sb("rpp", [128, 128])
            self.memset('dve', pp[:], 0.0, ['rpp'])
            for g in range(NG):
                hb = g % 2
                self.p_norm_group(ph, g, xT_d, A2, B2, hT[hb], ('h2', hb), (xg, sq, rstd))
                self.dma('sp', h2_v[:, :, g * 512:(g + 1) * 512], hT[hb][:], [('h2', hb)], [('h2T', g)])
                for j in range(4):
                    ti = g * 4 + j
                    for kc in range(8):
                        self.mm(ps[0][:, 0:32], hT[hb][:, kc, j * 128:(j + 1) * 128], wr[:, kc, :], kc == 0, kc == 7, [('h2', hb), 'wr'], [PS[0]])
                    self.tt('dve', lg[:], ps[0][:, 0:32], br[:], ALU.add, [PS[0], 'br'], ['lg'])
                    self.S.op('dve', lambda e: e.max(out=top8[:], in_=lg[:]), ['lg'], ['rtop8'])
                    self.ts('dve', msk[:], lg[:], top8[:, 3:4], ALU.is_ge, ['lg', 'rtop8'], ['rmsk'])
                    self.ts('dve', ntop[:], top8[:, 0:1], -1.0, ALU.mult, ['rtop8'], ['ntop'])
                    self.act(ex[:], lg[:], AF.Exp, ['lg', 'ntop'], ['rex'], bias=ntop[:, 0:1])
                    self.tt('dve', ex[:], ex[:], msk[:], ALU.mult, ['rex', 'rmsk'], ['rex'])
                    self.S.op('dve', lambda e: e.tensor_reduce(out=ssum[:], in_=ex[:], axis=AX.X, op=ALU.add), ['rex'], ['rsum'])
                    self.S.op('dve', lambda e: e.reciprocal(out=ssum[:], in_=ssum[:]), ['rsum'], ['rsum'])
                    self.ts('dve', pp[:, 0:32], ex[:], ssum[:, 0:1], ALU.mult, ['rex', 'rsum'], ['rpp'])
                    self.tr(ps[1][:, 0:128], pp[:], self.ident_f[:], ['rpp'], [PS[1]])
                    self.cp('act', PT[:, ti * 128:(ti + 1) * 128], ps[1][0:32, 0:128], [PS[1]], ['PT'])

    def p_moe(self, h2T_d, PT, wg_d, wu_d, wd_d, bgT_d, buT_d, bd_d, G2, xT_d, n_exp=32, n_grp=NT // 1024):
        ps, PS = self.ps, self.PS
        import math
        SIGMAX = 1.0 / (1.0 + math.exp(-1.702 * 7.0))
        xT_v = xT_d.rearrange("(kc p) t -> p kc t", p=128)
        h2_v = h2T_d.rearrange("(kc p) t -> p kc t", p=128)
        with self.phase() as ph:
            pte = [ph.sb("pte", [32, 512]) for _ in range(2)]
            bg = ph.sb("bg", [128, 32, 8]); bu = ph.sb("bu", [128, 32, 8]); nbg = ph.sb("nbg", [128, 32, 8]); bd = ph.sb("bd", [32, 1024])
            self.dma('sp', bg[:], bgT_d, [], ['bg']); self.dma('sp', bu[:], buT_d, [], ['bu']); self.dma('sp', bd[:], bd_d, [], ['bd'])
            self.ts('dve', nbg[:], bg[:], 1.702, ALU.mult, ['bg'], ['nbg'])
            h2 = ph.sb("mh2", [128, 8, 1024], BF16)
            acc = ph.sb("macc", [128, 8, 1024])
            actT = ph.sb("actT", [128, 8, 1024], BF16)
            stg = [ph.sb("mstg", [128, 8, 512]) for _ in range(2)]
            wgb = [ph.sb("wgb", [128, 8, 512], BF16) for _ in range(2)]
            wub = [ph.sb("wub", [128, 8, 512], BF16) for _ in range(2)]
            wdb = [ph.sb("wdb", [128, 8, 512], BF16) for _ in range(2)]
            gtw = [ph.sb("gtw", [128, 1024]) for _ in range(2)]
            e1w = [ph.sb("e1w", [128, 1024]) for _ in range(2)]
            u1w = [ph.sb("u1w", [128, 1024]) for _ in range(2)]
            pbc = ph.sb("pbc", [128, 1024])
            xg = stg[0]
            cnt = {'stg': 0, 'wg': 0, 'wu': 0, 'wd': 0, 'tw': 0}

            import os
            NOLOAD = int(os.environ.get("NOLOAD", "0"))

            def load_blk(w_d, e, blk, dst_list, dk, conv_eng):
                si = cnt['stg'] % 2
                cnt['stg'] += 1
                di = cnt[dk] % 2
                cnt[dk] += 1
                if NOLOAD and e > 0:
                    if NOLOAD == 2:
                        wv = w_d[e].rearrange("(kc p) n -> p kc n", p=128)
                        self.dma('sp', stg[si][:], wv[:, :, blk * 512:(blk + 1) * 512], [], [('mstg', si)])
                    return dst_list[di], (dk, di)
                wv = w_d[e].rearrange("(kc p) n -> p kc n", p=128)
                self.dma('sp', stg[si][:], wv[:, :, blk * 512:(blk + 1) * 512], [], [('mstg', si)])
                for kc in range(8):
                    eng = conv_eng[kc % len(conv_eng)]
                    self.cp(eng, dst_list[di][:, kc, :], stg[si][:, kc, :], [('mstg', si)], [(dk, di)])
                return dst_list[di], (dk, di)

            for gq in range(n_grp):
                tcols = slice(gq * 1024, (gq + 1) * 1024)
                self.dma('sp', h2[:], h2_v[:, :, tcols], [], ['mh2'])
                for hf in range(2):
                    for c in range(8):
                        bk = (hf * 8 + c) % 2
                        self.mm(ps[bk][:], bd[:, c * 128:(c + 1) * 128], PT[:, gq * 1024 + hf * 512:gq * 1024 + (hf + 1) * 512], True, True,
                                ['bd', 'PT'], [PS[bk]])
                        self.cp('act', acc[:, c, hf * 512:(hf + 1) * 512], ps[bk][:], [PS[bk]], [('macc', c, hf)])
                units = []
                for e in range(n_exp):
                    units += [('gu', e, 0), ('gu', e, 1), ('d', e, 0), ('d', e, 1)]

                def prep(u):
                    kind, e, blk = u
                    if kind == 'gu':
                        return (load_blk(wg_d, e, blk, wgb, 'wg', ['act', 'pool', 'act']), load_blk(wu_d, e, blk, wub, 'wu', ['act', 'act', 'pool']))
                    return (load_blk(wd_d, e, blk, wdb, 'wd', ['act', 'pool', 'act']),)

                def compute(u, hd):
                    kind, e, blk = u
                    if kind == 'gu':
                        (wg_t, wgk), (wu_t, wuk) = hd
                        if blk == 0:
                            for hf in range(2):
                                self.ts('dve', pte[hf][:], PT[:, gq * 1024 + hf * 512:gq * 1024 + (hf + 1) * 512], self.ident_f[0:32, e:e + 1], ALU.mult,
                                        ['PT'], [('pte', hf)])
                                self.mm(ps[6 + hf][:], self.ones_f[0:32, :], pte[hf][:], True, True, [('pte', hf)], [PS[6 + hf]])
                                self.cp('act', pbc[:, hf * 512:(hf + 1) * 512], ps[6 + hf][:], [PS[6 + hf]], ['pbc'])
                        for f4 in range(4):
                            fc = blk * 4 + f4
                            ti = cnt['tw'] % 2
                            cnt['tw'] += 1
                            gt, e1, u1 = gtw[ti], e1w[ti], u1w[ti]
                            kg, ke, ku = ('gtw', ti), ('e1w', ti), ('u1w', ti)
                            for hf in range(2):
                                hcols = slice(hf * 512, (hf + 1) * 512)
                                ba, bb = 2 * hf, 2 * hf + 1
                                for kc in range(8):
                                    self.mm(ps[ba][:], wg_t[:, kc, f4 * 128:(f4 + 1) * 128], h2[:, kc, hcols], kc == 0, kc == 7, [wgk, 'mh2'], [PS[ba]])
                                for kc in range(8):
                                    self.mm(ps[bb][:], wu_t[:, kc, f4 * 128:(f4 + 1) * 128], h2[:, kc, hcols], kc == 0, kc == 7, [wuk, 'mh2'], [PS[bb]])
                                self.act(e1[:, hcols], ps[ba][:], AF.Sigmoid, [PS[ba], 'nbg'], [ke], bias=nbg[:, e, fc:fc + 1], scale=1.702)
                                self.ts('dve', gt[:, hcols], ps[ba][:], bg[:, e, fc:fc + 1], ALU.add, [PS[ba], 'bg'], [kg], s2=7.0, op1=ALU.min)
                                self.ts('dve', u1[:, hcols], ps[bb][:], bu[:, e, fc:fc + 1], ALU.add, [PS[bb], 'bu'], [ku], s2=7.0, op1=ALU.min)
                            self.act(u1[:], u1[:], AF.Relu, [ku], [ku], bias=self.c7[:, 0:1])
                            self.stt(gt[:], e1[:], SIGMAX, gt[:], ALU.min, ALU.mult, [ke, kg], [kg])
                            self.stt(gt[:], u1[:], -6.0, gt[:], ALU.add, ALU.mult, [ku, kg], [kg])
                            self.tt('pool', actT[:, fc, :], gt[:], pbc[:], ALU.mult, [kg, 'pbc'], [('actT', fc, 0), ('actT', fc, 1)])
                    else:
                        (wd_t, wdk), = hd
                        for hf in range(2):
                            hcols = slice(hf * 512, (hf + 1) * 512)
                            for c4 in range(4):
                                c = blk * 4 + c4
                                bk = 4 + (c4 % 2)
                                for fc in range(8):
                                    self.mm(ps[bk][:], wd_t[:, fc, c4 * 128:(c4 + 1) * 128], actT[:, fc, hcols], fc == 0, fc == 7,
                                            [wdk, ('actT', fc, hf)], [PS[bk]])
                                self.tt('dve', acc[:, c, hcols], acc[:, c, hcols], ps[bk][:], ALU.add, [PS[bk], ('macc', c, hf)], [('macc', c, hf)])

                hd_next = prep(units[0]) if units else None
                for ui, u in enumerate(units):
                    hd = hd_next
                    hd_next = prep(units[ui + 1]) if ui + 1 < len(units) else None
                    compute(u, hd)
                for hf in range(2):
                    g = gq * 2 + hf
                    self.dma('sp', xg[:], xT_v[:, :, g * 512:(g + 1) * 512], [('xT', g)], [('mstg', 0)])
                    for c in range(8):
                        self.stt(xg[:, c, :], acc[:, c, hf * 512:(hf + 1) * 512], G2[:, c:c + 1], xg[:, c, :], ALU.mult, ALU.add,
                                 [('macc', c, hf), ('mstg', 0), 'lconst'], [('mstg', 0)])
                    self.dma('sp', xT_v[:, :, g * 512:(g + 1) * 512], xg[:], [('mstg', 0)], [('xT', g)])

    def p_router_sparse(self, xT_d, A2, B2, w_router_d, brbc_d, Xg_d, slots_all, CAP, ibuf):
        ps, PS, psb = self.ps, self.PS, self.psb
        R = 32 * CAP
        with self.phase() as ph:
            wr = ph.sb("wr", [128, 8, 32], BF16); wrs = ph.sb("wrs", [128, 8, 32]); br = ph.sb("br", [128, 32])
            self.dma('sp', wrs[:], w_router_d.rearrange("(kc p) n -> p kc n", p=128), [], ['wrs'])
            self.cp('dve', wr[:], wrs[:], ['wrs'], ['wr'])
            self.dma('sp', br[:], brbc_d.partition_broadcast(128), [], ['br'])
            striu = ph.sb("striu", [128, 128], BF16); ones_b = ph.sb("ones_b", [128, 128], BF16)
            self.S.op('pool', lambda e: e.affine_select(out=striu[:], in_=self.ones_f[:], pattern=[[1, 128]], compare_op=ALU.is_gt,
                                                        fill=0.0, base=0, channel_multiplier=-1), ['ones_f'], ['striu'])
            self.cp('dve', ones_b[:], self.ones_f[:], ['ones_f'], ['ones_b'])
            ecol = ph.sb("ecol", [128, 32]); base = ph.sb("rbase", [128, 32])
            self.S.op('pool', lambda e: e.iota(ecol[:], pattern=[[CAP, 32]], base=1, channel_multiplier=0, allow_small_or_imprecise_dtypes=True), [], ['ecol'])
            self.memset('dve', base[:], 0.0, ['rbase'])
            xg = ph.sb("n_xg", [128, 8, 512]); sq = ph.sb("n_sq", [128, 8, 512]); rstd = ph.sb("n_rstd", [128, 512])
            hT = [ph.sb("h2", [128, 8, 512], BF16) for _ in range(2)]
            lg = ph.sb("lg", [128, 32]); top8 = ph.sb("rtop8", [128, 8]); ntop = ph.sb("ntop", [128, 1]); msk = ph.sb("rmsk", [128, 32])
            mskb = ph.sb("rmskb", [128, 32], BF16)
            ex = ph.sb("rex", [128, 32]); ssum = ph.sb("rsum", [128, 1]); pp = ph.sb("rpp", [128, 32])
            pos = ph.sb("rpos", [128, 32]); val = ph.sb("rval", [128, 32]); ovf = ph.sb("rovf", [128, 32]); v8 = ph.sb("rv8", [128, 8])
            junk = ph.sb("rjunk", [128, 32]); slf = ph.sb("rslf", [128, 4])
            hrow = [ph.sb("hrow", [128, 4, 1026], BF16) for rb_ in range(2)]
            for g in range(NG):
                hb = g % 2
                self.p_norm_group(ph, g, xT_d, A2, B2, hT[hb], ('h2', hb), (xg, sq, rstd))
                for j in range(4):
                    ti = g * 4 + j
                    rb = ti % 2
                    for kc in range(8):
                        self.mm(ps[0][:, 0:32], hT[hb][:, kc, j * 128:(j + 1) * 128], wr[:, kc, :], kc == 0, kc == 7, [('h2', hb), 'wr'], [PS[0]])
                    self.tt('dve', lg[:], ps[0][:, 0:32], br[:], ALU.add, [PS[0], 'br'], ['lg'])
                    self.S.op('dve', lambda e: e.max(out=top8[:], in_=lg[:]), ['lg'], ['rtop8'])
                    self.ts('dve', msk[:], lg[:], top8[:, 3:4], ALU.is_ge, ['lg', 'rtop8'], ['rmsk'])
                    self.cp('dve', mskb[:], msk[:], ['rmsk'], ['rmskb'])
                    self.ts('dve', ntop[:], top8[:, 0:1], -1.0, ALU.mult, ['rtop8'], ['ntop'])
                    self.act(ex[:], lg[:], AF.Exp, ['lg', 'ntop'], ['rex'], bias=ntop[:, 0:1])
                    self.tt('dve', ex[:], ex[:], msk[:], ALU.mult, ['rex', 'rmsk'], ['rex'])
                    self.S.op('dve', lambda e: e.tensor_reduce(out=ssum[:], in_=ex[:], axis=AX.X, op=ALU.add), ['rex'], ['rsum'])
                    self.S.op('dve', lambda e: e.reciprocal(out=ssum[:], in_=ssum[:]), ['rsum'], ['rsum'])
                    self.ts('dve', pp[:], ex[:], ssum[:, 0:1], ALU.mult, ['rex', 'rsum'], ['rpp'])
                    self.mm(ps[1][:, 0:32], striu[:], mskb[:], True, True, ['striu', 'rmskb'], [PS[1]])
                    self.mm(ps[1][:, 32:64], ones_b[:], mskb[:], True, True, ['ones_b', 'rmskb'], [PS[1]])
                    self.tt('dve', pos[:], ps[1][:, 0:32], base[:], ALU.add, [PS[1], 'rbase'], ['rpos'])
                    self.tt('dve', base[:], ps[1][:, 32:64], base[:], ALU.add, [PS[1], 'rbase'], ['rbase'])
                    self.ts('dve', ovf[:], pos[:], float(CAP) - 0.5, ALU.is_gt, ['rpos'], ['rovf'], s2=1.0e6, op1=ALU.mult)
                    self.tt('dve', val[:], pos[:], ecol[:], ALU.add, ['rpos', 'ecol'], ['rval'])
                    self.tt('dve', val[:], val[:], ovf[:], ALU.add, ['rval', 'rovf'], ['rval'])
                    self.tt('dve', val[:], val[:], msk[:], ALU.mult, ['rval', 'rmsk'], ['rval'])
                    self.S.op('dve', lambda e: e.max(out=v8[:], in_=val[:]), ['rval'], ['rv8'])
                    self.ts('dve', slf[:], v8[:, 0:4], -1.0, ALU.add, ['rv8'], ['rslf'])
                    self.cp('dve', slots_all[:, ti, :], slf[:], ['rslf'], [('slots', ti)])
                    for kk in range(8):
                        self.tr(psb[2][:, kk * 128:(kk + 1) * 128], hT[hb][:, kk, j * 128:(j + 1) * 128], self.ident_b[:], [('h2', hb)], [PS[2]])
                    for k in range(4):
                        self.cp('act' if k % 2 else 'pool' if False else 'act', hrow[rb][:, k, 0:1024], psb[2][:, :], [PS[2]], [('hrow', rb, k)])
                        pk = hrow[rb][:, k, 1024:1026].bitcast(F32)
                        self.S.op('dve', lambda e, k=k, pk=pk: e.scalar_tensor_tensor(out=junk[:], in0=val[:], scalar=v8[:, k:k + 1], in1=pp[:],
                                                                                    op0=ALU.is_equal, op1=ALU.mult, accum_out=pk),
                                  ['rval', 'rv8', 'rpp'], ['rjunk', ('hrow', rb, k)])
                        self.S.dma('pool', lambda e, k=k, rb=rb, ti=ti: e.indirect_dma_start(
                            out=Xg_d, out_offset=bass.IndirectOffsetOnAxis(ap=slots_all[:, ti, k:k + 1], axis=0),
                            in_=hrow[rb][:, k, :], in_offset=None, bounds_check=self.bc_reg(e, R - 1), oob_is_err=False),
                            [('hrow', rb, k), ('slots', ti)], [('Xg', ti, k)])

    def p_moe_sparse(self, Xg_d, Yg_d, slots_all, wg_d, wu_d, wd_d, bgT_d, buT_d, bd_d, G2, xT_d, CAP, ibuf=None, n_exp=32):
        ps, PS, psb = self.ps, self.PS, self.psb
        import math
        SIGMAX = 1.0 / (1.0 + math.exp(-1.702 * 7.0))
        NH = CAP // 1024
        R = 32 * CAP
        xT_v = xT_d.rearrange("(kc p) t -> p kc t", p=128)
        with self.phase() as ph:
            bg = ph.sb("bg", [128, 32, 8]); bu = ph.sb("bu", [128, 32, 8]); nbg = ph.sb("nbg", [128, 32, 8])
            self.dma('sp', bg[:], bgT_d, [], ['bg']); self.dma('sp', bu[:], buT_d, [], ['bu'])
            self.ts('dve', nbg[:], bg[:], 1.702, ALU.mult, ['bg'], ['nbg'])
            bdr = [ph.sb("bdr", [1, 1024]) for _ in range(2)]
            xs = [ph.sb("xs", [128, 8, 1026], BF16) for _ in range(2)]
            XT = ph.sb("XT", [128, 8, 1024], BF16)
            actT = ph.sb("actT", [128, 8, 1024], BF16)
            stg = [ph.sb("mstg", [128, 8, 512]) for _ in range(2)]
            wgb = [ph.sb("wgb", [128, 8, 512], BF16) for _ in range(2)]
            wub = [ph.sb("wub", [128, 8, 512], BF16) for _ in range(2)]
            wdb = [ph.sb("wdb", [128, 8, 512], BF16) for _ in range(2)]
            gt = ph.sb("gtw", [128, 1024]); e1 = ph.sb("e1w", [128, 1024]); u1 = ph.sb("u1w", [128, 1024])
            yo = [ph.sb("yo", [128, 512]) for _ in range(4)]
            cnt = {'stg': 0, 'wg': 0, 'wu': 0, 'wd': 0, 'yo': 0, 'xs': 0}

            def load_blk(w_d, e, blk, dst_list, dk, conv_eng):
                si = cnt['stg'] % 2
                cnt['stg'] += 1
                di = cnt[dk] % 2
                cnt[dk] += 1
                wv = w_d[e].rearrange("(kc p) n -> p kc n", p=128)
                self.dma('sp', stg[si][:], wv[:, :, blk * 512:(blk + 1) * 512], [], [('mstg', si)])
                for kc in range(8):
                    eng = conv_eng[kc % len(conv_eng)]
                    self.cp(eng, dst_list[di][:, kc, :], stg[si][:, kc, :], [('mstg', si)], [(dk, di)])
                return dst_list[di], (dk, di)

            units = []
            for e in range(n_exp):
                for hh in range(NH):
                    units += [('x', e, hh, 0), ('gu', e, hh, 0), ('gu', e, hh, 1), ('d', e, hh, 0), ('d', e, hh, 1)]

            def prep(u):
                kind, e, hh, blk = u
                if kind == 'x':
                    xb = cnt['xs'] % 2
                    cnt['xs'] += 1
                    r0 = e * CAP + hh * 1024
                    self.dma('sp', xs[xb][:], Xg_d[r0:r0 + 1024, :].rearrange("(s p) c -> p s c", p=128), [], [('xs', xb)])
                    self.dma('sp', bdr[xb][:], bd_d[e:e + 1, :], [], [('bdr', xb)])
                    return (xb,)
                if kind == 'gu':
                    return (load_blk(wg_d, e, blk, wgb, 'wg', ['act', 'pool', 'act']), load_blk(wu_d, e, blk, wub, 'wu', ['act', 'act', 'pool']))
                return (load_blk(wd_d, e, blk, wdb, 'wd', ['act', 'pool', 'act']),)

            cur = {'xb': 0}

            def compute(u, hd):
                kind, e, hh, blk = u
                if kind == 'x':
                    xb = hd[0]
                    cur['xb'] = xb
                    for kc in range(8):
                        bk = 6 + (kc % 2)
                        for st in range(8):
                            self.tr(psb[bk][:, st * 128:(st + 1) * 128], xs[xb][:, st, kc * 128:(kc + 1) * 128], self.ident_b[:], [('xs', xb)], [PS[bk]])
                        self.cp('dve', XT[:, kc, :], psb[bk][:, :], [PS[bk]], [('XT', kc)])
                elif kind == 'gu':
                    (wg_t, wgk), (wu_t, wuk) = hd
                    XTk = [('XT', kc) for kc in range(8)]
                    for f4 in range(4):
                        fc = blk * 4 + f4
                        kg, ke, ku = 'gtw', 'e1w', 'u1w'
                        for hf in range(2):
                            hcols = slice(hf * 512, (hf + 1) * 512)
                            ba, bb = 2 * hf, 2 * hf + 1
                            for kc in range(8):
                                self.mm(ps[ba][:], wg_t[:, kc, f4 * 128:(f4 + 1) * 128], XT[:, kc, hcols], kc == 0, kc == 7, [wgk] + XTk, [PS[ba]])
                            for kc in range(8):
                                self.mm(ps[bb][:], wu_t[:, kc, f4 * 128:(f4 + 1) * 128], XT[:, kc, hcols], kc == 0, kc == 7, [wuk] + XTk, [PS[bb]])
                            self.act(e1[:, hcols], ps[ba][:], AF.Sigmoid, [PS[ba], 'nbg'], [ke], bias=nbg[:, e, fc:fc + 1], scale=1.702)
                            self.ts('dve', gt[:, hcols], ps[ba][:], bg[:, e, fc:fc + 1], ALU.add, [PS[ba], 'bg'], [kg], s2=7.0, op1=ALU.min)
                            self.ts('dve', u1[:, hcols], ps[bb][:], bu[:, e, fc:fc + 1], ALU.add, [PS[bb], 'bu'], [ku], s2=7.0, op1=ALU.min)
                        self.act(u1[:], u1[:], AF.Relu, [ku], [ku], bias=self.c7[:, 0:1])
                        self.stt(gt[:], e1[:], SIGMAX, gt[:], ALU.min, ALU.mult, [ke, kg], [kg])
                        self.stt(actT[:, fc, :], u1[:], -6.0, gt[:], ALU.add, ALU.mult, [ku, kg], [('actT', fc)])
                else:
                    (wd_t, wdk), = hd
                    xb = cur['xb']
                    r0 = e * CAP + hh * 1024
                    for st in range(8):
                        yi = cnt['yo'] % 4
                        cnt['yo'] += 1
                        bk = 4 + (st % 2)
                        pcol = xs[xb][:, st, 1024:1026].bitcast(F32)
                        for fc in range(8):
                            self.mm(ps[bk][:], actT[:, fc, st * 128:(st + 1) * 128], wd_t[:, fc, :], fc == 0, False, [wdk, ('actT', fc)], [PS[bk]])
                        self.mm(ps[bk][:], self.ones_f[0:1, :], bdr[xb][0:1, blk * 512:(blk + 1) * 512], False, True, [('bdr', xb)], [PS[bk]])
                        self.act(yo[yi][:], ps[bk][:], AF.Copy, [PS[bk], ('xs', xb)], [('yo', yi)], scale=pcol)
                        self.dma('sp', Yg_d[r0 + st * 128:r0 + (st + 1) * 128, blk * 512:(blk + 1) * 512], yo[yi][:], [('yo', yi)], [('Yg', e, hh, st, blk)])

            hd_next = prep(units[0])
            for ui, u in enumerate(units):
                hd = hd_next
                hd_next = prep(units[ui + 1]) if ui + 1 < len(units) else None
                compute(u, hd)
        with self.phase() as ph:
            acc4 = [ph.sb("acc4", [128, 4, 1024]) for ab_ in range(2)]
            ysum = ph.sb("ysum", [128, 1024])
            xg = [ph.sb("cxg", [128, 8, 512]) for _ in range(2)]
            for g in range(NG):
                b = g % 2
                self.dma('sp', xg[b][:], xT_v[:, :, g * 512:(g + 1) * 512], [('xT', g)], [('cxg', b)])
                for j in range(4):
                    ti = g * 4 + j
                    ab = ti % 2
                    self.memset('pool', acc4[ab][:], 0.0, [('acc4', ab)])
                    for k in range(4):
                        self.S.dma('pool', lambda e, k=k, ab=ab, ti=ti: e.indirect_dma_start(
                            out=acc4[ab][:, k, :], out_offset=None, in_=Yg_d,
                            in_offset=bass.IndirectOffsetOnAxis(ap=slots_all[:, ti, k:k + 1], axis=0), bounds_check=self.bc_reg(e, R - 1), oob_is_err=False),
                            [('slots', ti)], [('acc4', ab)])
                    self.tt('dve', ysum[:], acc4[ab][:, 0, :], acc4[ab][:, 1, :], ALU.add, [('acc4', ab)], ['ysum'])
                    self.tt('dve', ysum[:], ysum[:], acc4[ab][:, 2, :], ALU.add, [('acc4', ab), 'ysum'], ['ysum'])
                    self.tt('dve', ysum[:], ysum[:], acc4[ab][:, 3, :], ALU.add, [('acc4', ab), 'ysum'], ['ysum'])
                    for kc in range(8):
                        bk = kc % 4
                        self.tr(ps[bk][:, 0:128], ysum[:, kc * 128:(kc + 1) * 128], self.ident_f[:], ['ysum'], [PS[bk]])
                        self.stt(xg[b][:, kc, j * 128:(j + 1) * 128], ps[bk][:, 0:128], G2[:, kc:kc + 1], xg[b][:, kc, j * 128:(j + 1) * 128],
                                 ALU.mult, ALU.add, [PS[bk], ('cxg', b), 'lconst'], [('cxg', b)])
                self.dma('sp', xT_v[:, :, g * 512:(g + 1) * 512], xg[b][:], [('cxg', b)], [('xT', g)])

    def p_router_blocks(self, xT_d, A2, B2, w_router_d, brbc_d, Xg_d, h2rows_d, slots_all, widx, besb, NBLK, BS=1024):
        ps, PS, psb = self.ps, self.PS, self.psb
        R = NBLK * BS
        with self.phase() as ph:
            wr = ph.sb("wr", [128, 8, 32], BF16); wrs = ph.sb("wrs", [128, 8, 32]); br = ph.sb("br", [128, 32])
            self.dma('sp', wrs[:], w_router_d.rearrange("(kc p) n -> p kc n", p=128), [], ['wrs'])
            self.cp('dve', wr[:], wrs[:], ['wrs'], ['wr'])
            self.dma('sp', br[:], brbc_d.partition_broadcast(128), [], ['br'])
            striu = ph.sb("striu", [128, 128], BF16); ones_b = ph.sb("ones_b", [128, 128], BF16)
            self.S.op('pool', lambda e: e.affine_select(out=striu[:], in_=self.ones_f[:], pattern=[[1, 128]], compare_op=ALU.is_gt,
                                                        fill=0.0, base=0, channel_multiplier=-1), ['ones_f'], ['striu'])
            self.cp('dve', ones_b[:], self.ones_f[:], ['ones_f'], ['ones_b'])
            base = ph.sb("rbase", [128, 32])
            self.memset('dve', base[:], 0.0, ['rbase'])
            mskA = ph.sb("mskA", [128, NTILE, 32]); ppA = ph.sb("ppA", [128, NTILE, 32]); posA = ph.sb("posA", [128, NTILE, 32])
            xg = ph.sb("n_xg", [128, 8, 512]); sq = ph.sb("n_sq", [128, 8, 512]); rstd = ph.sb("n_rstd", [128, 512])
            hT = [ph.sb("h2", [128, 8, 512], BF16) for _ in range(2)]
            lg = ph.sb("lg", [128, 32]); top8 = ph.sb("rtop8", [128, 8]); ntop = ph.sb("ntop", [128, 1])
            mskb = ph.sb("rmskb", [128, 32], BF16); ex = ph.sb("rex", [128, 32]); ssum = ph.sb("rsum", [128, 1])
            hr = [ph.sb("hr", [128, 1024], BF16) for _ in range(2)]
            for g in range(NG):
                hb = g % 2
                self.p_norm_group(ph, g, xT_d, A2, B2, hT[hb], ('h2', hb), (xg, sq, rstd))
                for j in range(4):
                    ti = g * 4 + j
                    rb = ti % 2
                    msk = mskA[:, ti, :]
                    for kc in range(8):
                        self.mm(ps[0][:, 0:32], hT[hb][:, kc, j * 128:(j + 1) * 128], wr[:, kc, :], kc == 0, kc == 7, [('h2', hb), 'wr'], [PS[0]])
                    self.tt('dve', lg[:], ps[0][:, 0:32], br[:], ALU.add, [PS[0], 'br'], ['lg'])
                    self.S.op('dve', lambda e: e.max(out=top8[:], in_=lg[:]), ['lg'], ['rtop8'])
                    self.ts('dve', msk, lg[:], top8[:, 3:4], ALU.is_ge, ['lg', 'rtop8'], [('mskA', ti)])
                    self.cp('dve', mskb[:], msk, [('mskA', ti)], ['rmskb'])
                    self.ts('dve', ntop[:], top8[:, 0:1], -1.0, ALU.mult, ['rtop8'], ['ntop'])
                    self.act(ex[:], lg[:], AF.Exp, ['lg', 'ntop'], ['rex'], bias=ntop[:, 0:1])
                    self.tt('dve', ex[:], ex[:], msk, ALU.mult, ['rex', ('mskA', ti)], ['rex'])
                    self.S.op('dve', lambda e: e.tensor_reduce(out=ssum[:], in_=ex[:], axis=AX.X, op=ALU.add), ['rex'], ['rsum'])
                    self.S.op('dve', lambda e: e.reciprocal(out=ssum[:], in_=ssum[:]), ['rsum'], ['rsum'])
                    self.ts('dve', ppA[:, ti, :], ex[:], ssum[:, 0:1], ALU.mult, ['rex', 'rsum'], [('ppA', ti)])
                    self.mm(ps[1][:, 0:32], striu[:], mskb[:], True, True, ['striu', 'rmskb'], [PS[1]])
                    self.mm(ps[1][:, 32:64], ones_b[:], mskb[:], True, True, ['ones_b', 'rmskb'], [PS[1]])
                    self.tt('dve', posA[:, ti, :], ps[1][:, 0:32], base[:], ALU.add, [PS[1], 'rbase'], [('posA', ti)])
                    self.tt('dve', base[:], ps[1][:, 32:64], base[:], ALU.add, [PS[1], 'rbase'], ['rbase'])
                    for kk in range(8):
                        self.tr(psb[2][:, kk * 128:(kk + 1) * 128], hT[hb][:, kk, j * 128:(j + 1) * 128], self.ident_b[:], [('h2', hb)], [PS[2]])
                    self.cp('act', hr[rb][:], psb[2][:, :], [PS[2]], [('hr', rb)])
                    self.dma('sp', h2rows_d[ti * 128:(ti + 1) * 128, :], hr[rb][:], [('hr', rb)], [('h2rows', ti)])
            nblk = ph.sb("nblk", [128, 32]); tmpc = ph.sb("tmpc", [128, 32]); bend = ph.sb("bend", [128, 32]); bst1 = ph.sb("bst1", [128, 32])
            ones32 = ph.sb("ones32", [128, 32])
            self.memset('dve', ones32[:], 1.0, ['ones32'])
            self.ts('dve', nblk[:], base[:], 0.0, ALU.is_gt, ['rbase'], ['nblk'])
            for m in range(1, NT // BS):
                self.ts('dve', tmpc[:], base[:], float(BS * m), ALU.is_gt, ['rbase'], ['tmpc'])
                self.tt('dve', nblk[:], nblk[:], tmpc[:], ALU.add, ['nblk', 'tmpc'], ['nblk'])
            self.S.op('dve', lambda e: e.tensor_tensor_scan(out=bend[:], data0=ones32[:], data1=nblk[:], initial=0.0, op0=ALU.mult, op1=ALU.add),
                      ['ones32', 'nblk'], ['bend'])
            self.tt('dve', bst1[:], bend[:], nblk[:], ALU.subtract, ['bend', 'nblk'], ['bst1'])
            self.ts('dve', bst1[:], bst1[:], float(BS), ALU.mult, ['bst1'], ['bst1'], s2=1.0, op1=ALU.add)
            jidx = ph.sb("jidx", [128, NBLK]); cmp = ph.sb("bcmp", [128, NBLK, 32]); kp = ph.sb("kp", [128, 8]); widf = ph.sb("widf", [128, NBLK, 8])
            self.S.op('pool', lambda e: e.iota(jidx[:], pattern=[[1, NBLK]], base=0, channel_multiplier=0, allow_small_or_imprecise_dtypes=True), [], ['jidx'])
            self.S.op('pool', lambda e: e.iota(kp[:], pattern=[[128, 8]], base=0, channel_multiplier=1, allow_small_or_imprecise_dtypes=True), [], ['kp'])
            self.tt('dve', cmp[:], bend[:].unsqueeze(1).to_broadcast([128, NBLK, 32]), jidx[:].unsqueeze(2).to_broadcast([128, NBLK, 32]), ALU.is_le,
                    ['bend', 'jidx'], ['bcmp'])
            self.S.op('dve', lambda e: e.tensor_reduce(out=besb[:], in_=cmp[:], axis=AX.X, op=ALU.add), ['bcmp'], ['besb'])
            self.stt(widf[:], besb[:].unsqueeze(2).to_broadcast([128, NBLK, 8]), 1024.0, kp[:].unsqueeze(1).to_broadcast([128, NBLK, 8]), ALU.mult, ALU.add,
                     ['besb', 'kp'], ['widf'])
            self.cp('dve', widx[:], widf[:], ['widf'], ['widx'])
            val = ph.sb("rval", [128, 32]); v8 = ph.sb("rv8", [128, 8]); junk = ph.sb("rjunk", [128, 32]); slf = ph.sb("rslf", [128, 4])
            hrow = [ph.sb("hrow", [128, 4, 1026], BF16) for _ in range(2)]
            for ti in range(NTILE):
                rb = ti % 2
                self.dma('sp', hr[rb][:], h2rows_d[ti * 128:(ti + 1) * 128, :], [('h2rows', ti)], [('hr', rb)])
                self.tt('dve', val[:], posA[:, ti, :], bst1[:], ALU.add, [('posA', ti), 'bst1'], ['rval'])
                self.tt('dve', val[:], val[:], mskA[:, ti, :], ALU.mult, ['rval', ('mskA', ti)], ['rval'])
                self.S.op('dve', lambda e: e.max(out=v8[:], in_=val[:]), ['rval'], ['rv8'])
                self.ts('dve', slf[:], v8[:, 0:4], -1.0, ALU.add, ['rv8'], ['rslf'])
                self.cp('dve', slots_all[:, ti, :], slf[:], ['rslf'], [('slots', ti)])
                for k in range(4):
                    self.cp('act' if k % 2 else 'pool', hrow[rb][:, k, 0:1024], hr[rb][:], [('hr', rb)], [('hrow', rb, k)])
                    pk = hrow[rb][:, k, 1024:1026].bitcast(F32)
                    self.S.op('dve', lambda e, k=k, pk=pk, ti=ti: e.scalar_tensor_tensor(out=junk[:], in0=val[:], scalar=v8[:, k:k + 1], in1=ppA[:, ti, :],
                                                                                       op0=ALU.is_equal, op1=ALU.mult, accum_out=pk),
                              ['rval', 'rv8', ('ppA', ti)], ['rjunk', ('hrow', rb, k)])
                    self.S.dma('pool', lambda e, k=k, rb=rb, ti=ti: e.indirect_dma_start(
                        out=Xg_d, out_offset=bass.IndirectOffsetOnAxis(ap=slots_all[:, ti, k:k + 1], axis=0),
                        in_=hrow[rb][:, k, :], in_offset=None, bounds_check=self.bc_reg(e, R - 1), oob_is_err=False),
                        [('hrow', rb, k), ('slots', ti)], [('Xg', ti, k)])

    def p_moe_blocks(self, Xg_d, Yg_d, slots_all, widx, besb, wg_d, wu_d, wd_d, bgT_d, buT_d, bd_d, G2, xT_d, NBLK, BS=1024):
        ps, PS, psb = self.ps, self.PS, self.psb
        import math
        SIGMAX = 1.0 / (1.0 + math.exp(-1.702 * 7.0))
        R = NBLK * BS
        NS = BS // 128
        NHF = BS // 512
        WR = 32 * 1024
        xT_v = xT_d.rearrange("(kc p) t -> p kc t", p=128)
        wflat = {'wg': wg_d.rearrange("e k n -> (e k) n"), 'wu': wu_d.rearrange("e k n -> (e k) n"), 'wd': wd_d.rearrange("e k n -> (e k) n")}
        with self.phase() as ph:
            bg = ph.sb("bg", [128, 32, 8]); bu = ph.sb("bu", [128, 32, 8]); bda = ph.sb("bda", [32, 1024])
            self.dma('sp', bg[:], bgT_d, [], ['bg']); self.dma('sp', bu[:], buT_d, [], ['bg']); self.dma('sp', bda[:], bd_d, [], ['bg'])
            efree = ph.sb("efree", [128, 32]); epart = ph.sb("epart", [32, 1])
            self.S.op('pool', lambda e: e.iota(efree[:], pattern=[[1, 32]], base=0, channel_multiplier=0, allow_small_or_imprecise_dtypes=True), [], ['bg'])
            self.S.op('pool', lambda e: e.iota(epart[:], pattern=[[0, 1]], base=0, channel_multiplier=1, allow_small_or_imprecise_dtypes=True), [], ['bg'])
            oh = ph.sb("oh", [128, 32]); ohc = ph.sb("ohc", [32, 1]); btmp = ph.sb("btmp", [128, 32, 8])
            bsel = [ph.sb("bsel", [128, 3, 8]) for _ in range(2)]
            bdr = [ph.sb("bdr", [1, 1024]) for _ in range(2)]
            xs = [ph.sb("xs", [128, NS, 1026], BF16) for _ in range(2)]
            XT = ph.sb("XT", [128, 8, BS], BF16)
            actT = ph.sb("actT", [128, 8, BS], BF16)
            wf = {'wg': ph.sb("wgf", [128, 8, 1024], BF16), 'wu': ph.sb("wuf", [128, 8, 1024], BF16), 'wd': ph.sb("wdf", [128, 8, 1024], BF16)}
            gtw = [ph.sb("gtw", [128, BS]) for _ in range(2)]; e1w = [ph.sb("e1w", [128, BS]) for _ in range(2)]; u1w = [ph.sb("u1w", [128, BS]) for _ in range(2)]
            yo = [ph.sb("yo", [128, 512]) for _ in range(4)]
            cnt = {'yo': 0, 'tw': 0}

            def gather_w(kind, j):
                for kc in range(8):
                    self.S.dma('pool', lambda e, kind=kind, j=j, kc=kc: e.indirect_dma_start(
                        out=wf[kind][:, kc, :], out_offset=None, in_=wflat[kind],
                        in_offset=bass.IndirectOffsetOnAxis(ap=widx[:, j, kc:kc + 1], axis=0), bounds_check=self.bc_reg(e, WR - 1), oob_is_err=False),
                        ['widx'], [(kind, kc)])

            units = []
            for j in range(NBLK):
                units += [('x', j), ('gu', j), ('d', j)]

            def prep(u):
                kind, j = u
                xb = j % 2
                if kind == 'x':
                    self.dma('sp', xs[xb][:], Xg_d[j * BS:(j + 1) * BS, :].rearrange("(s p) c -> p s c", p=128), [], [('xs', xb)])
                    self.ts('dve', oh[:], efree[:], besb[:, j:j + 1], ALU.is_equal, ['bg', 'besb'], ['oh'])
                    for bi, src in enumerate((bg, bu)):
                        self.tt('dve', btmp[:], src[:], oh[:].unsqueeze(2).to_broadcast([128, 32, 8]), ALU.mult, ['bg', 'oh'], ['btmp'])
                        self.S.op('dve', lambda e, bi=bi, xb=xb: e.tensor_reduce(out=bsel[xb][:, 2 * bi, :], in_=btmp[:].rearrange("p e f -> p f e"), axis=AX.X, op=ALU.add),
                                  ['btmp'], [('bsel', xb)])
                    self.ts('dve', bsel[xb][:, 1, :], bsel[xb][:, 0, :], 1.702, ALU.mult, [('bsel', xb)], [('bsel', xb)])
                    self.ts('dve', ohc[:], epart[:], besb[0:32, j:j + 1], ALU.is_equal, ['bg', 'besb'], ['ohc'])
                    for hf in range(2):
                        self.mm(ps[7][0:1, :], ohc[:, 0:1], bda[:, hf * 512:(hf + 1) * 512], True, True, ['ohc', 'bg'], [PS[7]])
                        self.cp('dve', bdr[xb][0:1, hf * 512:(hf + 1) * 512], ps[7][0:1, :], [PS[7]], [('bdr', xb)])
                elif kind == 'gu':
                    gather_w('wg', j)
                    gather_w('wu', j)
                else:
                    gather_w('wd', j)

            def compute(u):
                kind, j = u
                xb = j % 2
                if kind == 'x':
                    for kc in range(8):
                        bk = 5 + (kc % 2)
                        for st in range(NS):
                            self.tr(psb[bk][:, st * 128:(st + 1) * 128], xs[xb][:, st, kc * 128:(kc + 1) * 128], self.ident_b[:], [('xs', xb)], [PS[bk]])
                        self.cp('dve', XT[:, kc, :], psb[bk][:, 0:BS], [PS[bk]], [('XT', kc)])
                elif kind == 'gu':
                    XTk = [('XT', kc) for kc in range(8)]
                    wgk = [('wg', kc) for kc in range(8)]
                    wuk = [('wu', kc) for kc in range(8)]
                    bs = bsel[xb]
                    for fc in range(8):
                        ti_ = cnt['tw'] % 2
                        cnt['tw'] += 1
                        gt, e1, u1 = gtw[ti_], e1w[ti_], u1w[ti_]
                        kg, ke, ku = ('gtw', ti_), ('e1w', ti_), ('u1w', ti_)
                        for hf in range(NHF):
                            hcols = slice(hf * 512, (hf + 1) * 512)
                            ba, bb = 2 * hf, 2 * hf + 1
                            for kc in range(8):
                                self.mm(ps[ba][:], wf['wg'][:, kc, fc * 128:(fc + 1) * 128], XT[:, kc, hcols], kc == 0, kc == 7, wgk + XTk, [PS[ba]])
                            for kc in range(8):
                                self.mm(ps[bb][:], wf['wu'][:, kc, fc * 128:(fc + 1) * 128], XT[:, kc, hcols], kc == 0, kc == 7, wuk + XTk, [PS[bb]])
                            self.act(e1[:, hcols], ps[ba][:], AF.Sigmoid, [PS[ba], ('bsel', xb)], [ke], bias=bs[:, 1, fc:fc + 1], scale=1.702)
                            self.ts('dve', gt[:, hcols], ps[ba][:], bs[:, 0, fc:fc + 1], ALU.add, [PS[ba], ('bsel', xb)], [kg], s2=7.0, op1=ALU.min)
                            self.ts('dve', u1[:, hcols], ps[bb][:], bs[:, 2, fc:fc + 1], ALU.add, [PS[bb], ('bsel', xb)], [ku], s2=7.0, op1=ALU.min)
                        self.act(u1[:], u1[:], AF.Relu, [ku], [ku], bias=self.c7[:, 0:1])
                        self.stt(gt[:], e1[:], SIGMAX, gt[:], ALU.min, ALU.mult, [ke, kg], [kg])
                        self.stt(actT[:, fc, :], u1[:], -6.0, gt[:], ALU.add, ALU.mult, [ku, kg], [('actT', fc)])
                else:
                    wdk = [('wd', kc) for kc in range(8)]
                    r0 = j * BS
                    for st in range(NS):
                        pcol = xs[xb][:, st, 1024:1026].bitcast(F32)
                        for blk in range(2):
                            yi = cnt['yo'] % 4
                            cnt['yo'] += 1
                            bk = 4 if blk == 0 else 7
                            for fc in range(8):
                                self.mm(ps[bk][:], actT[:, fc, st * 128:(st + 1) * 128], wf['wd'][:, fc, blk * 512:(blk + 1) * 512], fc == 0, False,
                                        wdk + [('actT', fc)], [PS[bk]])
                            self.mm(ps[bk][:], self.ones_f[0:1, :], bdr[xb][0:1, blk * 512:(blk + 1) * 512], False, True, [('bdr', xb)], [PS[bk]])
                            self.act(yo[yi][:], ps[bk][:], AF.Copy, [PS[bk], ('xs', xb)], [('yo', yi)], scale=pcol)
                            self.dma('sp', Yg_d[r0 + st * 128:r0 + (st + 1) * 128, blk * 512:(blk + 1) * 512], yo[yi][:], [('yo', yi)], [('Yg', j, st, blk)])

            prep(units[0])
            for ui, u in enumerate(units):
                if ui + 1 < len(units):
                    prep(units[ui + 1])
                compute(u)
        with self.phase() as ph:
            acc4 = [ph.sb("acc4", [128, 4, 1024]) for ab_ in range(2)]
            ysum = ph.sb("ysum", [128, 1024])
            xg = [ph.sb("cxg", [128, 8, 512]) for _ in range(2)]
            for g in range(NG):
                b = g % 2
                self.dma('sp', xg[b][:], xT_v[:, :, g * 512:(g + 1) * 512], [('xT', g)], [('cxg', b)])
                for j in range(4):
                    ti = g * 4 + j
                    ab = ti % 2
                    for k in range(4):
                        self.S.dma('pool', lambda e, k=k, ab=ab, ti=ti: e.indirect_dma_start(
                            out=acc4[ab][:, k, :], out_offset=None, in_=Yg_d,
                            in_offset=bass.IndirectOffsetOnAxis(ap=slots_all[:, ti, k:k + 1], axis=0), bounds_check=self.bc_reg(e, R - 1), oob_is_err=False),
                            [('slots', ti)], [('acc4', ab)])
                    self.tt('dve', ysum[:], acc4[ab][:, 0, :], acc4[ab][:, 1, :], ALU.add, [('acc4', ab)], ['ysum'])
                    self.tt('dve', ysum[:], ysum[:], acc4[ab][:, 2, :], ALU.add, [('acc4', ab), 'ysum'], ['ysum'])
                    self.tt('dve', ysum[:], ysum[:], acc4[ab][:, 3, :], ALU.add, [('acc4', ab), 'ysum'], ['ysum'])
                    for kc in range(8):
                        bk = kc % 4
                        self.tr(ps[bk][:, 0:128], ysum[:, kc * 128:(kc + 1) * 128], self.ident_f[:], ['ysum'], [PS[bk]])
                        self.stt(xg[b][:, kc, j * 128:(j + 1) * 128], ps[bk][:, 0:128], G2[:, kc:kc + 1], xg[b][:, kc, j * 128:(j + 1) * 128],
                                 ALU.mult, ALU.add, [PS[bk], ('cxg', b), 'lconst'], [('cxg', b)])
                self.dma('sp', xT_v[:, :, g * 512:(g + 1) * 512], xg[b][:], [('cxg', b)], [('xT', g)])

    def p_final(self, xT_d, gfT_d, out_d):
        ps, PS = self.ps, self.PS
        with self.phase() as ph:
            gf = ph.sb("gf", [128, 8]); zb = ph.sb("zb", [128, 8])
            self.dma('sp', gf[:], gfT_d, [], ['lconst'])
            self.memset('dve', zb[:], 0.0, ['lconst'])
            xg = ph.sb("n_xg", [128, 8, 512]); sq = ph.sb("n_sq", [128, 8, 512]); rstd = ph.sb("n_rstd", [128, 512])
            yT = [ph.sb("yT", [128, 8, 512]) for _ in range(2)]
            ot = [ph.sb("ot", [128, D]) for _ in range(2)]
            for g in range(NG):
                hb = g % 2
                self.p_norm_group(ph, g, xT_d, gf, zb, yT[hb], ('yT', hb), (xg, sq, rstd))
                for j in range(4):
                    ti = g * 4 + j
                    ob = ti % 2
                    for k2 in range(2):
                        bk = k2
                        for kk in range(4):
                            kc = k2 * 4 + kk
                            self.tr(ps[bk][:, kk * 128:(kk + 1) * 128], yT[hb][:, kc, j * 128:(j + 1) * 128], self.ident_f[:], [('yT', hb)], [PS[bk]])
                        self.cp('act' if k2 else 'dve', ot[ob][:, k2 * 512:(k2 + 1) * 512], ps[bk][:], [PS[bk]], [('ot', ob)])
                    self.dma('sp', out_d[ti * 128:(ti + 1) * 128, :], ot[ob][:], [('ot', ob)], [('out', ti)])


    def p_conv_fix(self, zq0_d, halo_d, selc, cwT_d, cbT_d, cw, cb, ncb, qkfix_d, ktmfix_d, halo_packed=False):
        ps, PS, psb = self.ps, self.PS, self.psb
        with self.phase() as ph:
            zin = ph.sb("fzin", [128, 8, 131]); acc = ph.sb("facc", [128, 128]); ee = ph.sb("fee", [128, 128])
            qo = ph.sb("fqo", [128, 8, 128], BF16); kt = ph.sb("fkt", [128, 512], BF16)
            self.dma('sp', cw[:], cwT_d, [], ['cw'])
            self.dma('sp', cb[:], cbT_d, [], ['cw'])
            self.ts('dve', ncb[:], cb[:], -1.0, ALU.mult, ['cw'], ['cw'])
            if halo_packed:
                self.dma('sp', zin[:, :, 0:3], halo_d.rearrange("p (cc t) -> p cc t", t=3), [], ['fzin'])
            else:
                self.dma('sp', zin[:, :, 0:3], halo_d.rearrange("(cc p) t -> p cc t", p=128), [], ['fzin'])
            self.dma('sp', zin[:, :, 3:131], zq0_d.rearrange("(cc p) t -> p cc t", p=128), [], ['fzin'])
            self.ts('dve', zin[:, :, 0:3], zin[:, :, 0:3], selc[:, 0:1], ALU.mult, ['fzin', 'selb'], ['fzin'])
            for cc in range(8):
                self.conv_chunk(zin[:, cc, :], 128, cw, cb, ncb, cc, acc, ee, qo[:, cc, :], ['fzin'], ['fqo'])
            self.dma('sp', qkfix_d.rearrange("(cc p) t -> p cc t", p=128), qo[:], ['fqo'], ['qkfix'])
            for h in range(4):
                self.tr(psb[0][:, h * 128:(h + 1) * 128], qo[:, 4 + h, :], self.ident_b[:], ['fqo'], [PS[0]])
            self.cp('dve', kt[:], psb[0][:, 0:512], [PS[0]], ['fkt'])
            self.dma('sp', ktmfix_d, kt[:], ['fkt'], ['ktmfix'])


SCRATCH_KIND = "Internal"


def build_launch(first, last):
    B = Builder()
    B.scratch_kind = SCRATCH_KIND
    I = lambda name, shape, dt=F32: B.dram(name, shape, dt, kind="ExternalInput")
    O = lambda name, shape, dt=F32: B.dram(name, shape, dt, kind="ExternalOutput")
    T = lambda name, shape, dt=F32: B.dram(name, shape, dt, kind=SCRATCH_KIND)
    B.make_consts()
    modT = B.sbp("modT", [128, 48]); LC = B.sbp("LC", [128, 6, 8])
    gates = B.sbp("gates", [128, NTILE, 8])
    cw = B.sbp("cw", [128, 8, 4]); cb = B.sbp("cb", [128, 8]); ncb = B.sbp("ncb", [128, 8])
    Cst = B.sbp("Cst", [128, 4, 129])
    cs = B.sbp("cs", [128, NTILE, 8]); sn = B.sbp("sn", [128, NTILE, 8])
    PT = B.sbp("PT", [32, NT])
    selc = B.sbp("selc", [128, 1]); selb = B.sbp("selb", [128, 1])
    pos_d = I("pos", [128, NTILE], I32)
    B.p_rotary_tables(pos_d, cs, sn)
    if first:
        x_d = I("x", [NT, D])
        xT = O("xT_out", [D, NT])
        B.p_x_to_xT(x_d, xT)
    else:
        xT_in = I("xT_in", [D, NT]); zvo = I("zvo_in", [NT, 1024]); qkT = I("qkT_in", [D, NT], BF16); ktm = I("ktm_in", [NT, 512], BF16)
        qrot = I("qrot_in", [NT, 512]); gat_in = I("gates_in", [128, NTILE, 8])
        KTa_o = I("KTa_o", [8, 128, NT], BF16); Vaug_o = I("Vaug_o", [8, NT, 65], BF16); kmT_o = I("kmT_o", [64, 8, 16])
        KTa_p = I("KTa_p", [8, 128, NT], BF16); Vaug_p = I("Vaug_p", [8, NT, 65], BF16); kmT_p = I("kmT_p", [64, 8, 16])
        st_p = I("st_p", [128, 4, 129]); halo_p = I("halo_p", [D, 3]); zq0 = I("zq0_in", [D, 128]); modT_in = I("modT_in", [128, 48])
        sel_d = I("sel", [128, 1])
        b_g1n = I("b_g1n", [128, 8]); b_g2n = I("b_g2n", [128, 8]); b_cwT = I("b_cwT", [128, 8, 4]); b_cbT = I("b_cbT", [128, 8])
        b_gmbc = I("b_gmbc", [1, 512]); b_wout = I("b_wout", [D, D]); b_wr = I("b_wr", [D, 32]); b_brbc = I("b_brbc", [1, 32])
        b_wg = I("b_wg", [32, D, D]); b_wu = I("b_wu", [32, D, D]); b_wd = I("b_wd", [32, D, D])
        b_bgT = I("b_bgT", [128, 32, 8]); b_buT = I("b_buT", [128, 32, 8]); b_bd = I("b_bd", [32, D])
        xT = T("xT_work", [D, NT]) if last else O("xT_out", [D, NT])
        catT = T("catT", [D, NT], BF16); h2T = T("h2T", [D, NT], BF16)
        qkfix = T("qkfix", [D, 128], BF16); ktmfix = T("ktmfix", [128, 512], BF16)
        B.dma('sp', selc[:], sel_d, [], ['selb'])
        B.ts('dve', selb[:], selc[:], -1.0, ALU.add, ['selb'], ['selb'], s2=1e30, op1=ALU.mult)
        B.dma('sp', modT[:], modT_in, [], ['modT'])
        B.dma('sp', gates[:], gat_in, [], ['gates'])
        B.dma('sp', Cst[:], st_p, [], ['Cst'])
        B.ts('dve', Cst[:], Cst[:], selc[:, 0:1], ALU.mult, ['Cst', 'selb'], ['Cst'])
        B.S.barrier()
        B.p_layer_consts(modT, b_g1n, b_g2n, LC)
        B.p_conv_fix(zq0, halo_p, selc, b_cwT, b_cbT, cw, cb, ncb, qkfix, ktmfix)
        B.p_mlstm(True, qkT, ktm, zvo, gates, Cst, b_gmbc, catT, fix=(qkfix, ktmfix))
        B.p_moba_attn(qrot, KTa_o, Vaug_o, kmT_o, KTa_p, Vaug_p, kmT_p, selb, catT)
        B.p_outproj(catT, b_wout, LC[:, 2, :], xT, xT_src=xT_in)
        B.p_router(xT, LC[:, 3, :], LC[:, 4, :], b_wr, b_brbc, h2T, PT)
        B.p_moe(h2T, PT, b_wg, b_wu, b_wd, b_bgT, b_buT, b_bd, LC[:, 5, :], xT)
    if not last:
        cT = I("a_cT", [128, 8]); w_ada = I("a_w_ada", [D, 6 * D]); b_adaT = I("a_b_adaT", [128, 48])
        a_g1n = I("a_g1n", [128, 8]); a_g2n = I("a_g2n", [128, 8]); w_in = I("a_w_in", [D, N_IN])
        a_cwT = I("a_cwT", [128, 8, 4]); a_cbT = I("a_cbT", [128, 8]); a_bgate = I("a_bgate", [1, 8])
        ztm = T("ztm", [NT, N_TM]); zqk = T("zqk", [D, NT])
        zvo_o = O("zvo_out", [NT, 1024]); qkT_o = O("qkT_out", [D, NT], BF16); ktm_o = O("ktm_out", [NT, 512], BF16)
        qrot_o = O("qrot_out", [NT, 512]); gat_o = O("gates_out", [128, NTILE, 8])
        KTa = O("KTa_out", [8, 128, NT], BF16); Vaug = O("Vaug_out", [8, NT, 65], BF16); kmT = O("kmT_out", [64, 8, 16])
        st_o = O("st_out", [128, 4, 129]); halo_o = O("halo_out", [D, 3]); zq0_o = O("zq0_out", [D, 128]); modT_o = O("modT_out", [128, 48])
        B.p_mods(cT, w_ada, b_adaT, modT)
        B.dma('sp', modT_o, modT[:], ['modT'], ['modT_o'])
        B.p_layer_consts(modT, a_g1n, a_g2n, LC)
        B.p_inproj(0, xT, w_in, LC[:, 0, :], LC[:, 1, :], ztm, zqk, zvo_o)
        B.dma('sp', halo_o, zqk[:, NT - 3:NT], [], ['halo_o'])
        B.dma('sp', zq0_o, zqk[:, 0:128], [], ['zq0_o'])
        B.p_mlstm_prep(zqk, ztm, a_cwT, a_cbT, a_bgate, qkT_o, ktm_o, gates, cw, cb, ncb)
        B.dma('sp', gat_o, gates[:], ['gates'], ['gat_o'])
        B.memset('dve', Cst[:], 0.0, ['Cst'])
        B.p_mlstm(False, qkT_o, ktm_o, ztm, gates, Cst)
        B.dma('sp', st_o, Cst[:], ['Cst'], ['st_o'])
        B.p_moba_prep(ztm, cs, sn, qrot_o, KTa, Vaug, kmT)
    else:
        gfT = I("gfT", [128, 8]); out_d = O("out", [NT, D])
        B.p_final(xT, gfT, out_d)
    B.finish()
    return B


MOE_BLOCKS = 64
MOE_BS = 512
MOE_SPARSE_CAP = 0


def build_fused(ncores=8):
    B = Builder()
    I = lambda name, shape, dt=F32: B.dram(name, shape, dt, kind="ExternalInput")
    O = lambda name, shape, dt=F32: B.dram(name, shape, dt, kind="ExternalOutput")
    T = lambda name, shape, dt=F32: B.dram(name, shape, dt, kind="Internal")
    B.make_consts()
    modT = B.sbp("modT", [128, 48]); LC = B.sbp("LC", [128, 6, 8])
    gates = B.sbp("gates", [128, NTILE, 8])
    cw = B.sbp("cw", [128, 8, 4]); cb = B.sbp("cb", [128, 8]); ncb = B.sbp("ncb", [128, 8])
    Cst = B.sbp("Cst", [128, 4, 129])
    cs = B.sbp("cs", [128, NTILE, 8]); sn = B.sbp("sn", [128, NTILE, 8])
    CAPS = MOE_SPARSE_CAP
    NBLK = MOE_BLOCKS
    if NBLK:
        PT = None
        slots_all = B.sbp("slots_all", [128, NTILE, 4], I32); widx = B.sbp("widx", [128, NBLK, 8], I32); besb = B.sbp("besb", [128, NBLK])
        Xg = T("Xg", [NBLK * MOE_BS, 1026], BF16); Yg = T("Yg", [NBLK * MOE_BS, 1024]); h2rows = T("h2rows", [NT, 1024], BF16)
    elif CAPS:
        PT = None
        slots_all = B.sbp("slots_all", [128, NTILE, 4], I32)
        Xg = T("Xg", [32 * CAPS, 1026], BF16); Yg = T("Yg", [32 * CAPS, 1024])
    else:
        PT = B.sbp("PT", [32, NT])
    selc = B.sbp("selc", [128, 1]); selb = B.sbp("selb", [128, 1])
    pos_d = I("pos", [128, NTILE], I32)
    sel_d = I("sel", [128, 1])
    x_d = I("x", [NT, D]); cT = I("cT", [128, 8]); gfT = I("gfT", [128, 8]); out_d = O("out", [NT, D])
    W = []
    for l in range(2):
        p = "l%d_" % l
        W.append(dict(
            w_ada=I(p + "w_ada", [D, 6 * D]), b_adaT=I(p + "b_adaT", [128, 48]), g1n=I(p + "g1n", [128, 8]), g2n=I(p + "g2n", [128, 8]),
            w_in=I(p + "w_in", [D, N_IN]), cwT=I(p + "cwT", [128, 8, 4]), cbT=I(p + "cbT", [128, 8]), bgate=I(p + "bgate", [1, 8]),
            gmbc=I(p + "gmbc", [1, 512]), wout=I(p + "wout", [D, D]), wr=I(p + "wr", [D, 32]), brbc=I(p + "brbc", [1, 32]),
            wg=I(p + "wg", [32, D, D]), wu=I(p + "wu", [32, D, D]), wd=I(p + "wd", [32, D, D]),
            bgT=I(p + "bgT", [128, 32, 8]), buT=I(p + "buT", [128, 32, 8]), bd=I(p + "bd", [32, D])))
    xT = T("xT", [D, NT])
    ztm = T("ztm", [NT, N_TM]); zqk = T("zqk", [D, NT]); qkT = T("qkT", [D, NT], BF16); ktm = T("ktm", [NT, 512], BF16)
    qrot = T("qrot", [NT, 512]); catT = T("catT", [D, NT], BF16); h2T = T("h2T", [D, NT], BF16)
    qkfix = T("qkfix", [D, 128], BF16); ktmfix = T("ktmfix", [128, 512], BF16)
    ex_K = T("ex_K", [8 * 128, NT], BF16); ga_K = T("ga_K", [2 * 8 * 128, NT], BF16)
    VR = 8 * NT * 65 // 512
    ex_V = T("ex_V", [VR, 512], BF16); ga_V = T("ga_V", [2 * VR, 512], BF16)
    ex_S = T("ex_S", [128, 668]); ga_S = T("ga_S", [256, 668])
    KTa_o = ex_K.rearrange("(h r) t -> h r t", r=128)
    Vaug_o = ex_V.rearrange("r c -> (r c)").rearrange("(h n c) -> h n c", h=8, c=65)
    KTa_p = ga_K.rearrange("(h s r) t -> h s r t", h=8, s=2)[:, 0]
    Vaug_p = ga_V.rearrange("(h s r) c -> h s (r c)", h=8, s=2)[:, 0].rearrange("h (n c) -> h n c", c=65)
    gaK2 = ga_K.rearrange("(h s r) t -> h (s r) t", h=8, s=2)
    gaV2 = ga_V.rearrange("(h s r) c -> h (s r) c", h=8, s=2)
    exV2 = ex_V.rearrange("(h r) c -> h r c", h=8)
    ex_list = [(ex_K[h * 128:(h + 1) * 128, :], gaK2[h]) for h in range(8)] + [(exV2[h], gaV2[h]) for h in range(8)] + [(ex_S, ga_S)]
    st_o = ex_S[:, 0:516].rearrange("p (h c) -> p h c", c=129)
    kmT_o = ex_S[0:64, 516:644].rearrange("p (h n) -> p h n", n=16)
    halo_o = ex_S[:, 644:668]
    st_p = ga_S[0:128, 0:516].rearrange("p (h c) -> p h c", c=129)
    kmT_p = ga_S[0:64, 516:644].rearrange("p (h n) -> p h n", n=16)
    halo_p = ga_S[0:128, 644:668]
    groups = [[2 * i, 2 * i + 1] for i in range(ncores // 2)]

    B.dma('sp', selc[:], sel_d, [], ['selb'])
    B.ts('dve', selb[:], selc[:], -1.0, ALU.add, ['selb'], ['selb'], s2=1e30, op1=ALU.mult)
    B.p_rotary_tables(pos_d, cs, sn)
    B.p_x_to_xT(x_d, xT)
    for l in range(2):
        w = W[l]
        B.p_mods(cT, w['w_ada'], w['b_adaT'], modT)
        B.p_layer_consts(modT, w['g1n'], w['g2n'], LC)
        B.p_inproj(l, xT, w['w_in'], LC[:, 0, :], LC[:, 1, :], ztm, zqk)
        B.dma('sp', halo_o.rearrange("p (cc t) -> p cc t", t=3), zqk[:, NT - 3:NT].rearrange("(cc p) t -> p cc t", p=128), [], ['halo_o'])
        B.p_mlstm_prep(zqk, ztm, w['cwT'], w['cbT'], w['bgate'], qkT, ktm, gates, cw, cb, ncb)
        B.memset('dve', Cst[:], 0.0, ['Cst'])
        B.p_mlstm(False, qkT, ktm, ztm, gates, Cst)
        B.dma('sp', st_o, Cst[:], ['Cst'], ['st_o'])
        B.p_moba_prep(ztm, cs, sn, qrot, KTa_o, Vaug_o, kmT_o)
        B.S.barrier()
        for src_, dst_ in ex_list:
            B.S.collective(lambda e, src_=src_, dst_=dst_: e.collective_compute("AllGather", ALU.bypass, replica_groups=groups,
                                                                              ins=[src_.opt()], outs=[dst_.opt()]), 1)
        B.S.barrier()
        B.dma('sp', Cst[:], st_p, [], ['Cst'])
        B.ts('dve', Cst[:], Cst[:], selc[:, 0:1], ALU.mult, ['Cst', 'selb'], ['Cst'])
        B.S.barrier()
        B.p_conv_fix(zqk[:, 0:128], halo_p, selc, w['cwT'], w['cbT'], cw, cb, ncb, qkfix, ktmfix, halo_packed=True)
        B.p_mlstm(True, qkT, ktm, ztm, gates, Cst, w['gmbc'], catT, fix=(qkfix, ktmfix))
        B.p_moba_attn(qrot, KTa_o, Vaug_o, kmT_o, KTa_p, Vaug_p, kmT_p, selb, catT)
        B.p_outproj(catT, w['wout'], LC[:, 2, :], xT)
        if NBLK:
            B.p_router_blocks(xT, LC[:, 3, :], LC[:, 4, :], w['wr'], w['brbc'], Xg, h2rows, slots_all, widx, besb, NBLK, MOE_BS)
            B.p_moe_blocks(Xg, Yg, slots_all, widx, besb, w['wg'], w['wu'], w['wd'], w['bgT'], w['buT'], w['bd'], LC[:, 5, :], xT, NBLK, MOE_BS)
        elif CAPS:
            B.p_router_sparse(xT, LC[:, 3, :], LC[:, 4, :], w['wr'], w['brbc'], Xg, slots_all, CAPS, None)
            B.p_moe_sparse(Xg, Yg, slots_all, w['wg'], w['wu'], w['wd'], w['bgT'], w['buT'], w['bd'], LC[:, 5, :], xT, CAPS)
        else:
            B.p_router(xT, LC[:, 3, :], LC[:, 4, :], w['wr'], w['brbc'], h2T, PT)
            B.p_moe(h2T, PT, w['wg'], w['wu'], w['wd'], w['bgT'], w['buT'], w['bd'], LC[:, 5, :], xT)
    B.p_final(xT, gfT, out_d)
    B.finish()
    return B


def _fused_inputs(inp, c):
    b, s = c // 2, c & 1
    m = {"pos": np.ascontiguousarray(np.asarray(inp['positions'][b, s * NT:(s + 1) * NT], np.int32).reshape(NTILE, 128).T),
         "sel": np.full((128, 1), float(s), np.float32), "x": np.ascontiguousarray(inp['x'][b, s * NT:(s + 1) * NT]),
         "cT": colT(inp['c'][b]), "gfT": colT(inp['final_norm_g'])}
    for l in range(2):
        p = "l%d_" % l
        a = _a_weights(inp, l, b)
        bw = _b_weights(inp, l)
        m.update({p + "w_ada": a["a_w_ada"], p + "b_adaT": a["a_b_adaT"], p + "g1n": a["a_g1n"], p + "g2n": a["a_g2n"], p + "w_in": a["a_w_in"],
                  p + "cwT": a["a_cwT"], p + "cbT": a["a_cbT"], p + "bgate": a["a_bgate"], p + "gmbc": bw["b_gmbc"], p + "wout": bw["b_wout"],
                  p + "wr": bw["b_wr"], p + "brbc": bw["b_brbc"], p + "wg": bw["b_wg"], p + "wu": bw["b_wu"], p + "wd": bw["b_wd"],
                  p + "bgT": bw["b_bgT"], p + "buT": bw["b_buT"], p + "bd": bw["b_bd"]})
    return m


def _fused_pipeline(inp, NCORE=8):
    inp = {k: np.asarray(v) for k, v in inp.items()}
    if 'fused' not in _PROGS:
        _PROGS['fused'] = build_fused(NCORE)
    prog = _PROGS['fused']
    cores = list(range(NCORE))
    shared = {}
    ims = []
    for c in cores:
        m = _fused_inputs(inp, c)
        for k in list(m.keys()):
            if k[0] == 'l' and k[2] == '_':
                m[k] = shared.setdefault(k, m[k])
        ims.append(m)
    r = _run(prog, ims, cores)
    out = np.zeros((inp['x'].shape[0], 2 * NT, D), np.float32)
    for c in cores:
        out[c // 2, (c & 1) * NT:((c & 1) + 1) * NT] = r[c]["out"]
    return out


_PROGS = {}


def get_prog(first, last):
    k = (first, last)
    if k not in _PROGS:
        _PROGS[k] = build_launch(first, last)
    return _PROGS[k]


def _bT(b):
    return np.ascontiguousarray(np.asarray(b, np.float32).reshape(32, 8, 128).transpose(2, 0, 1))


def _a_weights(inp, l, b):
    return {"a_cT": colT(inp['c'][b]), "a_w_ada": np.ascontiguousarray(inp['w_ada'][l]), "a_b_adaT": colT(inp['b_ada'][l]),
            "a_g1n": colT(inp['norm1_g'][l]), "a_g2n": colT(inp['norm2_g'][l]), "a_w_in": np.ascontiguousarray(inp['w_in'][l]),
            "a_cwT": np.ascontiguousarray(np.asarray(inp['conv_w'][l], np.float32).T.reshape(8, 128, 4).transpose(1, 0, 2)),
            "a_cbT": colT(inp['conv_b'][l]),
            "a_bgate": np.concatenate([inp['b_igate'][l], inp['b_fgate'][l]])[None, :].astype(np.float32)}


def _b_weights(inp, l):
    return {"b_g1n": colT(inp['norm1_g'][l]), "b_g2n": colT(inp['norm2_g'][l]),
            "b_cwT": np.ascontiguousarray(np.asarray(inp['conv_w'][l], np.float32).T.reshape(8, 128, 4).transpose(1, 0, 2)),
            "b_cbT": colT(inp['conv_b'][l]), "b_gmbc": np.asarray(inp['mlstm_norm_g'][l], np.float32)[None, :],
            "b_wout": np.ascontiguousarray(inp['w_out'][l]), "b_wr": np.ascontiguousarray(inp['w_router'][l]),
            "b_brbc": np.asarray(inp['b_router'][l], np.float32)[None, :],
            "b_wg": np.ascontiguousarray(inp['w_gate'][l]), "b_wu": np.ascontiguousarray(inp['w_up'][l]), "b_wd": np.ascontiguousarray(inp['w_down'][l]),
            "b_bgT": _bT(inp['b_gate'][l]), "b_buT": _bT(inp['b_up'][l]), "b_bd": np.ascontiguousarray(inp['b_down'][l])}


def _handoff(prev, c):
    o = prev[c]
    p = prev[c ^ 1]
    return {"xT_in": o["xT_out"], "zvo_in": o["zvo_out"], "qkT_in": o["qkT_out"], "ktm_in": o["ktm_out"], "qrot_in": o["qrot_out"],
            "gates_in": o["gates_out"], "KTa_o": o["KTa_out"], "Vaug_o": o["Vaug_out"], "kmT_o": o["kmT_out"],
            "KTa_p": p["KTa_out"], "Vaug_p": p["Vaug_out"], "kmT_p": p["kmT_out"], "st_p": p["st_out"], "halo_p": p["halo_out"],
            "zq0_in": o["zq0_out"], "modT_in": o["modT_out"], "sel": np.full((128, 1), float(c & 1), np.float32)}


def kernel(**inp):
    return _fused_pipeline(inp, 8)


def _run(prog, ims, cores):
    import time
    t0 = time.time()
    res = run_bass_kernel_spmd(prog.nc, ims, core_ids=cores).results
    print("[kernel] launch done in %.1fs" % (time.time() - t0), flush=True)
    return res


def _pipeline(inp, NCORE):
    inp = {k: np.asarray(v) for k, v in inp.items()}
    x = inp['x']
    Bn = x.shape[0]
    pos = [np.ascontiguousarray(np.asarray(inp['positions'][c // 2, (c & 1) * NT:((c & 1) + 1) * NT], np.int32).reshape(NTILE, 128).T)
           for c in range(NCORE)]
    cores = list(range(NCORE))
    p1 = get_prog(True, False)
    ims = []
    for c in cores:
        b, s = c // 2, c & 1
        m = {"pos": pos[c], "x": np.ascontiguousarray(x[b, s * NT:(s + 1) * NT])}
        m.update(_a_weights(inp, 0, b))
        ims.append(m)
    r = _run(p1, ims, cores)
    p2 = get_prog(False, False)
    ims = []
    for c in cores:
        m = {"pos": pos[c]}
        m.update(_handoff(r, c))
        m.update(_b_weights(inp, 0))
        m.update(_a_weights(inp, 1, c // 2))
        ims.append(m)
    r = _run(p2, ims, cores)
    p3 = get_prog(False, True)
    ims = []
    for c in cores:
        m = {"pos": pos[c], "gfT": colT(inp['final_norm_g'])}
        m.update(_handoff(r, c))
        m.update(_b_weights(inp, 1))
        ims.append(m)
    r = _run(p3, ims, cores)
    out = np.zeros((Bn, 2 * NT, D), np.float32)
    for c in cores:
        out[c // 2, (c & 1) * NT:((c & 1) + 1) * NT] = r[c]["out"]
    return out


def colT(v):
    v = np.asarray(v, dtype=np.float32)
    return np.ascontiguousarray(v.reshape(-1, 128).T)
```
